# Optimizing a Trainium2 kernel written in Bass

```python
import jax, jax.numpy as jnp
from jax import lax
import numpy as np

D_MODEL = 1024
BATCH = 2
SEQ = 8192
DEPTH = 4

GRID_W = 64
CTX_LEN = 256
N_EVEN = (DEPTH + 1) // 2
N_ODD = DEPTH // 2
EPS = 1e-6

ML_HEADS = 4
ML_DK = 64
ML_DV = 128
ML_CHUNK = 64
SC_WIDTH = D_MODEL // 2
CONV_K = 3
HYB_SIZES = (ML_HEADS * ML_DK, ML_HEADS * ML_DK, ML_HEADS * ML_DV, ML_HEADS * ML_DV, 4 * ML_HEADS, SC_WIDTH, SC_WIDTH, SC_WIDTH)
HYB_IN = sum(HYB_SIZES)
HYB_OUT = ML_HEADS * ML_DV + SC_WIDTH
ATT_HEADS = 16
KV_HEADS = 4
HEAD_DIM = 64
ATT_IN = (ATT_HEADS + 2 * KV_HEADS) * HEAD_DIM
Q_BLOCK = 128
ROPE_THETA = 10000.0
MLP_HIDDEN = 4 * D_MODEL

kernel_name = "hybrid_mlstm_conv_gqa_dit_trunk"


def rms_norm(x, g):
    xf = x.astype(jnp.float32)
    y = xf * lax.rsqrt(jnp.mean(xf * xf, axis=-1, keepdims=True) + EPS)
    return (y * g.astype(jnp.float32)).astype(x.dtype)


def sqrelu_mlp(h, w1, w2):
    return jnp.square(jax.nn.relu(h @ w1)) @ w2


def axial_rope_tables(n_rows):
    half = HEAD_DIM // 2
    inv = ROPE_THETA ** (-jnp.arange(0, half, 2, dtype=jnp.float32) / half)
    t = jnp.arange(n_rows * GRID_W)
    row = (t // GRID_W).astype(jnp.float32)
    col = (t % GRID_W).astype(jnp.float32)
    ang = jnp.concatenate([row[:, None] * inv, col[:, None] * inv], axis=-1)
    return jnp.cos(ang), jnp.sin(ang)


def _rotate_half(part, cs, sn):
    n = part.shape[-1] // 2
    p1, p2 = part[..., :n], part[..., n:]
    return jnp.concatenate([p1 * cs - p2 * sn, p2 * cs + p1 * sn], axis=-1)


def apply_axial_rope(x, cos, sin):
    half, quarter = HEAD_DIM // 2, HEAD_DIM // 4
    xf = x.astype(jnp.float32)
    cs, sn = cos[None, :, None, :], sin[None, :, None, :]
    row = _rotate_half(xf[..., :half], cs[..., :quarter], sn[..., :quarter])
    col = _rotate_half(xf[..., half:], cs[..., quarter:], sn[..., quarter:])
    return jnp.concatenate([row, col], axis=-1).astype(x.dtype)


def mlstm_chunk_states(k, v, ig, lf, state0):
    b = jnp.cumsum(lf, axis=-1)
    b_end = b[..., -1]
    logw = b_end[..., None] - b + ig
    m_loc = jnp.max(logw, axis=-1)
    w = jnp.exp(logw - m_loc[..., None])
    c_loc = jnp.einsum('bhcsv,bhcsk->bhcvk', v * w[..., None], k)
    n_loc = jnp.einsum('bhcs,bhcsk->bhck', w, k)

    def step(carry, inp):
        c_st, n_st, m_st = carry
        bl, ml, cl, nl = inp
        m_new = jnp.maximum(bl + m_st, ml)
        a = jnp.exp(bl + m_st - m_new)
        g = jnp.exp(ml - m_new)
        c_new = a[..., None, None] * c_st + g[..., None, None] * cl
        n_new = a[..., None] * n_st + g[..., None] * nl
        return (c_new, n_new, m_new), (c_st, n_st, m_st)

    xs = tuple(jnp.moveaxis(t, 2, 0) for t in (b_end, m_loc, c_loc, n_loc))
    final, starts = lax.scan(step, state0, xs)
    starts = tuple(jnp.moveaxis(t, 0, 2) for t in starts)
    return starts, final


def mlstm_chunk_outputs(q, k, v, ig, lf, starts):
    c0, n0, m0 = starts
    b = jnp.cumsum(lf, axis=-1)
    length = q.shape[-2]
    tri = jnp.tril(jnp.ones((length, length), dtype=bool))
    log_d = jnp.where(tri, b[..., :, None] - b[..., None, :] + ig[..., None, :], -jnp.inf)
    m_inter = b + m0[..., None]
    m = jnp.maximum(m_inter, jnp.max(log_d, axis=-1))
    d = jnp.exp(log_d - m[..., None])
    a = jnp.exp(m_inter - m)
    s = jnp.einsum('bhcjd,bhcsd->bhcjs', q, k) * d
    num = jnp.einsum('bhcjs,bhcsv->bhcjv', s, v) + a[..., None] * jnp.einsum('bhcvd,bhcjd->bhcjv', c0, q)
    den = jnp.sum(s, axis=-1) + a * jnp.einsum('bhcd,bhcjd->bhcj', n0, q)
    return num / jnp.maximum(jnp.abs(den), jnp.exp(-m))[..., None]


def mlstm_direction(q, k, v, ig, lf, state0, reverse, need_h):
    if reverse:
        q, k, v, ig, lf = (jnp.flip(t, axis=2) for t in (q, k, v, ig, lf))
    bsz, nh, n, _ = q.shape
    nc = n // ML_CHUNK
    chunk = lambda t: t.reshape(bsz, nh, nc, ML_CHUNK, *t.shape[3:])
    q, k, v, ig, lf = (chunk(t) for t in (q, k, v, ig, lf))
    starts, final = mlstm_chunk_states(k, v, ig, lf, state0)
    if not need_h:
        return None, final
    h = mlstm_chunk_outputs(q, k, v, ig, lf, starts).reshape(bsz, nh, n, ML_DV)
    if reverse:
        h = jnp.flip(h, axis=2)
    return h, final


def short_conv(u, w):
    return lax.conv_general_dilated(u, w[:, None, :].astype(u.dtype), window_strides=(1,),
                                    padding=[(CONV_K // 2, CONV_K // 2)],
                                    dimension_numbers=('NWC', 'WIO', 'NWC'),
                                    feature_group_count=u.shape[-1])


def hybrid_mixer(hc, hx, w_in, gate_b, mnorm_g, conv_w, w_out, with_ctx):
    splits = np.cumsum(HYB_SIZES)[:-1].tolist()

    def project(h):
        bsz, n, _ = h.shape
        q, k, v, o, g, gb, gc, u = jnp.split(h @ w_in, splits, axis=-1)
        heads = lambda t, dd: t.reshape(bsz, n, ML_HEADS, dd).transpose(0, 2, 1, 3).astype(jnp.float32)
        g = (g.astype(jnp.float32) + gate_b.astype(jnp.float32)).reshape(bsz, n, 4, ML_HEADS).transpose(2, 0, 3, 1)
        fwd = (g[0], jax.nn.log_sigmoid(g[1]))
        bwd = (g[2], jax.nn.log_sigmoid(g[3]))
        qkv = (heads(q, ML_DK) * ML_DK ** -0.5, heads(k, ML_DK), heads(v, ML_DV))
        return qkv, fwd, bwd, (o, gb, gc, u)

    def combine(h, side):
        o, gb, gc, u = side
        bsz, n, _ = o.shape
        hn = rms_norm(h.transpose(0, 2, 1, 3), mnorm_g.reshape(ML_HEADS, ML_DV))
        og = jax.nn.sigmoid(o.astype(jnp.float32)).reshape(bsz, n, ML_HEADS, ML_DV)
        m_out = (hn * og).reshape(bsz, n, ML_HEADS * ML_DV).astype(o.dtype)
        c_out = gb * short_conv(gc * u, conv_w)
        return jnp.concatenate([m_out, c_out], axis=-1) @ w_out

    qkv_c, fwd_c, bwd_c, side_c = project(hc)
    qkv_x, fwd_x, bwd_x, side_x = project(hx)
    bsz = hx.shape[0]
    zero = (jnp.zeros((bsz, ML_HEADS, ML_DV, ML_DK), jnp.float32),
            jnp.zeros((bsz, ML_HEADS, ML_DK), jnp.float32),
            jnp.zeros((bsz, ML_HEADS), jnp.float32))
    h_cf, s_f = mlstm_direction(*qkv_c, *fwd_c, zero, False, with_ctx)
    h_cb, s_b = mlstm_direction(*qkv_c, *bwd_c, zero, True, with_ctx)
    h_xf, _ = mlstm_direction(*qkv_x, *fwd_x, s_f, False, True)
    h_xb, _ = mlstm_direction(*qkv_x, *bwd_x, s_b, True, True)
    y_x = combine(h_xf + h_xb, side_x)
    y_c = combine(h_cf + h_cb, side_c) if with_ctx else None
    return y_c, y_x


def gqa_attend(q, k, v):
    bsz, nq, _, _ = q.shape
    qg = q.reshape(bsz, nq, KV_HEADS, ATT_HEADS // KV_HEADS, HEAD_DIM)
    s = jnp.einsum('bqhgd,bkhd->bhgqk', qg, k).astype(jnp.float32) * HEAD_DIM ** -0.5
    p = jax.nn.softmax(s, axis=-1).astype(v.dtype)
    o = jnp.einsum('bhgqk,bkhd->bqhgd', p, v)
    return o.reshape(bsz, nq, ATT_HEADS * HEAD_DIM)


def attention_mixer(hc, hx, w_in, q_g, k_g, w_out, cos, sin, with_ctx):
    bsz, n, _ = hx.shape
    nq = ATT_HEADS * HEAD_DIM
    heads = lambda p, nh: p.reshape(bsz, p.shape[1], nh, HEAD_DIM)

    def kv(p):
        k, v = jnp.split(p, 2, axis=-1)
        return rms_norm(heads(k, KV_HEADS), k_g), heads(v, KV_HEADS)

    px = hx @ w_in
    qx = apply_axial_rope(rms_norm(heads(px[..., :nq], ATT_HEADS), q_g), cos, sin)
    kx, vx = kv(px[..., nq:])
    kx = apply_axial_rope(kx, cos, sin)
    if with_ctx:
        pc = hc @ w_in
        qc = rms_norm(heads(pc[..., :nq], ATT_HEADS), q_g)
        kc, vc = kv(pc[..., nq:])
    else:
        kc, vc = kv(hc @ w_in[:, nq:])
    k_all = jnp.concatenate([kc, kx], axis=1)
    v_all = jnp.concatenate([vc, vx], axis=1)
    nb = n // Q_BLOCK
    q_blocks = qx.reshape(bsz, nb, Q_BLOCK, ATT_HEADS, HEAD_DIM).swapaxes(0, 1)
    o_x = lax.map(lambda qb: gqa_attend(qb, k_all, v_all), q_blocks)
    y_x = o_x.swapaxes(0, 1).reshape(bsz, n, nq) @ w_out
    y_c = gqa_attend(qc, kc, vc) @ w_out if with_ctx else None
    return y_c, y_x


def setup_inputs(seed: int = 0) -> dict:
    key = jax.random.key(seed)
    ks = jax.random.split(key, 20)
    nrm = lambda k, shape, s: jax.random.normal(k, shape, jnp.float32) * s
    d = D_MODEL
    ig_b = nrm(ks[11], (N_EVEN, 2, ML_HEADS), 0.1)
    fg_b = jnp.linspace(3.0, 6.0, ML_HEADS, dtype=jnp.float32)[None, None, :] + nrm(ks[12], (N_EVEN, 2, ML_HEADS), 0.1)
    return {
        "x": nrm(ks[0], (BATCH, SEQ, d), 1.0),
        "c": nrm(ks[1], (BATCH, d), 1.0),
        "ctx": nrm(ks[2], (BATCH, CTX_LEN, d), 1.0),
        "c_ctx": nrm(ks[3], (d,), 1.0),
        "ada_w": nrm(ks[4], (DEPTH, d, 6 * d), d ** -0.5),
        "ada_b": nrm(ks[5], (DEPTH, 6 * d), 0.02),
        "norm1_g": 1.0 + nrm(ks[6], (DEPTH, d), 0.05),
        "norm2_g": 1.0 + nrm(ks[7], (DEPTH, d), 0.05),
        "mlp_w1": nrm(ks[8], (DEPTH, d, MLP_HIDDEN), d ** -0.5),
        "mlp_w2": nrm(ks[9], (DEPTH, MLP_HIDDEN, d), MLP_HIDDEN ** -0.5),
        "hyb_w_in": nrm(ks[10], (N_EVEN, d, HYB_IN), d ** -0.5),
        "hyb_gate_b": jnp.stack([ig_b, fg_b], axis=2).reshape(N_EVEN, 4 * ML_HEADS),
        "mlstm_norm_g": 1.0 + nrm(ks[13], (N_EVEN, ML_HEADS * ML_DV), 0.05),
        "conv_w": nrm(ks[14], (N_EVEN, CONV_K, SC_WIDTH), CONV_K ** -0.5),
        "hyb_w_out": nrm(ks[15], (N_EVEN, HYB_OUT, d), HYB_OUT ** -0.5),
        "att_w_in": nrm(ks[16], (N_ODD, d, ATT_IN), d ** -0.5),
        "q_norm_g": 1.0 + nrm(ks[17], (N_ODD, HEAD_DIM), 0.05),
        "k_norm_g": 1.0 + nrm(ks[18], (N_ODD, HEAD_DIM), 0.05),
        "att_w_out": nrm(ks[19], (N_ODD, ATT_HEADS * HEAD_DIM, d), (ATT_HEADS * HEAD_DIM) ** -0.5),
    }


def reference(x, c, ctx, c_ctx, ada_w, ada_b, norm1_g, norm2_g, mlp_w1, mlp_w2, hyb_w_in, hyb_gate_b,
              mlstm_norm_g, conv_w, hyb_w_out, att_w_in, q_norm_g, k_norm_g, att_w_out):
    rows = x.shape[1] // GRID_W
    cos, sin = axial_rope_tables(rows)
    silu_c = jax.nn.silu(c)
    silu_cc = jax.nn.silu(c_ctx)
    s_c = ctx
    for layer in range(DEPTH):
        last = layer == DEPTH - 1
        mod_x = jnp.split((silu_c @ ada_w[layer] + ada_b[layer])[:, None, :], 6, axis=-1)
        mod_c = jnp.split((silu_cc @ ada_w[layer] + ada_b[layer])[None, None, :], 6, axis=-1)
        hx = rms_norm(x, norm1_g[layer]) * (1 + mod_x[1]) + mod_x[0]
        hc = rms_norm(s_c, norm1_g[layer]) * (1 + mod_c[1]) + mod_c[0]
        if layer % 2 == 0:
            e = layer // 2
            y_c, y_x = hybrid_mixer(hc, hx, hyb_w_in[e], hyb_gate_b[e], mlstm_norm_g[e], conv_w[e], hyb_w_out[e], not last)
        else:
            o = layer // 2
            y_c, y_x = attention_mixer(hc, hx, att_w_in[o], q_norm_g[o], k_norm_g[o], att_w_out[o], cos, sin, not last)
        x = x + mod_x[2] * y_x
        x = x + mod_x[5] * sqrelu_mlp(rms_norm(x, norm2_g[layer]) * (1 + mod_x[4]) + mod_x[3], mlp_w1[layer], mlp_w2[layer])
        if not last:
            s_c = s_c + mod_c[2] * y_c
            s_c = s_c + mod_c[5] * sqrelu_mlp(rms_norm(s_c, norm2_g[layer]) * (1 + mod_c[4]) + mod_c[3], mlp_w1[layer], mlp_w2[layer])
    return x
```

```python
import contextlib
import numpy as np
import ml_dtypes
import concourse.bass as bass
import concourse.mybir as mybir
from concourse.bass_utils import run_bass_kernel_spmd

F32 = mybir.dt.float32
BF16 = mybir.dt.bfloat16
AF = mybir.ActivationFunctionType
ALU = mybir.AluOpType
AX = mybir.AxisListType
NPBF = ml_dtypes.bfloat16

D = 1024
SEQ = 8192
CTX = 256
NCORE = 8
LAT = 2048
NTOK = CTX + LAT
NT = NTOK // 128
HID = 4096
EPS = 1e-6
EPOCH = 20000


class Prog:
    ENG = ("pe", "act", "dve", "pool", "sp")

    def __init__(self):
        self.nc = bass.Bass("TRN2", target_bir_lowering=False)
        self.q = {e: [] for e in self.ENG}
        self.cnt = {e: 0 for e in self.ENG}
        self.esem = {e: [] for e in self.ENG}
        self.known = {e: {} for e in self.ENG}
        self.sems = []
        self.lastw = {}
        self.readers = {}
        self.dslot = {}
        self.uid = 0

    def new_sem(self, name):
        s = self.nc.alloc_semaphore(name)
        self.sems.append(s)
        return len(self.sems) - 1

    def sb(self, name, shape, dtype):
        return self.nc.alloc_sbuf_tensor(name, list(shape), dtype)

    def ps(self, name, shape, dtype):
        return self.nc.alloc_psum_tensor(name, list(shape), dtype)

    def dram_in(self, name, shape, dtype):
        return self.nc.dram_tensor(name, list(shape), dtype, kind="ExternalInput")

    def dram_out(self, name, shape, dtype):
        return self.nc.dram_tensor(name, list(shape), dtype, kind="ExternalOutput")

    def _needs(self, reads, writes):
        need = {}

        def add(tok):
            if tok is None:
                return
            s, v = tok
            if need.get(s, 0) < v:
                need[s] = v
        for r in reads:
            add(self.lastw.get(r))
        for w in writes:
            add(self.lastw.get(w))
            for s, v in self.readers.get(w, {}).items():
                add((s, v))
        return need

    def _emit_waits(self, eng, need, own_sems=()):
        kn = self.known[eng]
        for s, v in need.items():
            if s in own_sems:
                continue
            if kn.get(s, 0) >= v:
                continue
            kn[s] = v
            self.q[eng].append(("wait", s, v))

    def _record(self, tok, reads, writes):
        for r in reads:
            d = self.readers.setdefault(r, {})
            if d.get(tok[0], 0) < tok[1]:
                d[tok[0]] = tok[1]
        for w in writes:
            self.lastw[w] = tok
            self.readers[w] = {}

    def op(self, eng, fn, reads=(), writes=()):
        need = self._needs(reads, writes)
        own = tuple(self.esem[eng]) if eng == "pe" else ()
        self._emit_waits(eng, need, own)
        idx = self.cnt[eng]
        self.cnt[eng] = idx + 1
        ep = idx // EPOCH
        while len(self.esem[eng]) <= ep:
            self.esem[eng].append(self.new_sem("e_%s_%d" % (eng, len(self.esem[eng]))))
        s = self.esem[eng][ep]
        tok = (s, idx - ep * EPOCH + 1)
        self.q[eng].append(("op", fn, s, 1))
        self._record(tok, reads, writes)
        return tok

    def dma(self, eng, out, in_, slot, reads=(), writes=()):
        if slot not in self.dslot:
            self.dslot[slot] = [self.new_sem("d%d" % len(self.dslot)), 0]
        s, c = self.dslot[slot]
        need = self._needs(reads, writes)
        if c > 0 and need.get(s, 0) < c:
            need[s] = c
        self._emit_waits(eng, need)
        tok = (s, c + 16)
        self.dslot[slot][1] = c + 16
        self.q[eng].append(("op", lambda e, o=out, i=in_: e.dma_start(out=o, in_=i), s, 16))
        self._record(tok, reads, writes)
        return tok

    def finish(self):
        for slot, (s, c) in self.dslot.items():
            if c > 0 and self.known["sp"].get(s, 0) < c:
                self.known["sp"][s] = c
                self.q["sp"].append(("wait", s, c))
        nc = self.nc
        sems = self.sems

        def replay(items, e):
            for it in items:
                if it[0] == "wait":
                    e.wait_ge(sems[it[1]], it[2])
                else:
                    ins = it[1](e)
                    ins.then_inc(sems[it[2]], it[3])

        with nc.Block() as block:
            @block.tensor
            def _(e):
                replay(self.q["pe"], e)

            @block.scalar
            def _(e):
                replay(self.q["act"], e)

            @block.vector
            def _(e):
                replay(self.q["dve"], e)

            @block.gpsimd
            def _(e):
                replay(self.q["pool"], e)

            @block.sync
            def _(e):
                replay(self.q["sp"], e)
        return nc


def run(prog, in_maps):
    nc = prog.finish()
    res = run_bass_kernel_spmd(nc, in_maps, core_ids=list(range(NCORE)))
    return res.results


def build_mods():
    P = Prog()
    CW = 6144 // NCORE
    cT = P.dram_in("cT", [128, 8, 4], F32)
    aw = P.dram_in("aw", [4, 1024, CW], F32)
    ab = P.dram_in("ab", [4, 4, CW], F32)
    out = P.dram_out("mods", [4, 4, CW], F32)
    cs = P.sb("cs", [128, 8, 4], F32)
    sg = P.sb("sg", [128, 8, 4], F32)
    w = [P.sb("w%d" % i, [128, 8, CW], F32) for i in range(2)]
    bs = P.sb("bs", [4, 4, CW], F32)
    o = P.sb("o", [4, 4, CW], F32)
    pp = [P.ps("pp%d" % i, [4, 2, 512], F32) for i in range(2)]
    P.dma("sp", cs[:], cT.ap(), "cs", writes=["cs"])
    P.dma("sp", bs[:], ab.ap().rearrange("l r c -> r l c"), "bs", writes=["bs"])
    P.op("act", lambda e: e.activation(out=sg[:], in_=cs[:], func=AF.Sigmoid), reads=["cs"], writes=["sg"])
    P.op("dve", lambda e: e.tensor_tensor(out=sg[:], in0=sg[:], in1=cs[:], op=ALU.mult), reads=["cs", "sg"], writes=["sg"])
    for L in range(4):
        wb = w[L % 2]
        P.dma("sp", wb[:], aw.ap()[L].rearrange("(c p) n -> p c n", p=128), "w%d" % (L % 2), writes=["w%d" % (L % 2)])
        for h in range(2):
            n0, n1 = h * 384, (h + 1) * 384
            for c in range(8):
                P.op("pe", lambda e, c=c, h=h, wb=wb, n0=n0, n1=n1, L=L: e.matmul(
                    pp[L % 2][:, h, 0:384], lhsT=sg[:, c, :], rhs=wb[:, c, n0:n1], start=(c == 0), stop=(c == 7)),
                    reads=["sg", "w%d" % (L % 2)], writes=["pp%d" % (L % 2)])
        for h in range(2):
            P.op("dve", lambda e, h=h, L=L: e.tensor_tensor(
                out=o[:, L, h * 384:(h + 1) * 384], in0=pp[L % 2][:, h, 0:384], in1=bs[:, L, h * 384:(h + 1) * 384], op=ALU.add),
                reads=["pp%d" % (L % 2), "bs"], writes=["o"])
    P.dma("sp", out.ap(), o[:], "o", reads=["o"])
    return P


def launch_mods(c, c_ctx, ada_w, ada_b):
    rows = np.zeros((4, D), np.float32)
    rows[0:2] = c
    rows[2] = c_ctx
    cT = np.ascontiguousarray(rows.reshape(4, 8, 128).transpose(2, 1, 0))
    CW = 6144 // NCORE
    maps = []
    for i in range(NCORE):
        sl = slice(i * CW, (i + 1) * CW)
        maps.append({"cT": cT, "aw": np.ascontiguousarray(ada_w[:, :, sl]),
                     "ab": np.ascontiguousarray(np.broadcast_to(ada_b[:, None, sl], (4, 4, CW)))})
    res = run(build_mods(), maps)
    mods = np.concatenate([r["mods"] for r in res], axis=2)
    return mods


def seg_of(tile):
    return 1 if tile < 2 else 0


def col_layout(v):
    v = np.asarray(v, np.float32)
    lead = v.shape[:-1]
    a = v.reshape(lead + (8, 128))
    return np.ascontiguousarray(np.moveaxis(a, -1, 0))


def bc_rows(v, n=128):
    v = np.asarray(v, np.float32)
    return np.ascontiguousarray(np.broadcast_to(v[None], (n,) + v.shape))


class NormT:
    def __init__(self, P, shift_idx, scale_idx):
        self.P = P
        self.ident_d = P.dram_in("ident", [128, 128], F32)
        self.g_d = P.dram_in("gcol", [128, 8], F32)
        self.m_d = P.dram_in("modcol", [128, 2, 6, 8], F32)
        self.ident = P.sb("ident_s", [128, 128], BF16)
        self.g = P.sb("g_s", [128, 8], F32)
        self.m = P.sb("m_s", [128, 2, 6, 8], F32)
        self.sc = P.sb("sc_s", [128, 2, 8], F32)
        self.junk = P.sb("junk", [128, 1024], BF16)
        self.ss = [P.sb("ss%d" % i, [128, 4], F32) for i in range(2)]
        self.xn = [P.sb("xn%d" % i, [128, 1024], BF16) for i in range(2)]
        self.pT = [P.ps("pT%d" % i, [128, 1024], BF16) for i in range(2)]
        self.n = 0
        self.shift_idx = shift_idx
        P.dma("pool", self.ident[:], self.ident_d.ap(), "ident", writes=["ident"])
        P.dma("sp", self.g[:], self.g_d.ap(), "g_s", writes=["g_s"])
        P.dma("sp", self.m[:], self.m_d.ap(), "m_s", writes=["m_s"])
        for s in range(2):
            P.op("dve", lambda e, s=s: e.scalar_tensor_tensor(
                out=self.sc[:, s, :], in0=self.m[:, s, scale_idx, :], scalar=1.0, in1=self.g[:],
                op0=ALU.add, op1=ALU.mult), reads=["g_s", "m_s"], writes=["sc_s"])

    def __call__(self, x_ap, xkey, seg, dst_fn, dkey):
        P = self.P
        b = self.n % 2
        self.n += 1
        ss, xn, pT = self.ss[b], self.xn[b], self.pT[b]
        kss, kxn, kpT = "ss%d" % b, "xn%d" % b, "pT%d" % b
        P.op("act", lambda e: e.activation(out=self.junk[:], in_=x_ap, func=AF.Square, accum_out=ss[:, 0:1]),
             reads=[xkey], writes=["junk", kss])
        P.op("dve", lambda e: e.tensor_scalar(out=ss[:, 1:2], in0=ss[:, 0:1], scalar1=1.0 / D, scalar2=EPS,
                                              op0=ALU.mult, op1=ALU.add), reads=[kss], writes=[kss])
        P.op("act", lambda e: e.activation(out=ss[:, 2:3], in_=ss[:, 1:2], func=AF.Sqrt), reads=[kss], writes=[kss])
        P.op("dve", lambda e: e.reciprocal(out=ss[:, 3:4], in_=ss[:, 2:3]), reads=[kss], writes=[kss])
        P.op("act", lambda e: e.activation(out=xn[:], in_=x_ap, func=AF.Copy, scale=ss[:, 3:4]),
             reads=[xkey, kss], writes=[kxn])
        for c in range(8):
            P.op("pe", lambda e, c=c: e.transpose(out=pT[:, c * 128:(c + 1) * 128], in_=xn[:, c * 128:(c + 1) * 128],
                                                   identity=self.ident[:]), reads=[kxn, "ident"], writes=[kpT])
        for c in range(8):
            sc = self.sc[:, seg, c:c + 1]
            tc = self.m[:, seg, self.shift_idx, c:c + 1]
            if c % 2 == 0:
                P.op("dve", lambda e, c=c, sc=sc, tc=tc: e.tensor_scalar(
                    out=dst_fn(c), in0=pT[:, c * 128:(c + 1) * 128], scalar1=sc, scalar2=tc, op0=ALU.mult, op1=ALU.add),
                    reads=[kpT, "sc_s", "m_s"], writes=[dkey])
            else:
                P.op("act", lambda e, c=c, sc=sc, tc=tc: e.activation(
                    out=dst_fn(c), in_=pT[:, c * 128:(c + 1) * 128], func=AF.Identity, scale=sc, bias=tc),
                    reads=[kpT, "sc_s", "m_s"], writes=[dkey])


def norm_inputs(gvec, mods_b, mods_c):
    m = np.stack([np.asarray(mods_b).reshape(6, D), np.asarray(mods_c).reshape(6, D)], 0)
    return {"ident": np.eye(128, dtype=np.float32), "gcol": col_layout(gvec), "modcol": col_layout(m)}


def load_weight_bf16(P, dst, src_ap, key, nsplit, split_axis_len, mk_dst, mk_src):
    for j in range(nsplit):
        P.dma("pool", mk_dst(j), mk_src(j), "%s_%d" % (key, j), writes=[key])


class Residual:
    def __init__(self, P, name):
        self.P = P
        self.gd = P.dram_in(name, [2, 128, 1024], F32)
        self.g = P.sb(name + "_s", [128, 2, 1024], F32)
        self.key = name
        self.tmp = [P.sb(name + "_t%d" % i, [128, 512], F32) for i in range(2)]
        self.n = 0
        P.dma("sp", self.g[:], self.gd.ap().rearrange("s p d -> p s d"), name, writes=[name])

    def __call__(self, py_ap, pykey, seg, half, x_ap, xkey):
        P = self.P
        b = self.n % 2
        self.n += 1
        t = self.tmp[b]
        tk = self.key + "_t%d" % b
        P.op("dve", lambda e: e.tensor_tensor(out=t[:], in0=py_ap, in1=self.g[:, seg, half * 512:(half + 1) * 512],
                                              op=ALU.mult), reads=[pykey, self.key], writes=[tk])
        P.op("pool", lambda e: e.tensor_tensor(out=x_ap, in0=x_ap, in1=t[:], op=ALU.add), reads=[tk, xkey], writes=[xkey])


MLP_GROUPS = [list(range(0, 4)), list(range(4, 8)), list(range(8, 12)), list(range(12, 16)), [16, 17]]


def build_mlp():
    P = Prog()
    xin = P.dram_in("xin", [NTOK, D], F32)
    w1 = P.dram_in("w1", [D, HID], F32)
    w2 = P.dram_in("w2", [HID, D], F32)
    xout = P.dram_out("xout", [NTOK, D], F32)
    N = NormT(P, 3, 4)
    R = Residual(P, "gate")
    xs = P.sb("xs", [128, 8, 1024], F32)
    hT = [P.sb("hT%d" % i, [128, 8, 512], BF16) for i in range(2)]
    hid = P.sb("hid", [128, 32, 512], BF16)
    w2s = P.sb("w2s", [128, 32, 1024], BF16)
    w1b = [P.sb("w1b%d" % i, [128, 8, 512], BF16) for i in range(2)]
    rl = [P.sb("rl%d" % i, [128, 512], BF16) for i in range(2)]
    ph = [P.ps("ph%d" % i, [128, 512], F32) for i in range(2)]
    py = [P.ps("py%d" % i, [128, 512], F32) for i in range(2)]
    w2v = w2.ap().rearrange("(k p) d -> p k d", p=128)
    for j in range(8):
        P.dma("pool", w2s[:, j * 4:(j + 1) * 4, :], w2v[:, j * 4:(j + 1) * 4, :], "w2s_%d" % j, writes=["w2s_%d" % j])
    w1v = w1.ap().rearrange("(c p) h -> p c h", p=128)
    nw = 0
    nph = 0
    npy = 0
    for gi, tiles in enumerate(MLP_GROUPS):
        n = len(tiles)
        NN = 128 * n
        hb_ = hT[gi % 2]
        hk = "hT%d" % (gi % 2)
        for j, t in enumerate(tiles):
            slot = t % 8
            xk = "xs%d" % slot
            P.dma("sp", xs[:, slot, :], xin.ap()[t * 128:(t + 1) * 128, :], xk, writes=[xk])
            N(xs[:, slot, :], xk, seg_of(t), lambda c, j=j, hb_=hb_: hb_[:, c, j * 128:(j + 1) * 128], hk)
        for hb in range(8):
            wb = w1b[nw % 2]
            wk = "w1b%d" % (nw % 2)
            nw += 1
            P.dma("pool", wb[:], w1v[:, :, hb * 512:(hb + 1) * 512], wk, writes=[wk])
            for hc in range(4):
                p = ph[nph % 2]
                pk = "ph%d" % (nph % 2)
                r = rl[nph % 2]
                rk = "rl%d" % (nph % 2)
                nph += 1
                for c in range(8):
                    P.op("pe", lambda e, c=c, p=p, wb=wb, hc=hc, hb_=hb_, NN=NN: e.matmul(
                        p[:, 0:NN], lhsT=wb[:, c, hc * 128:(hc + 1) * 128], rhs=hb_[:, c, 0:NN],
                        start=(c == 0), stop=(c == 7)), reads=[wk, hk], writes=[pk])
                P.op("act", lambda e, p=p, r=r, NN=NN: e.activation(out=r[:, 0:NN], in_=p[:, 0:NN], func=AF.Relu),
                     reads=[pk], writes=[rk])
                P.op("pool", lambda e, r=r, NN=NN, k=hb * 4 + hc: e.tensor_tensor(
                    out=hid[:, k, 0:NN], in0=r[:, 0:NN], in1=r[:, 0:NN], op=ALU.mult), reads=[rk], writes=["hid"])
        for j, t in enumerate(tiles):
            slot = t % 8
            xk = "xs%d" % slot
            for half in range(2):
                p = py[npy % 2]
                pk = "py%d" % (npy % 2)
                npy += 1
                for k in range(32):
                    P.op("pe", lambda e, k=k, p=p, j=j, half=half: e.matmul(
                        p[:], lhsT=hid[:, k, j * 128:(j + 1) * 128], rhs=w2s[:, k, half * 512:(half + 1) * 512],
                        start=(k == 0), stop=(k == 31)), reads=["hid", "w2s_%d" % (k // 4)], writes=[pk])
                R(p[:], pk, seg_of(t), half, xs[:, slot, half * 512:(half + 1) * 512], xk)
            P.dma("sp", xout.ap()[t * 128:(t + 1) * 128, :], xs[:, slot, :], xk, reads=[xk])
    return P


def launch_mlp(xc, w1, w2, g2, mods, layer):
    maps = []
    for i in range(NCORE):
        b = i // 4
        m = {"xin": xc[i], "w1": w1, "w2": w2}
        m.update(norm_inputs(g2, mods[b, layer], mods[2, layer]))
        m["gate"] = np.stack([bc_rows(mods[b, layer, 5 * D:6 * D]), bc_rows(mods[2, layer, 5 * D:6 * D])], 0)
        maps.append(m)
    res = run(build_mlp(), maps)
    return [r["xout"] for r in res]


HYB_IN = 3088


def build_p1():
    P = Prog()
    xin = P.dram_in("xin", [NTOK, D], F32)
    win = P.dram_in("win", [D, HYB_IN], F32)
    gbb = P.dram_in("gateb", [128, 16], F32)
    qk_o = P.dram_out("qk", [NTOK, 512], BF16)
    v_o = P.dram_out("v", [NTOK, 512], BF16)
    g_o = P.dram_out("gates", [NTOK, 16], F32)
    s_o = P.dram_out("side", [NTOK, 3, 512], F32)
    N = NormT(P, 0, 1)
    ws = P.sb("ws", [128, 8, HYB_IN], BF16)
    gb_s = P.sb("gb_s", [128, 16], F32)
    xs = [P.sb("xs%d" % i, [128, 1024], F32) for i in range(2)]
    hT = [P.sb("hT%d" % i, [128, 8, 128], BF16) for i in range(2)]
    qks = [P.sb("qks%d" % i, [128, 512], BF16) for i in range(2)]
    vs = [P.sb("vs%d" % i, [128, 512], BF16) for i in range(2)]
    gs = [P.sb("gs%d" % i, [128, 16], F32) for i in range(2)]
    ge = [P.sb("ge%d" % i, [128, 16], F32) for i in range(2)]
    sd = [P.sb("sd%d" % i, [128, 3, 512], F32) for i in range(2)]
    gc = [P.sb("gc%d" % i, [128, 512], F32) for i in range(2)]
    pp = [P.ps("pp%d" % i, [128, 512], F32) for i in range(4)]
    wv = win.ap().rearrange("(c p) n -> p c n", p=128)
    blocks = [(0, 512), (512, 1024), (1024, 1536), (1536, 1552), (1552, 2064), (2064, 2576), (2576, 3088)]
    for j, (a, b) in enumerate(blocks):
        P.dma("pool", ws[:, :, a:b], wv[:, :, a:b], "ws_%d" % j, writes=["ws_%d" % j])
    P.dma("sp", gb_s[:], gbb.ap(), "gb_s", writes=["gb_s"])
    npp = 0
    for t in range(NT):
        b = t % 2
        xk, hk = "xs%d" % b, "hT%d" % b
        rows = slice(t * 128, (t + 1) * 128)
        P.dma("sp", xs[b][:], xin.ap()[rows, :], xk, writes=[xk])
        N(xs[b][:], xk, seg_of(t), lambda c, b=b: hT[b][:, c, :], hk)
        for j, (a, bb) in enumerate(blocks):
            w = bb - a
            p = pp[npp % 4]
            pk = "pp%d" % (npp % 4)
            npp += 1
            for c in range(8):
                P.op("pe", lambda e, c=c, p=p, a=a, bb=bb, w=w, b=b: e.matmul(
                    p[:, 0:w], lhsT=hT[b][:, c, :], rhs=ws[:, c, a:bb], start=(c == 0), stop=(c == 7)),
                    reads=[hk, "ws_%d" % j], writes=[pk])
            if j == 0:
                P.op("act", lambda e, p=p, b=b: e.activation(out=qks[b][:, 0:256], in_=p[:, 0:256], func=AF.Copy, scale=0.125),
                     reads=[pk], writes=["qks%d" % b])
                P.op("act", lambda e, p=p, b=b: e.activation(out=qks[b][:, 256:512], in_=p[:, 256:512], func=AF.Copy),
                     reads=[pk], writes=["qks%d" % b])
                P.dma("sp", qk_o.ap()[rows, :], qks[b][:], "qks%d" % b, reads=["qks%d" % b])
            elif j == 1:
                P.op("act", lambda e, p=p, b=b: e.activation(out=vs[b][:], in_=p[:], func=AF.Copy),
                     reads=[pk], writes=["vs%d" % b])
                P.dma("sp", v_o.ap()[rows, :], vs[b][:], "vs%d" % b, reads=["vs%d" % b])
            elif j == 2:
                P.op("dve", lambda e, p=p, b=b: e.tensor_copy(out=sd[b][:, 0, :], in_=p[:]), reads=[pk], writes=["sd%d" % b])
            elif j == 3:
                gk = "gs%d" % b
                P.op("dve", lambda e, p=p, b=b: e.tensor_tensor(out=gs[b][:], in0=p[:, 0:16], in1=gb_s[:], op=ALU.add),
                     reads=[pk, "gb_s"], writes=[gk])
                P.op("act", lambda e, b=b: e.activation(out=ge[b][:], in_=gs[b][:], func=AF.Exp, scale=-1.0),
                     reads=[gk], writes=["ge%d" % b])
                P.op("act", lambda e, b=b: e.activation(out=ge[b][:], in_=ge[b][:], func=AF.Ln, bias=1.0),
                     reads=["ge%d" % b], writes=["ge%d" % b])
                for c0 in (4, 12):
                    P.op("dve", lambda e, b=b, c0=c0: e.tensor_scalar(out=gs[b][:, c0:c0 + 4], in0=ge[b][:, c0:c0 + 4],
                                                                      scalar1=-1.0, scalar2=None, op0=ALU.mult),
                         reads=["ge%d" % b, gk], writes=[gk])
                P.dma("sp", g_o.ap()[rows, :], gs[b][:], gk, reads=[gk])
            elif j == 4:
                P.op("act", lambda e, p=p, b=b: e.activation(out=sd[b][:, 1, :], in_=p[:], func=AF.Copy), reads=[pk], writes=["sd%d" % b])
            elif j == 5:
                P.op("act", lambda e, p=p, b=b: e.activation(out=gc[b][:], in_=p[:], func=AF.Copy), reads=[pk], writes=["gc%d" % b])
            else:
                P.op("dve", lambda e, p=p, b=b: e.tensor_tensor(out=sd[b][:, 2, :], in0=p[:], in1=gc[b][:], op=ALU.mult),
                     reads=[pk, "gc%d" % b], writes=["sd%d" % b])
                P.dma("sp", s_o.ap()[rows], sd[b][:], "sd%d" % b, reads=["sd%d" % b])
    return P


def launch_p1(xc, w_in, gate_b, g1, mods, layer):
    maps = []
    for i in range(NCORE):
        b = i // 4
        m = {"xin": xc[i], "win": w_in, "gateb": bc_rows(gate_b)}
        m.update(norm_inputs(g1, mods[b, layer], mods[2, layer]))
        maps.append(m)
    return run(build_p1(), maps)


NTL = (CTX + SEQ) // 128


def build_p2():
    P = Prog()
    qT_d = P.dram_in("qT", [64, NTL * 128], BF16)
    kT_d = P.dram_in("kT", [64, NTL * 128], BF16)
    kt_d = P.dram_in("ktm", [128, NTL, 64], BF16)
    v_d = P.dram_in("v", [128, NTL, 128], BF16)
    g_d = P.dram_in("g4", [128, NTL, 4], F32)
    tri_d = P.dram_in("tri", [2, 128, 128], F32)
    neg_d = P.dram_in("neg", [2, 128, 128], F32)
    idn_d = P.dram_in("ident", [128, 128], F32)
    h_o = P.dram_out("h", [128, NTL, 128], F32)
    qT = P.sb("qT_s", [64, NTL * 128], BF16)
    kT = P.sb("kT_s", [64, NTL * 128], BF16)
    ktm = P.sb("ktm_s", [128, NTL, 64], BF16)
    vp = P.sb("vp_s", [128, NTL, 129], BF16)
    g4 = P.sb("g4_s", [128, NTL, 4], F32)
    tri = P.sb("tri_s", [128, 2, 128], F32)
    neg = P.sb("neg_s", [128, 2, 128], BF16)
    idn = P.sb("idn_s", [128, 128], BF16)
    ones = P.sb("ones_s", [128, 128], F32)
    hacc = P.sb("hacc", [128, NTL, 128], F32)
    St = P.sb("St", [64, 129], F32)
    Stb = P.sb("Stb", [64, 129], BF16)
    NB = 2
    sb4 = [P.sb("sb4_%d" % i, [128, 4], F32) for i in range(NB)]
    cb = [P.sb("cb_%d" % i, [128, 8], F32) for i in range(NB)]
    LFb = [P.sb("LFb_%d" % i, [128, 128], F32) for i in range(NB)]
    Dm = [P.sb("Dm_%d" % i, [128, 128], F32) for i in range(NB)]
    Pm = [P.sb("Pm_%d" % i, [128, 128], BF16) for i in range(NB)]
    Kw = [P.sb("Kw_%d" % i, [128, 64], BF16) for i in range(NB)]
    tB = [P.sb("tB_%d" % i, [128, 129], F32) for i in range(NB)]
    num = [P.sb("num_%d" % i, [128, 129], F32) for i in range(NB)]
    pb = P.ps("pb", [128, 4], F32)
    pd = P.ps("pd", [128, 128], F32)
    pS = P.ps("pS", [128, 128], F32)
    pA = P.ps("pA", [128, 129], F32)
    pB = P.ps("pB", [128, 129], F32)
    pU = P.ps("pU", [64, 129], F32)
    P.dma("sp", qT[:], qT_d.ap(), "qT", writes=["qT"])
    P.dma("sp", kT[:], kT_d.ap(), "kT", writes=["kT"])
    P.dma("sp", ktm[:], kt_d.ap(), "ktm", writes=["ktm"])
    P.dma("sp", vp[:, :, 0:128], v_d.ap(), "vp", writes=["vp"])
    P.dma("sp", g4[:], g_d.ap(), "g4", writes=["g4"])
    P.dma("sp", tri[:], tri_d.ap().rearrange("a p n -> p a n"), "tri", writes=["tri"])
    P.dma("pool", neg[:], neg_d.ap().rearrange("a p n -> p a n"), "neg", writes=["neg"])
    P.dma("pool", idn[:], idn_d.ap(), "idn", writes=["idn"])
    P.op("dve", lambda e: e.memset(ones[:], 1.0), writes=["ones"])
    P.op("pool", lambda e: e.memset(vp[:, :, 128:129], 1.0), writes=["vp1"])
    u = 0
    for d in range(2):
        order = list(range(NTL)) if d == 0 else [1, 0] + list(range(NTL - 1, 1, -1))
        P.op("dve", lambda e: e.memset(St[:], 0.0), writes=["St"])
        P.op("dve", lambda e: e.memset(Stb[:], 0.0), writes=["Stb"])
        for t in order:
            b = u % NB
            u += 1
            cols = slice(t * 128, (t + 1) * 128)
            ig = g4[:, t, 2 * d:2 * d + 1]
            lf = g4[:, t, 2 * d + 1:2 * d + 2]
            k4, kc = "sb4_%d" % b, "cb_%d" % b
            P.op("pe", lambda e, t=t, d=d: e.matmul(pb[:, 0:2], lhsT=tri[:, d, :], rhs=g4[:, t, 2 * d:2 * d + 2], start=True, stop=True),
                 reads=["tri", "g4"], writes=["pb"])
            P.op("pe", lambda e, t=t, d=d: e.matmul(pb[:, 2:4], lhsT=ones[:], rhs=g4[:, t, 2 * d:2 * d + 2], start=True, stop=True),
                 reads=["ones", "g4"], writes=["pb"])
            P.op("dve", lambda e, b=b: e.tensor_copy(out=sb4[b][:], in_=pb[:]), reads=["pb"], writes=[k4])
            P.op("dve", lambda e, b=b, ig=ig: e.tensor_tensor(out=cb[b][:, 0:1], in0=ig, in1=sb4[b][:, 1:2], op=ALU.subtract),
                 reads=[k4, "g4"], writes=[kc])
            P.op("act", lambda e, b=b: e.activation(out=cb[b][:, 1:2], in_=sb4[b][:, 1:2], func=AF.Exp), reads=[k4, kc], writes=[kc])
            P.op("act", lambda e, b=b: e.activation(out=cb[b][:, 2:3], in_=cb[b][:, 0:1], func=AF.Exp, bias=sb4[b][:, 3:4]),
                 reads=[k4, kc], writes=[kc])
            P.op("act", lambda e, b=b: e.activation(out=cb[b][:, 3:4], in_=sb4[b][:, 3:4], func=AF.Exp), reads=[k4, kc], writes=[kc])
            P.op("dve", lambda e, b=b, lf=lf: e.tensor_scalar(out=LFb[b][:], in0=ones[:], scalar1=lf, scalar2=None, op0=ALU.mult),
                 reads=["ones", "g4"], writes=["LFb_%d" % b])
            P.op("pe", lambda e, d=d: e.matmul(pd[:], lhsT=idn[:], rhs=neg[:, d, :], start=True, stop=False),
                 reads=["idn", "neg"], writes=["pd"])
            P.op("pe", lambda e, b=b, d=d: e.matmul(pd[:], lhsT=LFb[b][:], rhs=tri[:, d, :], start=False, stop=True),
                 reads=["LFb_%d" % b, "tri"], writes=["pd"])
            P.op("act", lambda e, b=b: e.activation(out=Dm[b][:], in_=pd[:], func=AF.Exp, bias=cb[b][:, 0:1]),
                 reads=["pd", kc], writes=["Dm_%d" % b])
            P.op("pe", lambda e, cols=cols: e.matmul(pS[:], lhsT=kT[:, cols], rhs=qT[:, cols], start=True, stop=True),
                 reads=["kT", "qT"], writes=["pS"])
            P.op("dve", lambda e, b=b: e.tensor_tensor(out=Pm[b][:], in0=pS[:], in1=Dm[b][:], op=ALU.mult),
                 reads=["pS", "Dm_%d" % b], writes=["Pm_%d" % b])
            P.op("dve", lambda e, b=b, t=t: e.tensor_scalar(out=Kw[b][:], in0=ktm[:, t, :], scalar1=cb[b][:, 2:3], scalar2=None, op0=ALU.mult),
                 reads=["ktm", kc], writes=["Kw_%d" % b])
            P.op("pe", lambda e, b=b, t=t: e.matmul(pA[:], lhsT=Pm[b][:], rhs=vp[:, t, :], start=True, stop=True),
                 reads=["Pm_%d" % b, "vp", "vp1"], writes=["pA"])
            P.op("pe", lambda e, cols=cols: e.matmul(pB[:], lhsT=qT[:, cols], rhs=Stb[:], start=True, stop=True),
                 reads=["qT", "Stb"], writes=["pB"])
            P.op("act", lambda e, b=b: e.activation(out=tB[b][:], in_=pB[:], func=AF.Copy, scale=cb[b][:, 1:2]),
                 reads=["pB", kc], writes=["tB_%d" % b])
            P.op("dve", lambda e, b=b: e.tensor_tensor(out=num[b][:], in0=pA[:], in1=tB[b][:], op=ALU.add),
                 reads=["pA", "tB_%d" % b], writes=["num_%d" % b])
            P.op("act", lambda e, b=b: e.activation(out=cb[b][:, 4:5], in_=num[b][:, 128:129], func=AF.Abs),
                 reads=["num_%d" % b, kc], writes=[kc])
            P.op("dve", lambda e, b=b: e.tensor_scalar(out=cb[b][:, 4:5], in0=cb[b][:, 4:5], scalar1=1.0, scalar2=None,
                                                       op0=ALU.max), reads=[kc], writes=[kc])
            P.op("dve", lambda e, b=b: e.reciprocal(out=cb[b][:, 5:6], in_=cb[b][:, 4:5]), reads=[kc], writes=[kc])
            hk = "hacc%d" % t
            if d == 0:
                P.op("dve", lambda e, b=b, t=t: e.tensor_scalar(out=hacc[:, t, :], in0=num[b][:, 0:128], scalar1=cb[b][:, 5:6],
                                                                scalar2=None, op0=ALU.mult), reads=["num_%d" % b, kc], writes=[hk])
            else:
                P.op("dve", lambda e, b=b, t=t: e.scalar_tensor_tensor(out=hacc[:, t, :], in0=num[b][:, 0:128], scalar=cb[b][:, 5:6],
                                                                       in1=hacc[:, t, :], op0=ALU.mult, op1=ALU.add),
                     reads=["num_%d" % b, kc, hk], writes=[hk])
                P.dma("sp", h_o.ap()[:, t, :], hacc[:, t, :], "hout%d" % (t % 4), reads=[hk])
            P.op("pe", lambda e, b=b, t=t: e.matmul(pU[:], lhsT=Kw[b][:], rhs=vp[:, t, :], start=True, stop=True),
                 reads=["Kw_%d" % b, "vp", "vp1"], writes=["pU"])
            P.op("dve", lambda e, b=b: e.scalar_tensor_tensor(out=St[:], in0=St[:], scalar=cb[b][0:64, 3:4], in1=pU[:],
                                                              op0=ALU.mult, op1=ALU.add), reads=["pU", kc, "St"], writes=["St"])
            P.op("act", lambda e: e.activation(out=Stb[:], in_=St[:], func=AF.Copy), reads=["St"], writes=["Stb"])
    return P


def p2_consts():
    t = np.arange(128)
    tf = (t[:, None] <= t[None, :]).astype(np.float32)
    tb = (t[:, None] >= t[None, :]).astype(np.float32)
    negf = np.where(t[:, None] <= t[None, :], 0.0, -30000.0).astype(np.float32)
    negb = np.where(t[:, None] >= t[None, :], 0.0, -30000.0).astype(np.float32)
    return {"tri": np.stack([tf, tb]), "neg": np.stack([negf, negb]), "ident": np.eye(128, dtype=np.float32)}


def launch_p2(p1res):
    def full(name, b):
        parts = [p1res[4 * b][name][0:CTX]] + [p1res[4 * b + j][name][CTX:] for j in range(4)]
        return np.concatenate(parts, 0)
    consts = p2_consts()
    maps = []
    for i in range(NCORE):
        b, h = i // 4, i % 4
        qk = full("qk", b)
        v = full("v", b)
        g = full("gates", b)
        q = qk[:, h * 64:(h + 1) * 64]
        k = qk[:, 256 + h * 64:256 + (h + 1) * 64]
        m = {"qT": np.ascontiguousarray(q.T), "kT": np.ascontiguousarray(k.T),
             "ktm": np.ascontiguousarray(k.reshape(NTL, 128, 64).transpose(1, 0, 2)),
             "v": np.ascontiguousarray(v[:, h * 128:(h + 1) * 128].reshape(NTL, 128, 128).transpose(1, 0, 2)),
             "g4": np.ascontiguousarray(g[:, [h, 4 + h, 8 + h, 12 + h]].reshape(NTL, 128, 4).transpose(1, 0, 2))}
        m.update(consts)
        maps.append(m)
    res = run(build_p2(), maps)
    H = []
    for b in range(2):
        H.append(np.concatenate([res[4 * b + h]["h"].transpose(1, 0, 2).reshape(NTL * 128, 128) for h in range(4)], 1))
    return H


def build_p3():
    P = Prog()
    xin = P.dram_in("xin", [NTOK, D], F32)
    h_d = P.dram_in("h", [NTOK, 512], F32)
    side = P.dram_in("side", [NTOK, 3, 512], F32)
    gm1 = P.dram_in("gm1", [NTOK, 512], F32)
    gp1 = P.dram_in("gp1", [NTOK, 512], F32)
    mng = P.dram_in("mng", [128, 512], F32)
    cw = P.dram_in("cw", [128, 3, 512], F32)
    wo = P.dram_in("wo", [D, D], F32)
    idn_d = P.dram_in("ident", [128, 128], F32)
    xout = P.dram_out("xout", [NTOK, D], F32)
    R = Residual(P, "gate")
    idn = P.sb("idn", [128, 128], BF16)
    mng_s = P.sb("mng_s", [128, 512], F32)
    cw_s = P.sb("cw_s", [128, 3, 512], F32)
    wos = P.sb("wos", [128, 8, D], BF16)
    xs = [P.sb("xs%d" % i, [128, 1024], F32) for i in range(2)]
    hs = [P.sb("hs%d" % i, [128, 512], F32) for i in range(2)]
    sd = [P.sb("sd%d" % i, [128, 3, 512], F32) for i in range(2)]
    g1 = [P.sb("g1_%d" % i, [128, 2, 512], F32) for i in range(2)]
    hsq = P.sb("hsq", [128, 512], F32)
    st = [P.sb("st%d" % i, [128, 16], F32) for i in range(2)]
    og = P.sb("og", [128, 512], F32)
    t1 = P.sb("t1", [128, 512], F32)
    t2 = P.sb("t2", [128, 512], F32)
    mc = [P.sb("mc%d" % i, [128, 1024], BF16) for i in range(2)]
    mcT = [P.sb("mcT%d" % i, [128, 8, 128], BF16) for i in range(2)]
    pT = [P.ps("pT%d" % i, [128, 1024], BF16) for i in range(2)]
    py = [P.ps("py%d" % i, [128, 512], F32) for i in range(2)]
    P.dma("pool", idn[:], idn_d.ap(), "idn", writes=["idn"])
    P.dma("sp", mng_s[:], mng.ap(), "mng", writes=["mng"])
    P.dma("sp", cw_s[:], cw.ap(), "cw", writes=["cw"])
    wv = wo.ap().rearrange("(c p) n -> p c n", p=128)
    for j in range(2):
        P.dma("pool", wos[:, :, j * 512:(j + 1) * 512], wv[:, :, j * 512:(j + 1) * 512], "wos%d" % j, writes=["wos%d" % j])
    npy = 0
    for t in range(NT):
        b = t % 2
        rows = slice(t * 128, (t + 1) * 128)
        xk, hk, sk, gk, stk, mk, mtk, ptk = ("xs%d" % b, "hs%d" % b, "sd%d" % b, "g1_%d" % b, "st%d" % b, "mc%d" % b, "mcT%d" % b, "pT%d" % b)
        P.dma("sp", xs[b][:], xin.ap()[rows, :], xk, writes=[xk])
        P.dma("sp", hs[b][:], h_d.ap()[rows, :], hk, writes=[hk])
        P.dma("sp", sd[b][:], side.ap()[rows], sk, writes=[sk])
        P.dma("sp", g1[b][:, 0, :], gm1.ap()[rows, :], gk + "a", writes=[gk])
        P.dma("sp", g1[b][:, 1, :], gp1.ap()[rows, :], gk + "b", writes=[gk])
        P.op("pool", lambda e, b=b: e.tensor_tensor(out=hsq[:], in0=hs[b][:], in1=hs[b][:], op=ALU.mult), reads=[hk], writes=["hsq"])
        P.op("dve", lambda e, b=b: e.tensor_reduce(out=st[b][:, 0:4], in_=hsq[:].rearrange("p (h d) -> p h d", h=4), axis=AX.X, op=ALU.add),
             reads=["hsq"], writes=[stk])
        P.op("dve", lambda e, b=b: e.tensor_scalar(out=st[b][:, 4:8], in0=st[b][:, 0:4], scalar1=1.0 / 128, scalar2=EPS, op0=ALU.mult, op1=ALU.add),
             reads=[stk], writes=[stk])
        P.op("act", lambda e, b=b: e.activation(out=st[b][:, 8:12], in_=st[b][:, 4:8], func=AF.Sqrt), reads=[stk], writes=[stk])
        P.op("dve", lambda e, b=b: e.reciprocal(out=st[b][:, 12:16], in_=st[b][:, 8:12]), reads=[stk], writes=[stk])
        P.op("dve", lambda e, b=b: e.tensor_tensor(
            out=hsq[:].rearrange("p (h d) -> p h d", h=4), in0=hs[b][:].rearrange("p (h d) -> p h d", h=4),
            in1=st[b][:, 12:16].unsqueeze(2).to_broadcast([128, 4, 128]), op=ALU.mult), reads=[hk, stk, "hsq"], writes=["hsq"])
        P.op("pool", lambda e: e.tensor_tensor(out=hsq[:], in0=hsq[:], in1=mng_s[:], op=ALU.mult), reads=["hsq", "mng"], writes=["hsq"])
        P.op("act", lambda e, b=b: e.activation(out=og[:], in_=sd[b][:, 0, :], func=AF.Sigmoid), reads=[sk], writes=["og"])
        P.op("dve", lambda e, b=b: e.tensor_tensor(out=mc[b][:, 0:512], in0=hsq[:], in1=og[:], op=ALU.mult), reads=["hsq", "og"], writes=[mk])
        P.op("pool", lambda e, b=b: e.tensor_tensor(out=t1[:], in0=g1[b][:, 0, :], in1=cw_s[:, 0, :], op=ALU.mult), reads=[gk, "cw"], writes=["t1"])
        P.op("pool", lambda e, b=b: e.tensor_tensor(out=t2[:], in0=sd[b][:, 2, :], in1=cw_s[:, 1, :], op=ALU.mult), reads=[sk, "cw"], writes=["t2"])
        P.op("pool", lambda e: e.tensor_tensor(out=t1[:], in0=t1[:], in1=t2[:], op=ALU.add), reads=["t1", "t2"], writes=["t1"])
        P.op("pool", lambda e, b=b: e.tensor_tensor(out=t2[:], in0=g1[b][:, 1, :], in1=cw_s[:, 2, :], op=ALU.mult), reads=[gk, "cw", "t1"], writes=["t2"])
        P.op("pool", lambda e: e.tensor_tensor(out=t1[:], in0=t1[:], in1=t2[:], op=ALU.add), reads=["t1", "t2"], writes=["t1"])
        P.op("pool", lambda e, b=b: e.tensor_tensor(out=mc[b][:, 512:1024], in0=t1[:], in1=sd[b][:, 1, :], op=ALU.mult), reads=["t1", sk], writes=[mk])
        for c in range(8):
            P.op("pe", lambda e, c=c, b=b: e.transpose(out=pT[b][:, c * 128:(c + 1) * 128], in_=mc[b][:, c * 128:(c + 1) * 128], identity=idn[:]),
                 reads=[mk, "idn"], writes=[ptk])
        P.op("dve", lambda e, b=b: e.tensor_copy(out=mcT[b][:, 0:4, :], in_=pT[b][:, 0:512].rearrange("p (c n) -> p c n", c=4)), reads=[ptk], writes=[mtk])
        P.op("act", lambda e, b=b: e.activation(out=mcT[b][:, 4:8, :], in_=pT[b][:, 512:1024].rearrange("p (c n) -> p c n", c=4), func=AF.Copy),
             reads=[ptk], writes=[mtk])
        for half in range(2):
            p = py[npy % 2]
            pk = "py%d" % (npy % 2)
            npy += 1
            for c in range(8):
                P.op("pe", lambda e, c=c, p=p, b=b, half=half: e.matmul(p[:], lhsT=mcT[b][:, c, :], rhs=wos[:, c, half * 512:(half + 1) * 512],
                                                                       start=(c == 0), stop=(c == 7)), reads=[mtk, "wos%d" % half], writes=[pk])
            R(p[:], pk, seg_of(t), half, xs[b][:, half * 512:(half + 1) * 512], xk)
        P.dma("sp", xout.ap()[rows, :], xs[b][:], xk, reads=[xk])
    return P


def launch_p3(xc, H, p1res, mng, conv_w, w_out, mods, layer):
    maps = []
    for i in range(NCORE):
        b, j = i // 4, i % 4
        gcu_c = p1res[4 * b]["side"][0:CTX, 2]
        gcu_l = np.concatenate([p1res[4 * b + jj]["side"][CTX:, 2] for jj in range(4)], 0)
        z = np.zeros((1, 512), np.float32)
        cm1 = np.concatenate([z, gcu_c[:-1]], 0)
        cp1 = np.concatenate([gcu_c[1:], z], 0)
        lm1 = np.concatenate([z, gcu_l[:-1]], 0)[j * LAT:(j + 1) * LAT]
        lp1 = np.concatenate([gcu_l[1:], z], 0)[j * LAT:(j + 1) * LAT]
        hh = np.concatenate([H[b][0:CTX], H[b][CTX + j * LAT:CTX + (j + 1) * LAT]], 0)
        m = {"xin": xc[i], "h": np.ascontiguousarray(hh), "side": p1res[i]["side"],
             "gm1": np.ascontiguousarray(np.concatenate([cm1, lm1], 0)), "gp1": np.ascontiguousarray(np.concatenate([cp1, lp1], 0)),
             "mng": bc_rows(mng), "cw": bc_rows(conv_w), "wo": w_out, "ident": np.eye(128, dtype=np.float32),
             "gate": np.stack([bc_rows(mods[b, layer, 2 * D:3 * D]), bc_rows(mods[2, layer, 2 * D:3 * D])], 0)}
        maps.append(m)
    res = run(build_p3(), maps)
    return [r["xout"] for r in res]


def build_a1(ntiles=NT, stage=9):
    P = Prog()
    xin = P.dram_in("xin", [NTOK, D], F32)
    win = P.dram_in("win", [D, 1536], F32)
    qkg = P.dram_in("qkg", [128, 20, 64], F32)
    cs_d = P.dram_in("ropeC", [LAT, 64], F32)
    sn_d = P.dram_in("ropeS", [LAT, 64], F32)
    qT_o = P.dram_out("qT", [1024, NTOK], BF16)
    kT_o = P.dram_out("kT", [256, NTOK], BF16)
    v_o = P.dram_out("v", [NTOK, 256], BF16)
    N = NormT(P, 0, 1)
    ws = P.sb("ws", [128, 8, 1536], BF16)
    qkg_s = P.sb("qkg_s", [128, 20, 64], F32)
    xs = [P.sb("xs%d" % i, [128, 1024], F32) for i in range(2)]
    hT = [P.sb("hT%d" % i, [128, 8, 128], BF16) for i in range(2)]
    pq = [P.sb("pq%d" % i, [128, 1280], F32) for i in range(2)]
    sq = P.sb("sq", [128, 1280], F32)
    ra = P.sb("ra", [128, 1280], F32)
    rb = P.sb("rb", [128, 1280], F32)
    st = [P.sb("st%d" % i, [128, 80], F32) for i in range(2)]
    cs = [P.sb("cs%d" % i, [128, 2, 64], F32) for i in range(2)]
    vs = [P.sb("vs%d" % i, [128, 256], BF16) for i in range(2)]
    rot = [P.sb("rot%d" % i, [128, 1280], BF16) for i in range(2)]
    qkT = [P.sb("qkT%d" % i, [128, 10, 128], BF16) for i in range(2)]
    pp = [P.ps("pp%d" % i, [128, 512], F32) for i in range(3)]
    pt1 = P.ps("pt1", [128, 1024], BF16)
    pt2 = P.ps("pt2", [128, 256], BF16)
    wv = win.ap().rearrange("(c p) n -> p c n", p=128)
    for j in range(3):
        P.dma("pool", ws[:, :, j * 512:(j + 1) * 512], wv[:, :, j * 512:(j + 1) * 512], "ws_%d" % j, writes=["ws_%d" % j])
    P.dma("sp", qkg_s[:], qkg.ap(), "qkg", writes=["qkg"])
    for t in range(ntiles):
        b = t % 2
        rows = slice(t * 128, (t + 1) * 128)
        xk, hk, pk_, stk, rk, tk = "xs%d" % b, "hT%d" % b, "pq%d" % b, "st%d" % b, "rot%d" % b, "qkT%d" % b
        P.dma("sp", xs[b][:], xin.ap()[rows, :], xk, writes=[xk])
        if t >= 2:
            lr = slice((t - 2) * 128, (t - 1) * 128)
            P.dma("sp", cs[b][:, 0, :], cs_d.ap()[lr, :], "cs%da" % b, writes=["cs%d" % b])
            P.dma("sp", cs[b][:, 1, :], sn_d.ap()[lr, :], "cs%db" % b, writes=["cs%d" % b])
        N(xs[b][:], xk, seg_of(t), lambda c, b=b: hT[b][:, c, :], hk)
        for j in range(3):
            for c in range(8):
                P.op("pe", lambda e, c=c, j=j, b=b: e.matmul(pp[j][:], lhsT=hT[b][:, c, :], rhs=ws[:, c, j * 512:(j + 1) * 512],
                                                           start=(c == 0), stop=(c == 7)), reads=[hk, "ws_%d" % j], writes=["pp%d" % j])
        P.op("act", lambda e, b=b: e.activation(out=pq[b][:, 0:512], in_=pp[0][:], func=AF.Copy), reads=["pp0"], writes=[pk_])
        P.op("dve", lambda e, b=b: e.tensor_copy(out=pq[b][:, 512:1024], in_=pp[1][:]), reads=["pp1"], writes=[pk_])
        P.op("act", lambda e, b=b: e.activation(out=pq[b][:, 1024:1280], in_=pp[2][:, 0:256], func=AF.Copy), reads=["pp2"], writes=[pk_])
        P.op("act", lambda e, b=b: e.activation(out=vs[b][:], in_=pp[2][:, 256:512], func=AF.Copy), reads=["pp2"], writes=["vs%d" % b])
        P.dma("sp", v_o.ap()[rows, :], vs[b][:], "vs%d" % b, reads=["vs%d" % b])
        if stage < 2:
            continue
        P.op("pool", lambda e, b=b: e.tensor_tensor(out=sq[:], in0=pq[b][:], in1=pq[b][:], op=ALU.mult), reads=[pk_], writes=["sq"])
        P.op("dve", lambda e, b=b: e.tensor_reduce(out=st[b][:, 0:20], in_=sq[:].rearrange("p (h d) -> p h d", h=20), axis=AX.X, op=ALU.add),
             reads=["sq"], writes=[stk])
        P.op("dve", lambda e, b=b: e.tensor_scalar(out=st[b][:, 20:40], in0=st[b][:, 0:20], scalar1=1.0 / 64, scalar2=EPS, op0=ALU.mult, op1=ALU.add),
             reads=[stk], writes=[stk])
        P.op("act", lambda e, b=b: e.activation(out=st[b][:, 40:60], in_=st[b][:, 20:40], func=AF.Sqrt), reads=[stk], writes=[stk])
        P.op("dve", lambda e, b=b: e.reciprocal(out=st[b][:, 60:80], in_=st[b][:, 40:60]), reads=[stk], writes=[stk])
        P.op("dve", lambda e, b=b: e.tensor_tensor(
            out=sq[:].rearrange("p (h d) -> p h d", h=20), in0=pq[b][:].rearrange("p (h d) -> p h d", h=20),
            in1=st[b][:, 60:80].unsqueeze(2).to_broadcast([128, 20, 64]), op=ALU.mult), reads=[pk_, stk, "sq"], writes=["sq"])
        if stage < 3:
            continue
        if t < 2:
            P.op("pool", lambda e, b=b: e.tensor_tensor(out=rot[b][:].rearrange("p (h d) -> p h d", h=20),
                                                       in0=sq[:].rearrange("p (h d) -> p h d", h=20), in1=qkg_s[:], op=ALU.mult),
                 reads=["sq", "qkg"], writes=[rk])
        else:
            P.op("pool", lambda e: e.tensor_tensor(out=sq[:].rearrange("p (h d) -> p h d", h=20),
                                                  in0=sq[:].rearrange("p (h d) -> p h d", h=20), in1=qkg_s[:], op=ALU.mult),
                 reads=["sq", "qkg"], writes=["sq"])
            P.op("dve", lambda e, b=b: e.tensor_tensor(
                out=ra[:].rearrange("p (h d) -> p h d", h=20), in0=sq[:].rearrange("p (h d) -> p h d", h=20),
                in1=cs[b][:, 0, :].unsqueeze(1).to_broadcast([128, 20, 64]), op=ALU.mult), reads=["sq", "cs%d" % b], writes=["ra"])
            for hf in range(2):
                P.op("dve", lambda e, b=b, hf=hf: e.tensor_tensor(
                    out=rb[:].rearrange("p (h r a d) -> p h r a d", h=20, r=2, a=2)[:, :, :, hf, :],
                    in0=sq[:].rearrange("p (h r a d) -> p h r a d", h=20, r=2, a=2)[:, :, :, 1 - hf, :],
                    in1=cs[b][:, 1, :].rearrange("p (r a d) -> p r a d", r=2, a=2)[:, :, hf, :].unsqueeze(1).to_broadcast([128, 20, 2, 16]),
                    op=ALU.mult), reads=["sq", "cs%d" % b], writes=["rb"])
            P.op("dve", lambda e, b=b: e.tensor_tensor(out=rot[b][:], in0=ra[:], in1=rb[:], op=ALU.add), reads=["ra", "rb"], writes=[rk])
        if stage < 4:
            continue
        for c in range(10):
            dst = pt1[:, c * 128:(c + 1) * 128] if c < 8 else pt2[:, (c - 8) * 128:(c - 7) * 128]
            P.op("pe", lambda e, c=c, b=b, dst=dst: e.transpose(out=dst, in_=rot[b][:, c * 128:(c + 1) * 128], identity=N.ident[:]),
                 reads=[rk, "ident"], writes=["pt1" if c < 8 else "pt2"])
        P.op("dve", lambda e, b=b: e.tensor_copy(out=qkT[b][:, 0:8, :], in_=pt1[:].rearrange("p (c n) -> p c n", c=8)), reads=["pt1"], writes=[tk])
        P.op("act", lambda e, b=b: e.activation(out=qkT[b][:, 8:10, :], in_=pt2[:].rearrange("p (c n) -> p c n", c=2), func=AF.Copy),
             reads=["pt2"], writes=[tk])
        if stage < 5:
            continue
        P.dma("sp", qT_o.ap()[:, rows].rearrange("(c p) n -> p c n", p=128), qkT[b][:, 0:8, :], tk + "q", reads=[tk])
        P.dma("sp", kT_o.ap()[:, rows].rearrange("(c p) n -> p c n", p=128), qkT[b][:, 8:10, :], tk + "k", reads=[tk])
    return P


def rope_tables():
    half = 32
    inv = 10000.0 ** (-np.arange(0, half, 2, dtype=np.float32) / half)
    t = np.arange(SEQ)
    row = (t // 64).astype(np.float32)
    col = (t % 64).astype(np.float32)
    ang = np.concatenate([row[:, None] * inv, col[:, None] * inv], -1).astype(np.float32)
    c, s = np.cos(ang), np.sin(ang)
    C = np.concatenate([c[:, :16], c[:, :16], c[:, 16:], c[:, 16:]], -1)
    S = np.concatenate([-s[:, :16], s[:, :16], -s[:, 16:], s[:, 16:]], -1)
    return C.astype(np.float32), S.astype(np.float32)


def launch_a1(xc, w_in, qg, kg, g1, mods, layer):
    C, S = rope_tables()
    gains = np.concatenate([np.broadcast_to(qg[None], (16, 64)), np.broadcast_to(kg[None], (4, 64))], 0)
    maps = []
    for i in range(NCORE):
        b, j = i // 4, i % 4
        m = {"xin": xc[i], "win": w_in, "qkg": bc_rows(gains),
             "ropeC": np.ascontiguousarray(C[j * LAT:(j + 1) * LAT]), "ropeS": np.ascontiguousarray(S[j * LAT:(j + 1) * LAT])}
        m.update(norm_inputs(g1, mods[b, layer], mods[2, layer]))
        maps.append(m)
    return run(build_a1(), maps)


def build_a2g():
    P = Prog()
    NK = NTL
    qT_d = P.dram_in("qT", [256, NTOK], BF16)
    kT_d = P.dram_in("kT", [64, NK * 128], BF16)
    v_d = P.dram_in("v", [NK * 128, 64], BF16)
    o_d = P.dram_out("oT", [4, 64, NTOK], BF16)
    qs = P.sb("qs", [64, 4, NTOK], BF16)
    ks = P.sb("ks", [64, NK * 128], BF16)
    vp = P.sb("vp", [128, NK, 65], BF16)
    oT = P.sb("oT_s", [65, 4, NTOK], BF16)
    ones = P.sb("ones", [1, 65], F32)
    rr = P.sb("rr", [1, 512], F32)
    bcs = P.sb("bcs", [65, 512], F32)
    pt = [P.sb("pt%d" % i, [128, 2, 512], BF16) for i in range(3)]
    pS = [P.ps("pS%d" % i, [128, 2, 512], F32) for i in range(2)]
    pO = [P.ps("pO%d" % i, [65, 512], F32) for i in range(4)]
    P.op("dve", lambda e: e.memset(ones[:], 1.0), writes=["ones"])
    P.op("dve", lambda e: e.memset(vp[:, :, 0:1], 1.0), writes=["vp1"])
    P.dma("sp", ks[:], kT_d.ap(), "ks", writes=["ks"])
    P.dma("sp", vp[:, :, 1:65], v_d.ap().rearrange("(t p) d -> p t d", p=128), "vp", writes=["vp"])
    P.dma("sp", qs[:], qT_d.ap().rearrange("(h p) n -> p h n", p=64), "qs", writes=["qs"])
    blocks = [(0, 256, 2)] + [(CTX + 512 * j, 512, NK) for j in range(4)]
    ns = 0
    npt = 0
    LAG = 1
    for (q0, nq, nkt) in blocks:
        steps = [(kt, hp) for kt in range(nkt) for hp in range(2)]
        pend = []
        for si in range(len(steps) + LAG):
            if si < len(steps):
                kt, hp = steps[si]
                sl = ns % 2
                ns += 1
                pl = npt % 3
                npt += 1
                for hl in range(2):
                    P.op("pe", lambda e, kt=kt, h=hp * 2 + hl, hl=hl, sl=sl, q0=q0, nq=nq: e.matmul(
                        pS[sl][:, hl, 0:nq], lhsT=ks[:, kt * 128:(kt + 1) * 128], rhs=qs[:, h, q0:q0 + nq], start=True, stop=True),
                        reads=["ks", "qs"], writes=["pS%d" % sl])
                P.op("act", lambda e, sl=sl, pl=pl, nq=nq: e.activation(out=pt[pl][:, :, 0:nq], in_=pS[sl][:, :, 0:nq], func=AF.Exp, scale=0.125),
                     reads=["pS%d" % sl], writes=["pt%d" % pl])
                pend.append((kt, hp, pl))
            if si >= LAG:
                kt, hp, pl = pend[si - LAG]
                for hl in range(2):
                    h = hp * 2 + hl
                    P.op("pe", lambda e, kt=kt, h=h, hl=hl, pl=pl, nq=nq, nkt=nkt: e.matmul(
                        pO[h][:, 0:nq], lhsT=vp[:, kt, :], rhs=pt[pl][:, hl, 0:nq], start=(kt == 0), stop=(kt == nkt - 1)),
                        reads=["vp", "vp1", "pt%d" % pl], writes=["pO%d" % h])
        for h in range(4):
            sl = ns % 2
            ns += 1
            P.op("dve", lambda e, h=h, nq=nq: e.reciprocal(out=rr[:, 0:nq], in_=pO[h][0:1, 0:nq]), reads=["pO%d" % h], writes=["rr"])
            P.op("pe", lambda e, sl=sl, nq=nq: e.matmul(pS[sl][0:65, 0, 0:nq], lhsT=ones[:], rhs=rr[:, 0:nq], start=True, stop=True),
                 reads=["ones", "rr"], writes=["pS%d" % sl])
            P.op("act", lambda e, sl=sl, nq=nq: e.activation(out=bcs[:, 0:nq], in_=pS[sl][0:65, 0, 0:nq], func=AF.Copy),
                 reads=["pS%d" % sl], writes=["bcs"])
            P.op("dve", lambda e, h=h, q0=q0, nq=nq: e.tensor_tensor(out=oT[:, h, q0:q0 + nq], in0=pO[h][:, 0:nq], in1=bcs[:, 0:nq], op=ALU.mult),
                 reads=["pO%d" % h, "bcs"], writes=["oT"])
    for h in range(4):
        P.dma("sp", o_d.ap()[h], oT[1:65, h, :], "oT%d" % h, reads=["oT"])
    return P


def launch_a2g(a1res):
    outs = [[] for _ in range(NCORE)]
    kTs, vs = [], []
    for b in range(2):
        kTs.append(np.concatenate([a1res[4 * b]["kT"][:, 0:CTX]] + [a1res[4 * b + j]["kT"][:, CTX:] for j in range(4)], 1))
        vs.append(np.concatenate([a1res[4 * b]["v"][0:CTX]] + [a1res[4 * b + j]["v"][CTX:] for j in range(4)], 0))
    for g in range(4):
        maps = []
        for i in range(NCORE):
            b = i // 4
            maps.append({"qT": np.ascontiguousarray(a1res[i]["qT"][g * 256:(g + 1) * 256]),
                         "kT": np.ascontiguousarray(kTs[b][g * 64:(g + 1) * 64]),
                         "v": np.ascontiguousarray(vs[b][:, g * 64:(g + 1) * 64])})
        res = run(build_a2g(), maps)
        for i in range(NCORE):
            outs[i].append(np.asarray(res[i]["oT"]))
    return [np.ascontiguousarray(np.concatenate(o, 0).transpose(1, 0, 2)) for o in outs]


def build_a3():
    P = Prog()
    xin = P.dram_in("xin", [NTOK, D], F32)
    o_d = P.dram_in("oT", [64, 16, NTOK], BF16)
    wo = P.dram_in("wo", [D, D], F32)
    xout = P.dram_out("xout", [NTOK, D], F32)
    R = Residual(P, "gate")
    oT = P.sb("oT_s", [64, 16, NTOK], BF16)
    wos = P.sb("wos", [64, 16, D], BF16)
    xs = [P.sb("xs%d" % i, [128, 1024], F32) for i in range(2)]
    py = [P.ps("py%d" % i, [128, 512], F32) for i in range(2)]
    wv = wo.ap().rearrange("(h p) n -> p h n", p=64)
    for j in range(2):
        P.dma("pool", wos[:, :, j * 512:(j + 1) * 512], wv[:, :, j * 512:(j + 1) * 512], "wos%d" % j, writes=["wos%d" % j])
    for j in range(4):
        P.dma("sp", oT[:, j * 4:(j + 1) * 4, :], o_d.ap()[:, j * 4:(j + 1) * 4, :], "oT%d" % j, writes=["oT%d" % j])
    npy = 0
    for t in range(NT):
        b = t % 2
        rows = slice(t * 128, (t + 1) * 128)
        xk = "xs%d" % b
        P.dma("sp", xs[b][:], xin.ap()[rows, :], xk, writes=[xk])
        for half in range(2):
            p = py[npy % 2]
            pk = "py%d" % (npy % 2)
            npy += 1
            for hh in range(16):
                P.op("pe", lambda e, hh=hh, p=p, half=half, t=t: e.matmul(
                    p[:], lhsT=oT[:, hh, t * 128:(t + 1) * 128], rhs=wos[:, hh, half * 512:(half + 1) * 512],
                    start=(hh == 0), stop=(hh == 15)), reads=["oT%d" % (hh // 4), "wos%d" % half], writes=[pk])
            R(p[:], pk, seg_of(t), half, xs[b][:, half * 512:(half + 1) * 512], xk)
        P.dma("sp", xout.ap()[rows, :], xs[b][:], xk, reads=[xk])
    return P


def launch_a3(xc, oTs, w_out, mods, layer, with_ctx):
    maps = []
    for i in range(NCORE):
        b = i // 4
        maps.append({"xin": xc[i], "oT": oTs[i], "wo": w_out,
                     "gate": np.stack([bc_rows(mods[b, layer, 2 * D:3 * D]), bc_rows(mods[2, layer, 2 * D:3 * D])], 0)})
    res = run(build_a3(), maps)
    out = []
    for i in range(NCORE):
        xo = np.array(res[i]["xout"])
        if not with_ctx:
            xo[0:CTX] = xc[i][0:CTX]
        out.append(xo)
    return out


def kernel(x, c, ctx, c_ctx, ada_w, ada_b, norm1_g, norm2_g, mlp_w1, mlp_w2, hyb_w_in, hyb_gate_b,
           mlstm_norm_g, conv_w, hyb_w_out, att_w_in, q_norm_g, k_norm_g, att_w_out):
    f = lambda a: np.ascontiguousarray(np.asarray(a, np.float32))
    x, c, ctx, c_ctx, ada_w, ada_b = f(x), f(c), f(ctx), f(c_ctx), f(ada_w), f(ada_b)
    mods = launch_mods(c, c_ctx, ada_w, ada_b)
    xc = [np.ascontiguousarray(np.concatenate([ctx[i // 4], x[i // 4, (i % 4) * LAT:(i % 4 + 1) * LAT]], 0)) for i in range(NCORE)]
    for layer in range(4):
        if layer % 2 == 0:
            e = layer // 2
            p1 = launch_p1(xc, f(hyb_w_in[e]), f(hyb_gate_b[e]), f(norm1_g[layer]), mods, layer)
            H = launch_p2(p1)
            xc = launch_p3(xc, H, p1, f(mlstm_norm_g[e]), f(conv_w[e]), f(hyb_w_out[e]), mods, layer)
        else:
            o = layer // 2
            a1 = launch_a1(xc, f(att_w_in[o]), f(q_norm_g[o]), f(k_norm_g[o]), f(norm1_g[layer]), mods, layer)
            oTs = launch_a2g(a1)
            xc = launch_a3(xc, oTs, f(att_w_out[o]), mods, layer, layer != 3)
        xc = launch_mlp(xc, f(mlp_w1[layer]), f(mlp_w2[layer]), f(norm2_g[layer]), mods, layer)
    out = np.zeros((2, SEQ, D), np.float32)
    for i in range(NCORE):
        out[i // 4, (i % 4) * LAT:(i % 4 + 1) * LAT] = xc[i][CTX:]
    return out
```

```python
import contextlib
import numpy as np
import ml_dtypes
import concourse.bass as bass
import concourse.mybir as mybir
from concourse.bass_utils import run_bass_kernel_spmd

F32 = mybir.dt.float32
BF16 = mybir.dt.bfloat16
AF = mybir.ActivationFunctionType
ALU = mybir.AluOpType
AX = mybir.AxisListType
NPBF = ml_dtypes.bfloat16

D = 1024
SEQ = 8192
CTX = 256
NCORE = 8
LAT = 2048
NTOK = CTX + LAT
NT = NTOK // 128
HID = 4096
EPS = 1e-6
EPOCH = 20000


class Prog:
    ENG = ("pe", "act", "dve", "pool", "sp")

    def __init__(self):
        self.nc = bass.Bass("TRN2", target_bir_lowering=False)
        self.q = {e: [] for e in self.ENG}
        self.cnt = {e: 0 for e in self.ENG}
        self.esem = {e: [] for e in self.ENG}
        self.known = {e: {} for e in self.ENG}
        self.sems = []
        self.lastw = {}
        self.readers = {}
        self.dslot = {}
        self.uid = 0

    def new_sem(self, name):
        s = self.nc.alloc_semaphore(name)
        self.sems.append(s)
        return len(self.sems) - 1

    def sb(self, name, shape, dtype):
        return self.nc.alloc_sbuf_tensor(name, list(shape), dtype)

    def ps(self, name, shape, dtype):
        return self.nc.alloc_psum_tensor(name, list(shape), dtype)

    def dram_in(self, name, shape, dtype):
        return self.nc.dram_tensor(name, list(shape), dtype, kind="ExternalInput")

    def dram_out(self, name, shape, dtype):
        return self.nc.dram_tensor(name, list(shape), dtype, kind="ExternalOutput")

    def _needs(self, reads, writes):
        need = {}

        def add(tok):
            if tok is None:
                return
            s, v = tok
            if need.get(s, 0) < v:
                need[s] = v
        for r in reads:
            add(self.lastw.get(r))
        for w in writes:
            add(self.lastw.get(w))
            for s, v in self.readers.get(w, {}).items():
                add((s, v))
        return need

    def _emit_waits(self, eng, need, own_sems=()):
        kn = self.known[eng]
        for s, v in need.items():
            if s in own_sems:
                continue
            if kn.get(s, 0) >= v:
                continue
            kn[s] = v
            self.q[eng].append(("wait", s, v))

    def _record(self, tok, reads, writes):
        for r in reads:
            d = self.readers.setdefault(r, {})
            if d.get(tok[0], 0) < tok[1]:
                d[tok[0]] = tok[1]
        for w in writes:
            self.lastw[w] = tok
            self.readers[w] = {}

    def op(self, eng, fn, reads=(), writes=()):
        need = self._needs(reads, writes)
        own = tuple(self.esem[eng]) if eng == "pe" else ()
        self._emit_waits(eng, need, own)
        idx = self.cnt[eng]
        self.cnt[eng] = idx + 1
        ep = idx // EPOCH
        while len(self.esem[eng]) <= ep:
            self.esem[eng].append(self.new_sem("e_%s_%d" % (eng, len(self.esem[eng]))))
        s = self.esem[eng][ep]
        tok = (s, idx - ep * EPOCH + 1)
        self.q[eng].append(("op", fn, s, 1))
        self._record(tok, reads, writes)
        return tok

    def dma(self, eng, out, in_, slot, reads=(), writes=()):
        if slot not in self.dslot:
            self.dslot[slot] = [self.new_sem("d%d" % len(self.dslot)), 0]
        s, c = self.dslot[slot]
        need = self._needs(reads, writes)
        if c > 0 and need.get(s, 0) < c:
            need[s] = c
        self._emit_waits(eng, need)
        tok = (s, c + 16)
        self.dslot[slot][1] = c + 16
        self.q[eng].append(("op", lambda e, o=out, i=in_: e.dma_start(out=o, in_=i), s, 16))
        self._record(tok, reads, writes)
        return tok

    def finish(self):
        for slot, (s, c) in self.dslot.items():
            if c > 0 and self.known["sp"].get(s, 0) < c:
                self.known["sp"][s] = c
                self.q["sp"].append(("wait", s, c))
        nc = self.nc
        sems = self.sems

        def replay(items, e):
            for it in items:
                if it[0] == "wait":
                    e.wait_ge(sems[it[1]], it[2])
                else:
                    ins = it[1](e)
                    ins.then_inc(sems[it[2]], it[3])

        with nc.Block() as block:
            @block.tensor
            def _(e):
                replay(self.q["pe"], e)

            @block.scalar
            def _(e):
                replay(self.q["act"], e)

            @block.vector
            def _(e):
                replay(self.q["dve"], e)

            @block.gpsimd
            def _(e):
                replay(self.q["pool"], e)

            @block.sync
            def _(e):
                replay(self.q["sp"], e)
        return nc


_PROF = None


def run(prog, in_maps):
    nc = prog.finish()
    if _PROF is not None:
        res = run_bass_kernel_spmd(nc, in_maps, core_ids=list(range(NCORE)), trace=True)
        _PROF.append(getattr(res, "exec_time_ns", None))
    else:
        res = run_bass_kernel_spmd(nc, in_maps, core_ids=list(range(NCORE)))
    return res.results


def build_mods():
    P = Prog()
    CW = 6144 // NCORE
    cT = P.dram_in("cT", [128, 8, 4], F32)
    aw = P.dram_in("aw", [4, 1024, CW], F32)
    ab = P.dram_in("ab", [4, 4, CW], F32)
    out = P.dram_out("mods", [4, 4, CW], F32)
    cs = P.sb("cs", [128, 8, 4], F32)
    sg = P.sb("sg", [128, 8, 4], F32)
    w = [P.sb("w%d" % i, [128, 8, CW], F32) for i in range(2)]
    bs = P.sb("bs", [4, 4, CW], F32)
    o = P.sb("o", [4, 4, CW], F32)
    pp = [P.ps("pp%d" % i, [4, 2, 512], F32) for i in range(2)]
    P.dma("sp", cs[:], cT.ap(), "cs", writes=["cs"])
    P.dma("sp", bs[:], ab.ap().rearrange("l r c -> r l c"), "bs", writes=["bs"])
    P.op("act", lambda e: e.activation(out=sg[:], in_=cs[:], func=AF.Sigmoid), reads=["cs"], writes=["sg"])
    P.op("dve", lambda e: e.tensor_tensor(out=sg[:], in0=sg[:], in1=cs[:], op=ALU.mult), reads=["cs", "sg"], writes=["sg"])
    for L in range(4):
        wb = w[L % 2]
        P.dma("sp", wb[:], aw.ap()[L].rearrange("(c p) n -> p c n", p=128), "w%d" % (L % 2), writes=["w%d" % (L % 2)])
        for h in range(2):
            n0, n1 = h * 384, (h + 1) * 384
            for c in range(8):
                P.op("pe", lambda e, c=c, h=h, wb=wb, n0=n0, n1=n1, L=L: e.matmul(
                    pp[L % 2][:, h, 0:384], lhsT=sg[:, c, :], rhs=wb[:, c, n0:n1], start=(c == 0), stop=(c == 7)),
                    reads=["sg", "w%d" % (L % 2)], writes=["pp%d" % (L % 2)])
        for h in range(2):
            P.op("dve", lambda e, h=h, L=L: e.tensor_tensor(
                out=o[:, L, h * 384:(h + 1) * 384], in0=pp[L % 2][:, h, 0:384], in1=bs[:, L, h * 384:(h + 1) * 384], op=ALU.add),
                reads=["pp%d" % (L % 2), "bs"], writes=["o"])
    P.dma("sp", out.ap(), o[:], "o", reads=["o"])
    return P


def launch_mods(c, c_ctx, ada_w, ada_b):
    rows = np.zeros((4, D), np.float32)
    rows[0:2] = c
    rows[2] = c_ctx
    cT = np.ascontiguousarray(rows.reshape(4, 8, 128).transpose(2, 1, 0))
    CW = 6144 // NCORE
    maps = []
    for i in range(NCORE):
        sl = slice(i * CW, (i + 1) * CW)
        maps.append({"cT": cT, "aw": np.ascontiguousarray(ada_w[:, :, sl]),
                     "ab": np.ascontiguousarray(np.broadcast_to(ada_b[:, None, sl], (4, 4, CW)))})
    res = run(build_mods(), maps)
    mods = np.concatenate([r["mods"] for r in res], axis=2)
    return mods


def seg_of(tile):
    return 1 if tile < 2 else 0


def col_layout(v):
    v = np.asarray(v, np.float32)
    lead = v.shape[:-1]
    a = v.reshape(lead + (8, 128))
    return np.ascontiguousarray(np.moveaxis(a, -1, 0))


def bc_rows(v, n=128):
    v = np.asarray(v, np.float32)
    return np.ascontiguousarray(np.broadcast_to(v[None], (n,) + v.shape))


class NormT:
    def __init__(self, P, shift_idx, scale_idx):
        self.P = P
        self.ident_d = P.dram_in("ident", [128, 128], F32)
        self.g_d = P.dram_in("gcol", [128, 8], F32)
        self.m_d = P.dram_in("modcol", [128, 2, 6, 8], F32)
        self.ident = P.sb("ident_s", [128, 128], BF16)
        self.g = P.sb("g_s", [128, 8], F32)
        self.m = P.sb("m_s", [128, 2, 6, 8], F32)
        self.sc = P.sb("sc_s", [128, 2, 8], F32)
        self.junk = P.sb("junk", [128, 1024], BF16)
        self.ss = [P.sb("ss%d" % i, [128, 4], F32) for i in range(2)]
        self.xn = [P.sb("xn%d" % i, [128, 1024], BF16) for i in range(2)]
        self.pT = [P.ps("pT%d" % i, [128, 1024], BF16) for i in range(2)]
        self.n = 0
        self.shift_idx = shift_idx
        P.dma("pool", self.ident[:], self.ident_d.ap(), "ident", writes=["ident"])
        P.dma("sp", self.g[:], self.g_d.ap(), "g_s", writes=["g_s"])
        P.dma("sp", self.m[:], self.m_d.ap(), "m_s", writes=["m_s"])
        for s in range(2):
            P.op("dve", lambda e, s=s: e.scalar_tensor_tensor(
                out=self.sc[:, s, :], in0=self.m[:, s, scale_idx, :], scalar=1.0, in1=self.g[:],
                op0=ALU.add, op1=ALU.mult), reads=["g_s", "m_s"], writes=["sc_s"])

    def __call__(self, x_ap, xkey, seg, dst_fn, dkey):
        P = self.P
        b = self.n % 2
        self.n += 1
        ss, xn, pT = self.ss[b], self.xn[b], self.pT[b]
        kss, kxn, kpT = "ss%d" % b, "xn%d" % b, "pT%d" % b
        P.op("act", lambda e: e.activation(out=self.junk[:], in_=x_ap, func=AF.Square, accum_out=ss[:, 0:1]),
             reads=[xkey], writes=["junk", kss])
        P.op("dve", lambda e: e.tensor_scalar(out=ss[:, 1:2], in0=ss[:, 0:1], scalar1=1.0 / D, scalar2=EPS,
                                              op0=ALU.mult, op1=ALU.add), reads=[kss], writes=[kss])
        P.op("act", lambda e: e.activation(out=ss[:, 2:3], in_=ss[:, 1:2], func=AF.Sqrt), reads=[kss], writes=[kss])
        P.op("dve", lambda e: e.reciprocal(out=ss[:, 3:4], in_=ss[:, 2:3]), reads=[kss], writes=[kss])
        P.op("act", lambda e: e.activation(out=xn[:], in_=x_ap, func=AF.Copy, scale=ss[:, 3:4]),
             reads=[xkey, kss], writes=[kxn])
        for c in range(8):
            P.op("pe", lambda e, c=c: e.transpose(out=pT[:, c * 128:(c + 1) * 128], in_=xn[:, c * 128:(c + 1) * 128],
                                                   identity=self.ident[:]), reads=[kxn, "ident"], writes=[kpT])
        for c in range(8):
            sc = self.sc[:, seg, c:c + 1]
            tc = self.m[:, seg, self.shift_idx, c:c + 1]
            if c % 2 == 0:
                P.op("dve", lambda e, c=c, sc=sc, tc=tc: e.tensor_scalar(
                    out=dst_fn(c), in0=pT[:, c * 128:(c + 1) * 128], scalar1=sc, scalar2=tc, op0=ALU.mult, op1=ALU.add),
                    reads=[kpT, "sc_s", "m_s"], writes=[dkey])
            else:
                P.op("act", lambda e, c=c, sc=sc, tc=tc: e.activation(
                    out=dst_fn(c), in_=pT[:, c * 128:(c + 1) * 128], func=AF.Identity, scale=sc, bias=tc),
                    reads=[kpT, "sc_s", "m_s"], writes=[dkey])


def norm_inputs(gvec, mods_b, mods_c):
    m = np.stack([np.asarray(mods_b).reshape(6, D), np.asarray(mods_c).reshape(6, D)], 0)
    return {"ident": np.eye(128, dtype=np.float32), "gcol": col_layout(gvec), "modcol": col_layout(m)}


def load_weight_bf16(P, dst, src_ap, key, nsplit, split_axis_len, mk_dst, mk_src):
    for j in range(nsplit):
        P.dma("pool", mk_dst(j), mk_src(j), "%s_%d" % (key, j), writes=[key])


class Residual:
    def __init__(self, P, name):
        self.P = P
        self.gd = P.dram_in(name, [2, 128, 1024], F32)
        self.g = P.sb(name + "_s", [128, 2, 1024], F32)
        self.key = name
        self.tmp = [P.sb(name + "_t%d" % i, [128, 512], F32) for i in range(2)]
        self.n = 0
        P.dma("sp", self.g[:], self.gd.ap().rearrange("s p d -> p s d"), name, writes=[name])

    def __call__(self, py_ap, pykey, seg, half, x_ap, xkey):
        P = self.P
        b = self.n % 2
        self.n += 1
        t = self.tmp[b]
        tk = self.key + "_t%d" % b
        P.op("dve", lambda e: e.tensor_tensor(out=t[:], in0=py_ap, in1=self.g[:, seg, half * 512:(half + 1) * 512],
                                              op=ALU.mult), reads=[pykey, self.key], writes=[tk])
        P.op("pool", lambda e: e.tensor_tensor(out=x_ap, in0=x_ap, in1=t[:], op=ALU.add), reads=[tk, xkey], writes=[xkey])


MLP_GROUPS = [list(range(0, 4)), list(range(4, 8)), list(range(8, 12)), list(range(12, 16)), [16, 17]]


def build_mlp():
    P = Prog()
    xin = P.dram_in("xin", [NTOK, D], F32)
    w1 = P.dram_in("w1", [D, HID], F32)
    w2 = P.dram_in("w2", [HID, D], F32)
    xout = P.dram_out("xout", [NTOK, D], F32)
    N = NormT(P, 3, 4)
    R = Residual(P, "gate")
    xs = P.sb("xs", [128, 8, 1024], F32)
    hT = [P.sb("hT%d" % i, [128, 8, 512], BF16) for i in range(2)]
    hid = P.sb("hid", [128, 32, 512], BF16)
    w2s = P.sb("w2s", [128, 32, 1024], BF16)
    w1b = [P.sb("w1b%d" % i, [128, 8, 512], BF16) for i in range(2)]
    rl = [P.sb("rl%d" % i, [128, 512], BF16) for i in range(2)]
    ph = [P.ps("ph%d" % i, [128, 512], F32) for i in range(2)]
    py = [P.ps("py%d" % i, [128, 512], F32) for i in range(2)]
    w2v = w2.ap().rearrange("(k p) d -> p k d", p=128)
    w1v = w1.ap().rearrange("(c p) h -> p c h", p=128)
    nw = 0
    nph = 0
    npy = 0
    for gi, tiles in enumerate(MLP_GROUPS):
        n = len(tiles)
        NN = 128 * n
        hb_ = hT[gi % 2]
        hk = "hT%d" % (gi % 2)
        for j, t in enumerate(tiles):
            slot = t % 8
            xk = "xs%d" % slot
            P.dma("sp", xs[:, slot, :], xin.ap()[t * 128:(t + 1) * 128, :], xk, writes=[xk])
            N(xs[:, slot, :], xk, seg_of(t), lambda c, j=j, hb_=hb_: hb_[:, c, j * 128:(j + 1) * 128], hk)
        for hb in range(8):
            wb = w1b[nw % 2]
            wk = "w1b%d" % (nw % 2)
            nw += 1
            P.dma("pool", wb[:], w1v[:, :, hb * 512:(hb + 1) * 512], wk, writes=[wk])
            if gi == 0:
                P.dma("pool", w2s[:, hb * 4:(hb + 1) * 4, :], w2v[:, hb * 4:(hb + 1) * 4, :], "w2s_%d" % hb, writes=["w2s_%d" % hb])
            for hc in range(4):
                p = ph[nph % 2]
                pk = "ph%d" % (nph % 2)
                r = rl[nph % 2]
                rk = "rl%d" % (nph % 2)
                nph += 1
                for c in range(8):
                    P.op("pe", lambda e, c=c, p=p, wb=wb, hc=hc, hb_=hb_, NN=NN: e.matmul(
                        p[:, 0:NN], lhsT=wb[:, c, hc * 128:(hc + 1) * 128], rhs=hb_[:, c, 0:NN],
                        start=(c == 0), stop=(c == 7)), reads=[wk, hk], writes=[pk])
                P.op("act", lambda e, p=p, r=r, NN=NN: e.activation(out=r[:, 0:NN], in_=p[:, 0:NN], func=AF.Relu),
                     reads=[pk], writes=[rk])
                P.op("pool", lambda e, r=r, NN=NN, k=hb * 4 + hc: e.tensor_tensor(
                    out=hid[:, k, 0:NN], in0=r[:, 0:NN], in1=r[:, 0:NN], op=ALU.mult), reads=[rk], writes=["hid"])
        for j, t in enumerate(tiles):
            slot = t % 8
            xk = "xs%d" % slot
            for half in range(2):
                p = py[npy % 2]
                pk = "py%d" % (npy % 2)
                npy += 1
                for k in range(32):
                    P.op("pe", lambda e, k=k, p=p, j=j, half=half: e.matmul(
                        p[:], lhsT=hid[:, k, j * 128:(j + 1) * 128], rhs=w2s[:, k, half * 512:(half + 1) * 512],
                        start=(k == 0), stop=(k == 31)), reads=["hid", "w2s_%d" % (k // 4)], writes=[pk])
                R(p[:], pk, seg_of(t), half, xs[:, slot, half * 512:(half + 1) * 512], xk)
            P.dma("sp", xout.ap()[t * 128:(t + 1) * 128, :], xs[:, slot, :], xk, reads=[xk])
    return P


def launch_mlp(xc, w1, w2, g2, mods, layer):
    maps = []
    for i in range(NCORE):
        b = i // 4
        m = {"xin": xc[i], "w1": w1, "w2": w2}
        m.update(norm_inputs(g2, mods[b, layer], mods[2, layer]))
        m["gate"] = np.stack([bc_rows(mods[b, layer, 5 * D:6 * D]), bc_rows(mods[2, layer, 5 * D:6 * D])], 0)
        maps.append(m)
    res = run(build_mlp(), maps)
    return [r["xout"] for r in res]


HYB_IN = 3088


def build_p1():
    P = Prog()
    xin = P.dram_in("xin", [NTOK, D], F32)
    win = P.dram_in("win", [D, HYB_IN], F32)
    gbb = P.dram_in("gateb", [128, 16], F32)
    qk_o = P.dram_out("qk", [NTOK, 512], BF16)
    v_o = P.dram_out("v", [NTOK, 512], BF16)
    g_o = P.dram_out("gates", [NTOK, 16], F32)
    s_o = P.dram_out("side", [NTOK, 3, 512], F32)
    N = NormT(P, 0, 1)
    ws = P.sb("ws", [128, 8, HYB_IN], BF16)
    gb_s = P.sb("gb_s", [128, 16], F32)
    xs = [P.sb("xs%d" % i, [128, 1024], F32) for i in range(2)]
    hT = [P.sb("hT%d" % i, [128, 8, 128], BF16) for i in range(2)]
    qks = [P.sb("qks%d" % i, [128, 512], BF16) for i in range(2)]
    vs = [P.sb("vs%d" % i, [128, 512], BF16) for i in range(2)]
    gs = [P.sb("gs%d" % i, [128, 16], F32) for i in range(2)]
    ge = [P.sb("ge%d" % i, [128, 16], F32) for i in range(2)]
    sd = [P.sb("sd%d" % i, [128, 3, 512], F32) for i in range(2)]
    gc = [P.sb("gc%d" % i, [128, 512], F32) for i in range(2)]
    pp = [P.ps("pp%d" % i, [128, 512], F32) for i in range(4)]
    wv = win.ap().rearrange("(c p) n -> p c n", p=128)
    blocks = [(0, 512), (512, 1024), (1024, 1536), (1536, 1552), (1552, 2064), (2064, 2576), (2576, 3088)]
    for j, (a, b) in enumerate(blocks):
        P.dma("pool", ws[:, :, a:b], wv[:, :, a:b], "ws_%d" % j, writes=["ws_%d" % j])
    P.dma("sp", gb_s[:], gbb.ap(), "gb_s", writes=["gb_s"])
    npp = 0
    for t in range(NT):
        b = t % 2
        xk, hk = "xs%d" % b, "hT%d" % b
        rows = slice(t * 128, (t + 1) * 128)
        P.dma("sp", xs[b][:], xin.ap()[rows, :], xk, writes=[xk])
        N(xs[b][:], xk, seg_of(t), lambda c, b=b: hT[b][:, c, :], hk)
        for j, (a, bb) in enumerate(blocks):
            w = bb - a
            p = pp[npp % 4]
            pk = "pp%d" % (npp % 4)
            npp += 1
            for c in range(8):
                P.op("pe", lambda e, c=c, p=p, a=a, bb=bb, w=w, b=b: e.matmul(
                    p[:, 0:w], lhsT=hT[b][:, c, :], rhs=ws[:, c, a:bb], start=(c == 0), stop=(c == 7)),
                    reads=[hk, "ws_%d" % j], writes=[pk])
            if j == 0:
                P.op("act", lambda e, p=p, b=b: e.activation(out=qks[b][:, 0:256], in_=p[:, 0:256], func=AF.Copy, scale=0.125),
                     reads=[pk], writes=["qks%d" % b])
                P.op("act", lambda e, p=p, b=b: e.activation(out=qks[b][:, 256:512], in_=p[:, 256:512], func=AF.Copy),
                     reads=[pk], writes=["qks%d" % b])
                P.dma("sp", qk_o.ap()[rows, :], qks[b][:], "qks%d" % b, reads=["qks%d" % b])
            elif j == 1:
                P.op("act", lambda e, p=p, b=b: e.activation(out=vs[b][:], in_=p[:], func=AF.Copy),
                     reads=[pk], writes=["vs%d" % b])
                P.dma("sp", v_o.ap()[rows, :], vs[b][:], "vs%d" % b, reads=["vs%d" % b])
            elif j == 2:
                P.op("dve", lambda e, p=p, b=b: e.tensor_copy(out=sd[b][:, 0, :], in_=p[:]), reads=[pk], writes=["sd%d" % b])
            elif j == 3:
                gk = "gs%d" % b
                P.op("dve", lambda e, p=p, b=b: e.tensor_tensor(out=gs[b][:], in0=p[:, 0:16], in1=gb_s[:], op=ALU.add),
                     reads=[pk, "gb_s"], writes=[gk])
                P.op("act", lambda e, b=b: e.activation(out=ge[b][:], in_=gs[b][:], func=AF.Exp, scale=-1.0),
                     reads=[gk], writes=["ge%d" % b])
                P.op("act", lambda e, b=b: e.activation(out=ge[b][:], in_=ge[b][:], func=AF.Ln, bias=1.0),
                     reads=["ge%d" % b], writes=["ge%d" % b])
                for c0 in (4, 12):
                    P.op("dve", lambda e, b=b, c0=c0: e.tensor_scalar(out=gs[b][:, c0:c0 + 4], in0=ge[b][:, c0:c0 + 4],
                                                                      scalar1=-1.0, scalar2=None, op0=ALU.mult),
                         reads=["ge%d" % b, gk], writes=[gk])
                P.dma("sp", g_o.ap()[rows, :], gs[b][:], gk, reads=[gk])
            elif j == 4:
                P.op("act", lambda e, p=p, b=b: e.activation(out=sd[b][:, 1, :], in_=p[:], func=AF.Copy), reads=[pk], writes=["sd%d" % b])
            elif j == 5:
                P.op("act", lambda e, p=p, b=b: e.activation(out=gc[b][:], in_=p[:], func=AF.Copy), reads=[pk], writes=["gc%d" % b])
            else:
                P.op("dve", lambda e, p=p, b=b: e.tensor_tensor(out=sd[b][:, 2, :], in0=p[:], in1=gc[b][:], op=ALU.mult),
                     reads=[pk, "gc%d" % b], writes=["sd%d" % b])
                P.dma("sp", s_o.ap()[rows], sd[b][:], "sd%d" % b, reads=["sd%d" % b])
    return P


def launch_p1(xc, w_in, gate_b, g1, mods, layer):
    maps = []
    for i in range(NCORE):
        b = i // 4
        m = {"xin": xc[i], "win": w_in, "gateb": bc_rows(gate_b)}
        m.update(norm_inputs(g1, mods[b, layer], mods[2, layer]))
        maps.append(m)
    return run(build_p1(), maps)


NTL = (CTX + SEQ) // 128


def build_p2():
    P = Prog()
    qT_d = P.dram_in("qT", [64, NTL * 128], BF16)
    kT_d = P.dram_in("kT", [64, NTL * 128], BF16)
    kt_d = P.dram_in("ktm", [128, NTL, 64], BF16)
    v_d = P.dram_in("v", [128, NTL, 128], BF16)
    g_d = P.dram_in("g4", [128, NTL, 4], F32)
    tri_d = P.dram_in("tri", [2, 128, 128], F32)
    neg_d = P.dram_in("neg", [2, 128, 128], F32)
    idn_d = P.dram_in("ident", [128, 128], F32)
    h_o = P.dram_out("h", [128, NTL, 128], F32)
    qT = P.sb("qT_s", [64, NTL * 128], BF16)
    kT = P.sb("kT_s", [64, NTL * 128], BF16)
    ktm = P.sb("ktm_s", [128, NTL, 64], BF16)
    vp = P.sb("vp_s", [128, NTL, 129], BF16)
    g4 = P.sb("g4_s", [128, NTL, 4], F32)
    tri = P.sb("tri_s", [128, 2, 128], F32)
    neg = P.sb("neg_s", [128, 2, 128], BF16)
    idn = P.sb("idn_s", [128, 128], BF16)
    ones = P.sb("ones_s", [128, 128], F32)
    hacc = P.sb("hacc", [128, NTL, 128], F32)
    St = P.sb("St", [64, 129], F32)
    Stb = P.sb("Stb", [64, 129], BF16)
    NB = 2
    sb4 = [P.sb("sb4_%d" % i, [128, 4], F32) for i in range(NB)]
    cb = [P.sb("cb_%d" % i, [128, 8], F32) for i in range(NB)]
    LFb = [P.sb("LFb_%d" % i, [128, 128], F32) for i in range(NB)]
    Dm = [P.sb("Dm_%d" % i, [128, 128], F32) for i in range(NB)]
    Pm = [P.sb("Pm_%d" % i, [128, 128], BF16) for i in range(NB)]
    Kw = [P.sb("Kw_%d" % i, [128, 64], BF16) for i in range(NB)]
    tB = [P.sb("tB_%d" % i, [128, 129], F32) for i in range(NB)]
    num = [P.sb("num_%d" % i, [128, 129], F32) for i in range(NB)]
    pb = P.ps("pb", [128, 4], F32)
    pd = P.ps("pd", [128, 128], F32)
    pS = P.ps("pS", [128, 128], F32)
    pA = P.ps("pA", [128, 129], F32)
    pB = P.ps("pB", [128, 129], F32)
    pU = P.ps("pU", [64, 129], F32)
    P.dma("sp", qT[:], qT_d.ap(), "qT", writes=["qT"])
    P.dma("sp", kT[:], kT_d.ap(), "kT", writes=["kT"])
    P.dma("sp", ktm[:], kt_d.ap(), "ktm", writes=["ktm"])
    P.dma("sp", vp[:, :, 0:128], v_d.ap(), "vp", writes=["vp"])
    P.dma("sp", g4[:], g_d.ap(), "g4", writes=["g4"])
    P.dma("sp", tri[:], tri_d.ap().rearrange("a p n -> p a n"), "tri", writes=["tri"])
    P.dma("pool", neg[:], neg_d.ap().rearrange("a p n -> p a n"), "neg", writes=["neg"])
    P.dma("pool", idn[:], idn_d.ap(), "idn", writes=["idn"])
    P.op("dve", lambda e: e.memset(ones[:], 1.0), writes=["ones"])
    P.op("pool", lambda e: e.memset(vp[:, :, 128:129], 1.0), writes=["vp1"])
    u = 0
    for d in range(2):
        order = list(range(NTL)) if d == 0 else [1, 0] + list(range(NTL - 1, 1, -1))
        P.op("dve", lambda e: e.memset(St[:], 0.0), writes=["St"])
        P.op("dve", lambda e: e.memset(Stb[:], 0.0), writes=["Stb"])
        for t in order:
            b = u % NB
            u += 1
            cols = slice(t * 128, (t + 1) * 128)
            ig = g4[:, t, 2 * d:2 * d + 1]
            lf = g4[:, t, 2 * d + 1:2 * d + 2]
            k4, kc = "sb4_%d" % b, "cb_%d" % b
            P.op("pe", lambda e, t=t, d=d: e.matmul(pb[:, 0:2], lhsT=tri[:, d, :], rhs=g4[:, t, 2 * d:2 * d + 2], start=True, stop=True),
                 reads=["tri", "g4"], writes=["pb"])
            P.op("pe", lambda e, t=t, d=d: e.matmul(pb[:, 2:4], lhsT=ones[:], rhs=g4[:, t, 2 * d:2 * d + 2], start=True, stop=True),
                 reads=["ones", "g4"], writes=["pb"])
            P.op("dve", lambda e, b=b: e.tensor_copy(out=sb4[b][:], in_=pb[:]), reads=["pb"], writes=[k4])
            P.op("dve", lambda e, b=b, ig=ig: e.tensor_tensor(out=cb[b][:, 0:1], in0=ig, in1=sb4[b][:, 1:2], op=ALU.subtract),
                 reads=[k4, "g4"], writes=[kc])
            P.op("act", lambda e, b=b: e.activation(out=cb[b][:, 1:2], in_=sb4[b][:, 1:2], func=AF.Exp), reads=[k4, kc], writes=[kc])
            P.op("act", lambda e, b=b: e.activation(out=cb[b][:, 2:3], in_=cb[b][:, 0:1], func=AF.Exp, bias=sb4[b][:, 3:4]),
                 reads=[k4, kc], writes=[kc])
            P.op("act", lambda e, b=b: e.activation(out=cb[b][:, 3:4], in_=sb4[b][:, 3:4], func=AF.Exp), reads=[k4, kc], writes=[kc])
            P.op("dve", lambda e, b=b, lf=lf: e.tensor_scalar(out=LFb[b][:], in0=ones[:], scalar1=lf, scalar2=None, op0=ALU.mult),
                 reads=["ones", "g4"], writes=["LFb_%d" % b])
            P.op("pe", lambda e, d=d: e.matmul(pd[:], lhsT=idn[:], rhs=neg[:, d, :], start=True, stop=False),
                 reads=["idn", "neg"], writes=["pd"])
            P.op("pe", lambda e, b=b, d=d: e.matmul(pd[:], lhsT=LFb[b][:], rhs=tri[:, d, :], start=False, stop=True),
                 reads=["LFb_%d" % b, "tri"], writes=["pd"])
            P.op("act", lambda e, b=b: e.activation(out=Dm[b][:], in_=pd[:], func=AF.Exp, bias=cb[b][:, 0:1]),
                 reads=["pd", kc], writes=["Dm_%d" % b])
            P.op("pe", lambda e, cols=cols: e.matmul(pS[:], lhsT=kT[:, cols], rhs=qT[:, cols], start=True, stop=True),
                 reads=["kT", "qT"], writes=["pS"])
            P.op("dve", lambda e, b=b: e.tensor_tensor(out=Pm[b][:], in0=pS[:], in1=Dm[b][:], op=ALU.mult),
                 reads=["pS", "Dm_%d" % b], writes=["Pm_%d" % b])
            P.op("dve", lambda e, b=b, t=t: e.tensor_scalar(out=Kw[b][:], in0=ktm[:, t, :], scalar1=cb[b][:, 2:3], scalar2=None, op0=ALU.mult),
                 reads=["ktm", kc], writes=["Kw_%d" % b])
            P.op("pe", lambda e, b=b, t=t: e.matmul(pA[:], lhsT=Pm[b][:], rhs=vp[:, t, :], start=True, stop=True),
                 reads=["Pm_%d" % b, "vp", "vp1"], writes=["pA"])
            P.op("pe", lambda e, cols=cols: e.matmul(pB[:], lhsT=qT[:, cols], rhs=Stb[:], start=True, stop=True),
                 reads=["qT", "Stb"], writes=["pB"])
            P.op("act", lambda e, b=b: e.activation(out=tB[b][:], in_=pB[:], func=AF.Copy, scale=cb[b][:, 1:2]),
                 reads=["pB", kc], writes=["tB_%d" % b])
            P.op("dve", lambda e, b=b: e.tensor_tensor(out=num[b][:], in0=pA[:], in1=tB[b][:], op=ALU.add),
                 reads=["pA", "tB_%d" % b], writes=["num_%d" % b])
            P.op("act", lambda e, b=b: e.activation(out=cb[b][:, 4:5], in_=num[b][:, 128:129], func=AF.Abs),
                 reads=["num_%d" % b, kc], writes=[kc])
            P.op("dve", lambda e, b=b: e.tensor_scalar(out=cb[b][:, 4:5], in0=cb[b][:, 4:5], scalar1=1.0, scalar2=None,
                                                       op0=ALU.max), reads=[kc], writes=[kc])
            P.op("dve", lambda e, b=b: e.reciprocal(out=cb[b][:, 5:6], in_=cb[b][:, 4:5]), reads=[kc], writes=[kc])
            hk = "hacc%d" % t
            if d == 0:
                P.op("dve", lambda e, b=b, t=t: e.tensor_scalar(out=hacc[:, t, :], in0=num[b][:, 0:128], scalar1=cb[b][:, 5:6],
                                                                scalar2=None, op0=ALU.mult), reads=["num_%d" % b, kc], writes=[hk])
            else:
                P.op("dve", lambda e, b=b, t=t: e.scalar_tensor_tensor(out=hacc[:, t, :], in0=num[b][:, 0:128], scalar=cb[b][:, 5:6],
                                                                       in1=hacc[:, t, :], op0=ALU.mult, op1=ALU.add),
                     reads=["num_%d" % b, kc, hk], writes=[hk])
                P.dma("sp", h_o.ap()[:, t, :], hacc[:, t, :], "hout%d" % (t % 4), reads=[hk])
            P.op("pe", lambda e, b=b, t=t: e.matmul(pU[:], lhsT=Kw[b][:], rhs=vp[:, t, :], start=True, stop=True),
                 reads=["Kw_%d" % b, "vp", "vp1"], writes=["pU"])
            P.op("dve", lambda e, b=b: e.scalar_tensor_tensor(out=St[:], in0=St[:], scalar=cb[b][0:64, 3:4], in1=pU[:],
                                                              op0=ALU.mult, op1=ALU.add), reads=["pU", kc, "St"], writes=["St"])
            P.op("act", lambda e: e.activation(out=Stb[:], in_=St[:], func=AF.Copy), reads=["St"], writes=["Stb"])
    return P


def p2_consts():
    t = np.arange(128)
    tf = (t[:, None] <= t[None, :]).astype(np.float32)
    tb = (t[:, None] >= t[None, :]).astype(np.float32)
    negf = np.where(t[:, None] <= t[None, :], 0.0, -30000.0).astype(np.float32)
    negb = np.where(t[:, None] >= t[None, :], 0.0, -30000.0).astype(np.float32)
    return {"tri": np.stack([tf, tb]), "neg": np.stack([negf, negb]), "ident": np.eye(128, dtype=np.float32)}


def launch_p2(p1res):
    def full(name, b):
        parts = [p1res[4 * b][name][0:CTX]] + [p1res[4 * b + j][name][CTX:] for j in range(4)]
        return np.concatenate(parts, 0)
    consts = p2_consts()
    maps = []
    for i in range(NCORE):
        b, h = i // 4, i % 4
        qk = full("qk", b)
        v = full("v", b)
        g = full("gates", b)
        q = qk[:, h * 64:(h + 1) * 64]
        k = qk[:, 256 + h * 64:256 + (h + 1) * 64]
        m = {"qT": np.ascontiguousarray(q.T), "kT": np.ascontiguousarray(k.T),
             "ktm": np.ascontiguousarray(k.reshape(NTL, 128, 64).transpose(1, 0, 2)),
             "v": np.ascontiguousarray(v[:, h * 128:(h + 1) * 128].reshape(NTL, 128, 128).transpose(1, 0, 2)),
             "g4": np.ascontiguousarray(g[:, [h, 4 + h, 8 + h, 12 + h]].reshape(NTL, 128, 4).transpose(1, 0, 2))}
        m.update(consts)
        maps.append(m)
    res = run(build_p2(), maps)
    H = []
    for b in range(2):
        H.append(np.concatenate([res[4 * b + h]["h"].transpose(1, 0, 2).reshape(NTL * 128, 128) for h in range(4)], 1))
    return H


def build_p3():
    P = Prog()
    xin = P.dram_in("xin", [NTOK, D], F32)
    h_d = P.dram_in("h", [NTOK, 512], F32)
    side = P.dram_in("side", [NTOK, 3, 512], F32)
    gm1 = P.dram_in("gm1", [NTOK, 512], F32)
    gp1 = P.dram_in("gp1", [NTOK, 512], F32)
    mng = P.dram_in("mng", [128, 512], F32)
    cw = P.dram_in("cw", [128, 3, 512], F32)
    wo = P.dram_in("wo", [D, D], F32)
    idn_d = P.dram_in("ident", [128, 128], F32)
    xout = P.dram_out("xout", [NTOK, D], F32)
    R = Residual(P, "gate")
    idn = P.sb("idn", [128, 128], BF16)
    mng_s = P.sb("mng_s", [128, 512], F32)
    cw_s = P.sb("cw_s", [128, 3, 512], F32)
    wos = P.sb("wos", [128, 8, D], BF16)
    xs = [P.sb("xs%d" % i, [128, 1024], F32) for i in range(2)]
    hs = [P.sb("hs%d" % i, [128, 512], F32) for i in range(2)]
    sd = [P.sb("sd%d" % i, [128, 3, 512], F32) for i in range(2)]
    g1 = [P.sb("g1_%d" % i, [128, 2, 512], F32) for i in range(2)]
    hsq = P.sb("hsq", [128, 512], F32)
    st = [P.sb("st%d" % i, [128, 16], F32) for i in range(2)]
    og = P.sb("og", [128, 512], F32)
    t1 = P.sb("t1", [128, 512], F32)
    t2 = P.sb("t2", [128, 512], F32)
    mc = [P.sb("mc%d" % i, [128, 1024], BF16) for i in range(2)]
    mcT = [P.sb("mcT%d" % i, [128, 8, 128], BF16) for i in range(2)]
    pT = [P.ps("pT%d" % i, [128, 1024], BF16) for i in range(2)]
    py = [P.ps("py%d" % i, [128, 512], F32) for i in range(2)]
    P.dma("pool", idn[:], idn_d.ap(), "idn", writes=["idn"])
    P.dma("sp", mng_s[:], mng.ap(), "mng", writes=["mng"])
    P.dma("sp", cw_s[:], cw.ap(), "cw", writes=["cw"])
    wv = wo.ap().rearrange("(c p) n -> p c n", p=128)
    for j in range(2):
        P.dma("pool", wos[:, :, j * 512:(j + 1) * 512], wv[:, :, j * 512:(j + 1) * 512], "wos%d" % j, writes=["wos%d" % j])
    npy = 0
    for t in range(NT):
        b = t % 2
        rows = slice(t * 128, (t + 1) * 128)
        xk, hk, sk, gk, stk, mk, mtk, ptk = ("xs%d" % b, "hs%d" % b, "sd%d" % b, "g1_%d" % b, "st%d" % b, "mc%d" % b, "mcT%d" % b, "pT%d" % b)
        P.dma("sp", xs[b][:], xin.ap()[rows, :], xk, writes=[xk])
        P.dma("sp", hs[b][:], h_d.ap()[rows, :], hk, writes=[hk])
        P.dma("sp", sd[b][:], side.ap()[rows], sk, writes=[sk])
        P.dma("sp", g1[b][:, 0, :], gm1.ap()[rows, :], gk + "a", writes=[gk])
        P.dma("sp", g1[b][:, 1, :], gp1.ap()[rows, :], gk + "b", writes=[gk])
        P.op("pool", lambda e, b=b: e.tensor_tensor(out=hsq[:], in0=hs[b][:], in1=hs[b][:], op=ALU.mult), reads=[hk], writes=["hsq"])
        P.op("dve", lambda e, b=b: e.tensor_reduce(out=st[b][:, 0:4], in_=hsq[:].rearrange("p (h d) -> p h d", h=4), axis=AX.X, op=ALU.add),
             reads=["hsq"], writes=[stk])
        P.op("dve", lambda e, b=b: e.tensor_scalar(out=st[b][:, 4:8], in0=st[b][:, 0:4], scalar1=1.0 / 128, scalar2=EPS, op0=ALU.mult, op1=ALU.add),
             reads=[stk], writes=[stk])
        P.op("act", lambda e, b=b: e.activation(out=st[b][:, 8:12], in_=st[b][:, 4:8], func=AF.Sqrt), reads=[stk], writes=[stk])
        P.op("dve", lambda e, b=b: e.reciprocal(out=st[b][:, 12:16], in_=st[b][:, 8:12]), reads=[stk], writes=[stk])
        P.op("dve", lambda e, b=b: e.tensor_tensor(
            out=hsq[:].rearrange("p (h d) -> p h d", h=4), in0=hs[b][:].rearrange("p (h d) -> p h d", h=4),
            in1=st[b][:, 12:16].unsqueeze(2).to_broadcast([128, 4, 128]), op=ALU.mult), reads=[hk, stk, "hsq"], writes=["hsq"])
        P.op("pool", lambda e: e.tensor_tensor(out=hsq[:], in0=hsq[:], in1=mng_s[:], op=ALU.mult), reads=["hsq", "mng"], writes=["hsq"])
        P.op("act", lambda e, b=b: e.activation(out=og[:], in_=sd[b][:, 0, :], func=AF.Sigmoid), reads=[sk], writes=["og"])
        P.op("dve", lambda e, b=b: e.tensor_tensor(out=mc[b][:, 0:512], in0=hsq[:], in1=og[:], op=ALU.mult), reads=["hsq", "og"], writes=[mk])
        P.op("pool", lambda e, b=b: e.tensor_tensor(out=t1[:], in0=g1[b][:, 0, :], in1=cw_s[:, 0, :], op=ALU.mult), reads=[gk, "cw"], writes=["t1"])
        P.op("pool", lambda e, b=b: e.tensor_tensor(out=t2[:], in0=sd[b][:, 2, :], in1=cw_s[:, 1, :], op=ALU.mult), reads=[sk, "cw"], writes=["t2"])
        P.op("pool", lambda e: e.tensor_tensor(out=t1[:], in0=t1[:], in1=t2[:], op=ALU.add), reads=["t1", "t2"], writes=["t1"])
        P.op("pool", lambda e, b=b: e.tensor_tensor(out=t2[:], in0=g1[b][:, 1, :], in1=cw_s[:, 2, :], op=ALU.mult), reads=[gk, "cw", "t1"], writes=["t2"])
        P.op("pool", lambda e: e.tensor_tensor(out=t1[:], in0=t1[:], in1=t2[:], op=ALU.add), reads=["t1", "t2"], writes=["t1"])
        P.op("pool", lambda e, b=b: e.tensor_tensor(out=mc[b][:, 512:1024], in0=t1[:], in1=sd[b][:, 1, :], op=ALU.mult), reads=["t1", sk], writes=[mk])
        for c in range(8):
            P.op("pe", lambda e, c=c, b=b: e.transpose(out=pT[b][:, c * 128:(c + 1) * 128], in_=mc[b][:, c * 128:(c + 1) * 128], identity=idn[:]),
                 reads=[mk, "idn"], writes=[ptk])
        P.op("dve", lambda e, b=b: e.tensor_copy(out=mcT[b][:, 0:4, :], in_=pT[b][:, 0:512].rearrange("p (c n) -> p c n", c=4)), reads=[ptk], writes=[mtk])
        P.op("act", lambda e, b=b: e.activation(out=mcT[b][:, 4:8, :], in_=pT[b][:, 512:1024].rearrange("p (c n) -> p c n", c=4), func=AF.Copy),
             reads=[ptk], writes=[mtk])
        for half in range(2):
            p = py[npy % 2]
            pk = "py%d" % (npy % 2)
            npy += 1
            for c in range(8):
                P.op("pe", lambda e, c=c, p=p, b=b, half=half: e.matmul(p[:], lhsT=mcT[b][:, c, :], rhs=wos[:, c, half * 512:(half + 1) * 512],
                                                                       start=(c == 0), stop=(c == 7)), reads=[mtk, "wos%d" % half], writes=[pk])
            R(p[:], pk, seg_of(t), half, xs[b][:, half * 512:(half + 1) * 512], xk)
        P.dma("sp", xout.ap()[rows, :], xs[b][:], xk, reads=[xk])
    return P


def launch_p3(xc, H, p1res, mng, conv_w, w_out, mods, layer):
    maps = []
    for i in range(NCORE):
        b, j = i // 4, i % 4
        gcu_c = p1res[4 * b]["side"][0:CTX, 2]
        gcu_l = np.concatenate([p1res[4 * b + jj]["side"][CTX:, 2] for jj in range(4)], 0)
        z = np.zeros((1, 512), np.float32)
        cm1 = np.concatenate([z, gcu_c[:-1]], 0)
        cp1 = np.concatenate([gcu_c[1:], z], 0)
        lm1 = np.concatenate([z, gcu_l[:-1]], 0)[j * LAT:(j + 1) * LAT]
        lp1 = np.concatenate([gcu_l[1:], z], 0)[j * LAT:(j + 1) * LAT]
        hh = np.concatenate([H[b][0:CTX], H[b][CTX + j * LAT:CTX + (j + 1) * LAT]], 0)
        m = {"xin": xc[i], "h": np.ascontiguousarray(hh), "side": p1res[i]["side"],
             "gm1": np.ascontiguousarray(np.concatenate([cm1, lm1], 0)), "gp1": np.ascontiguousarray(np.concatenate([cp1, lp1], 0)),
             "mng": bc_rows(mng), "cw": bc_rows(conv_w), "wo": w_out, "ident": np.eye(128, dtype=np.float32),
             "gate": np.stack([bc_rows(mods[b, layer, 2 * D:3 * D]), bc_rows(mods[2, layer, 2 * D:3 * D])], 0)}
        maps.append(m)
    res = run(build_p3(), maps)
    return [r["xout"] for r in res]


def build_a1(ntiles=NT, stage=9):
    P = Prog()
    xin = P.dram_in("xin", [NTOK, D], F32)
    win = P.dram_in("win", [D, 1536], F32)
    qkg = P.dram_in("qkg", [128, 20, 64], F32)
    cs_d = P.dram_in("ropeC", [LAT, 64], F32)
    sn_d = P.dram_in("ropeS", [LAT, 64], F32)
    qT_o = P.dram_out("qT", [1024, NTOK], BF16)
    kT_o = P.dram_out("kT", [256, NTOK], BF16)
    v_o = P.dram_out("v", [NTOK, 256], BF16)
    N = NormT(P, 0, 1)
    ws = P.sb("ws", [128, 8, 1536], BF16)
    qkg_s = P.sb("qkg_s", [128, 20, 64], F32)
    xs = [P.sb("xs%d" % i, [128, 1024], F32) for i in range(2)]
    hT = [P.sb("hT%d" % i, [128, 8, 128], BF16) for i in range(2)]
    pq = [P.sb("pq%d" % i, [128, 1280], F32) for i in range(2)]
    sq = P.sb("sq", [128, 1280], F32)
    ra = P.sb("ra", [128, 1280], F32)
    rb = P.sb("rb", [128, 1280], F32)
    st = [P.sb("st%d" % i, [128, 80], F32) for i in range(2)]
    cs = [P.sb("cs%d" % i, [128, 2, 64], F32) for i in range(2)]
    vs = [P.sb("vs%d" % i, [128, 256], BF16) for i in range(2)]
    rot = [P.sb("rot%d" % i, [128, 1280], BF16) for i in range(2)]
    qkT = [P.sb("qkT%d" % i, [128, 10, 128], BF16) for i in range(2)]
    pp = [P.ps("pp%d" % i, [128, 512], F32) for i in range(3)]
    pt1 = P.ps("pt1", [128, 1024], BF16)
    pt2 = P.ps("pt2", [128, 256], BF16)
    wv = win.ap().rearrange("(c p) n -> p c n", p=128)
    for j in range(3):
        P.dma("pool", ws[:, :, j * 512:(j + 1) * 512], wv[:, :, j * 512:(j + 1) * 512], "ws_%d" % j, writes=["ws_%d" % j])
    P.dma("sp", qkg_s[:], qkg.ap(), "qkg", writes=["qkg"])
    for t in range(ntiles):
        b = t % 2
        rows = slice(t * 128, (t + 1) * 128)
        xk, hk, pk_, stk, rk, tk = "xs%d" % b, "hT%d" % b, "pq%d" % b, "st%d" % b, "rot%d" % b, "qkT%d" % b
        P.dma("sp", xs[b][:], xin.ap()[rows, :], xk, writes=[xk])
        if t >= 2:
            lr = slice((t - 2) * 128, (t - 1) * 128)
            P.dma("sp", cs[b][:, 0, :], cs_d.ap()[lr, :], "cs%da" % b, writes=["cs%d" % b])
            P.dma("sp", cs[b][:, 1, :], sn_d.ap()[lr, :], "cs%db" % b, writes=["cs%d" % b])
        N(xs[b][:], xk, seg_of(t), lambda c, b=b: hT[b][:, c, :], hk)
        for j in range(3):
            for c in range(8):
                P.op("pe", lambda e, c=c, j=j, b=b: e.matmul(pp[j][:], lhsT=hT[b][:, c, :], rhs=ws[:, c, j * 512:(j + 1) * 512],
                                                           start=(c == 0), stop=(c == 7)), reads=[hk, "ws_%d" % j], writes=["pp%d" % j])
        P.op("act", lambda e, b=b: e.activation(out=pq[b][:, 0:512], in_=pp[0][:], func=AF.Copy), reads=["pp0"], writes=[pk_])
        P.op("dve", lambda e, b=b: e.tensor_copy(out=pq[b][:, 512:1024], in_=pp[1][:]), reads=["pp1"], writes=[pk_])
        P.op("act", lambda e, b=b: e.activation(out=pq[b][:, 1024:1280], in_=pp[2][:, 0:256], func=AF.Copy), reads=["pp2"], writes=[pk_])
        P.op("act", lambda e, b=b: e.activation(out=vs[b][:], in_=pp[2][:, 256:512], func=AF.Copy), reads=["pp2"], writes=["vs%d" % b])
        P.dma("sp", v_o.ap()[rows, :], vs[b][:], "vs%d" % b, reads=["vs%d" % b])
        if stage < 2:
            continue
        P.op("pool", lambda e, b=b: e.tensor_tensor(out=sq[:], in0=pq[b][:], in1=pq[b][:], op=ALU.mult), reads=[pk_], writes=["sq"])
        P.op("dve", lambda e, b=b: e.tensor_reduce(out=st[b][:, 0:20], in_=sq[:].rearrange("p (h d) -> p h d", h=20), axis=AX.X, op=ALU.add),
             reads=["sq"], writes=[stk])
        P.op("dve", lambda e, b=b: e.tensor_scalar(out=st[b][:, 20:40], in0=st[b][:, 0:20], scalar1=1.0 / 64, scalar2=EPS, op0=ALU.mult, op1=ALU.add),
             reads=[stk], writes=[stk])
        P.op("act", lambda e, b=b: e.activation(out=st[b][:, 40:60], in_=st[b][:, 20:40], func=AF.Sqrt), reads=[stk], writes=[stk])
        P.op("dve", lambda e, b=b: e.reciprocal(out=st[b][:, 60:80], in_=st[b][:, 40:60]), reads=[stk], writes=[stk])
        P.op("dve", lambda e, b=b: e.tensor_tensor(
            out=sq[:].rearrange("p (h d) -> p h d", h=20), in0=pq[b][:].rearrange("p (h d) -> p h d", h=20),
            in1=st[b][:, 60:80].unsqueeze(2).to_broadcast([128, 20, 64]), op=ALU.mult), reads=[pk_, stk, "sq"], writes=["sq"])
        if stage < 3:
            continue
        if t < 2:
            P.op("pool", lambda e, b=b: e.tensor_tensor(out=rot[b][:].rearrange("p (h d) -> p h d", h=20),
                                                       in0=sq[:].rearrange("p (h d) -> p h d", h=20), in1=qkg_s[:], op=ALU.mult),
                 reads=["sq", "qkg"], writes=[rk])
        else:
            P.op("pool", lambda e: e.tensor_tensor(out=sq[:].rearrange("p (h d) -> p h d", h=20),
                                                  in0=sq[:].rearrange("p (h d) -> p h d", h=20), in1=qkg_s[:], op=ALU.mult),
                 reads=["sq", "qkg"], writes=["sq"])
            P.op("dve", lambda e, b=b: e.tensor_tensor(
                out=ra[:].rearrange("p (h d) -> p h d", h=20), in0=sq[:].rearrange("p (h d) -> p h d", h=20),
                in1=cs[b][:, 0, :].unsqueeze(1).to_broadcast([128, 20, 64]), op=ALU.mult), reads=["sq", "cs%d" % b], writes=["ra"])
            for hf in range(2):
                P.op("dve", lambda e, b=b, hf=hf: e.tensor_tensor(
                    out=rb[:].rearrange("p (h r a d) -> p h r a d", h=20, r=2, a=2)[:, :, :, hf, :],
                    in0=sq[:].rearrange("p (h r a d) -> p h r a d", h=20, r=2, a=2)[:, :, :, 1 - hf, :],
                    in1=cs[b][:, 1, :].rearrange("p (r a d) -> p r a d", r=2, a=2)[:, :, hf, :].unsqueeze(1).to_broadcast([128, 20, 2, 16]),
                    op=ALU.mult), reads=["sq", "cs%d" % b], writes=["rb"])
            P.op("dve", lambda e, b=b: e.tensor_tensor(out=rot[b][:], in0=ra[:], in1=rb[:], op=ALU.add), reads=["ra", "rb"], writes=[rk])
        if stage < 4:
            continue
        for c in range(10):
            dst = pt1[:, c * 128:(c + 1) * 128] if c < 8 else pt2[:, (c - 8) * 128:(c - 7) * 128]
            P.op("pe", lambda e, c=c, b=b, dst=dst: e.transpose(out=dst, in_=rot[b][:, c * 128:(c + 1) * 128], identity=N.ident[:]),
                 reads=[rk, "ident"], writes=["pt1" if c < 8 else "pt2"])
        P.op("dve", lambda e, b=b: e.tensor_copy(out=qkT[b][:, 0:8, :], in_=pt1[:].rearrange("p (c n) -> p c n", c=8)), reads=["pt1"], writes=[tk])
        P.op("act", lambda e, b=b: e.activation(out=qkT[b][:, 8:10, :], in_=pt2[:].rearrange("p (c n) -> p c n", c=2), func=AF.Copy),
             reads=["pt2"], writes=[tk])
        if stage < 5:
            continue
        P.dma("sp", qT_o.ap()[:, rows].rearrange("(c p) n -> p c n", p=128), qkT[b][:, 0:8, :], tk + "q", reads=[tk])
        P.dma("sp", kT_o.ap()[:, rows].rearrange("(c p) n -> p c n", p=128), qkT[b][:, 8:10, :], tk + "k", reads=[tk])
    return P


def rope_tables():
    half = 32
    inv = 10000.0 ** (-np.arange(0, half, 2, dtype=np.float32) / half)
    t = np.arange(SEQ)
    row = (t // 64).astype(np.float32)
    col = (t % 64).astype(np.float32)
    ang = np.concatenate([row[:, None] * inv, col[:, None] * inv], -1).astype(np.float32)
    c, s = np.cos(ang), np.sin(ang)
    C = np.concatenate([c[:, :16], c[:, :16], c[:, 16:], c[:, 16:]], -1)
    S = np.concatenate([-s[:, :16], s[:, :16], -s[:, 16:], s[:, 16:]], -1)
    return C.astype(np.float32), S.astype(np.float32)


def launch_a1(xc, w_in, qg, kg, g1, mods, layer):
    C, S = rope_tables()
    gains = np.concatenate([np.broadcast_to(qg[None], (16, 64)), np.broadcast_to(kg[None], (4, 64))], 0)
    maps = []
    for i in range(NCORE):
        b, j = i // 4, i % 4
        m = {"xin": xc[i], "win": w_in, "qkg": bc_rows(gains),
             "ropeC": np.ascontiguousarray(C[j * LAT:(j + 1) * LAT]), "ropeS": np.ascontiguousarray(S[j * LAT:(j + 1) * LAT])}
        m.update(norm_inputs(g1, mods[b, layer], mods[2, layer]))
        maps.append(m)
    return run(build_a1(), maps)


def build_a2g():
    P = Prog()
    NK = NTL
    qT_d = P.dram_in("qT", [256, NTOK], BF16)
    kT_d = P.dram_in("kT", [64, NK * 128], BF16)
    v_d = P.dram_in("v", [NK * 128, 64], BF16)
    o_d = P.dram_out("oT", [4, 64, NTOK], BF16)
    qs = P.sb("qs", [128, 2, NTOK], BF16)
    ks = P.sb("ks", [128, NK * 128], BF16)
    vp = P.sb("vp", [128, NK, 65], BF16)
    oT = P.sb("oT_s", [65, 4, NTOK], BF16)
    ones = P.sb("ones", [1, 65], F32)
    rr = P.sb("rr", [1, 512], F32)
    bcs = P.sb("bcs", [65, 512], F32)
    NPT = 4
    pt = [P.sb("pt%d" % i, [128, 2, 512], BF16) for i in range(NPT)]
    pS = [P.ps("pS%d" % i, [128, 2, 512], F32) for i in range(2)]
    pO = [P.ps("pO%d" % i, [65, 512], F32) for i in range(4)]
    P.op("dve", lambda e: e.memset(ones[:], 1.0), writes=["ones"])
    P.op("dve", lambda e: e.memset(vp[:, :, 0:1], 1.0), writes=["vp1"])
    P.dma("sp", ks[0:64, :], kT_d.ap(), "ks", writes=["ks"])
    P.dma("sp", ks[64:128, :], kT_d.ap(), "ksb", writes=["ksb"])
    P.dma("sp", vp[:, :, 1:65], v_d.ap().rearrange("(t p) d -> p t d", p=128), "vp", writes=["vp"])
    qv = qT_d.ap().rearrange("(hp par p) n -> par p hp n", hp=2, par=2, p=64)
    P.dma("sp", qs[0:64, :, :], qv[0], "qs", writes=["qs"])
    P.dma("sp", qs[64:128, :, :], qv[1], "qsb", writes=["qsb"])
    blocks = [(0, 256, 2)] + [(CTX + 512 * j, 512, NK) for j in range(4)]
    ns = 0
    npt = 0
    LAG = 2
    for (q0, nq, nkt) in blocks:
        steps = [(kt, hp) for kt in range(nkt) for hp in range(2)]
        pend = []
        for si in range(len(steps) + LAG):
            if si < len(steps):
                kt, hp = steps[si]
                sl = ns % 2
                ns += 1
                pl = npt % NPT
                npt += 1
                for hl in range(2):
                    P.op("pe", lambda e, kt=kt, hp=hp, hl=hl, sl=sl, q0=q0, nq=nq: e.matmul(
                        pS[sl][:, hl, 0:nq], lhsT=ks[hl * 64:(hl + 1) * 64, kt * 128:(kt + 1) * 128],
                        rhs=qs[hl * 64:(hl + 1) * 64, hp, q0:q0 + nq], start=True, stop=True),
                        reads=["ks", "ksb", "qs", "qsb"], writes=["pS%d" % sl])
                P.op("act", lambda e, sl=sl, pl=pl, nq=nq: e.activation(out=pt[pl][:, :, 0:nq], in_=pS[sl][:, :, 0:nq], func=AF.Exp, scale=0.125),
                     reads=["pS%d" % sl], writes=["pt%d" % pl])
                pend.append((kt, hp, pl))
            if si >= LAG:
                kt, hp, pl = pend[si - LAG]
                for hl in range(2):
                    h = hp * 2 + hl
                    P.op("pe", lambda e, kt=kt, h=h, hl=hl, pl=pl, nq=nq, nkt=nkt: e.matmul(
                        pO[h][:, 0:nq], lhsT=vp[:, kt, :], rhs=pt[pl][:, hl, 0:nq], start=(kt == 0), stop=(kt == nkt - 1)),
                        reads=["vp", "vp1", "pt%d" % pl], writes=["pO%d" % h])
        for h in range(4):
            sl = ns % 2
            ns += 1
            P.op("dve", lambda e, h=h, nq=nq: e.reciprocal(out=rr[:, 0:nq], in_=pO[h][0:1, 0:nq]), reads=["pO%d" % h], writes=["rr"])
            P.op("pe", lambda e, sl=sl, nq=nq: e.matmul(pS[sl][0:65, 0, 0:nq], lhsT=ones[:], rhs=rr[:, 0:nq], start=True, stop=True),
                 reads=["ones", "rr"], writes=["pS%d" % sl])
            P.op("act", lambda e, sl=sl, nq=nq: e.activation(out=bcs[:, 0:nq], in_=pS[sl][0:65, 0, 0:nq], func=AF.Copy),
                 reads=["pS%d" % sl], writes=["bcs"])
            P.op("dve", lambda e, h=h, q0=q0, nq=nq: e.tensor_tensor(out=oT[:, h, q0:q0 + nq], in0=pO[h][:, 0:nq], in1=bcs[:, 0:nq], op=ALU.mult),
                 reads=["pO%d" % h, "bcs"], writes=["oT"])
    for h in range(4):
        P.dma("sp", o_d.ap()[h], oT[1:65, h, :], "oT%d" % h, reads=["oT"])
    return P


def launch_a2g(a1res):
    outs = [[] for _ in range(NCORE)]
    kTs, vs = [], []
    for b in range(2):
        kTs.append(np.concatenate([a1res[4 * b]["kT"][:, 0:CTX]] + [a1res[4 * b + j]["kT"][:, CTX:] for j in range(4)], 1))
        vs.append(np.concatenate([a1res[4 * b]["v"][0:CTX]] + [a1res[4 * b + j]["v"][CTX:] for j in range(4)], 0))
    for g in range(4):
        maps = []
        for i in range(NCORE):
            b = i // 4
            maps.append({"qT": np.ascontiguousarray(a1res[i]["qT"][g * 256:(g + 1) * 256]),
                         "kT": np.ascontiguousarray(kTs[b][g * 64:(g + 1) * 64]),
                         "v": np.ascontiguousarray(vs[b][:, g * 64:(g + 1) * 64])})
        res = run(build_a2g(), maps)
        for i in range(NCORE):
            outs[i].append(np.asarray(res[i]["oT"]))
    return [np.ascontiguousarray(np.concatenate(o, 0).transpose(1, 0, 2)) for o in outs]


def build_a3():
    P = Prog()
    xin = P.dram_in("xin", [NTOK, D], F32)
    o_d = P.dram_in("oT", [64, 16, NTOK], BF16)
    wo = P.dram_in("wo", [D, D], F32)
    xout = P.dram_out("xout", [NTOK, D], F32)
    R = Residual(P, "gate")
    oT = P.sb("oT_s", [64, 16, NTOK], BF16)
    wos = P.sb("wos", [64, 16, D], BF16)
    xs = [P.sb("xs%d" % i, [128, 1024], F32) for i in range(2)]
    py = [P.ps("py%d" % i, [128, 512], F32) for i in range(2)]
    wv = wo.ap().rearrange("(h p) n -> p h n", p=64)
    for j in range(2):
        P.dma("pool", wos[:, :, j * 512:(j + 1) * 512], wv[:, :, j * 512:(j + 1) * 512], "wos%d" % j, writes=["wos%d" % j])
    for j in range(4):
        P.dma("sp", oT[:, j * 4:(j + 1) * 4, :], o_d.ap()[:, j * 4:(j + 1) * 4, :], "oT%d" % j, writes=["oT%d" % j])
    npy = 0
    for t in range(NT):
        b = t % 2
        rows = slice(t * 128, (t + 1) * 128)
        xk = "xs%d" % b
        P.dma("sp", xs[b][:], xin.ap()[rows, :], xk, writes=[xk])
        for half in range(2):
            p = py[npy % 2]
            pk = "py%d" % (npy % 2)
            npy += 1
            for hh in range(16):
                P.op("pe", lambda e, hh=hh, p=p, half=half, t=t: e.matmul(
                    p[:], lhsT=oT[:, hh, t * 128:(t + 1) * 128], rhs=wos[:, hh, half * 512:(half + 1) * 512],
                    start=(hh == 0), stop=(hh == 15)), reads=["oT%d" % (hh // 4), "wos%d" % half], writes=[pk])
            R(p[:], pk, seg_of(t), half, xs[b][:, half * 512:(half + 1) * 512], xk)
        P.dma("sp", xout.ap()[rows, :], xs[b][:], xk, reads=[xk])
    return P


def launch_a3(xc, oTs, w_out, mods, layer, with_ctx):
    maps = []
    for i in range(NCORE):
        b = i // 4
        maps.append({"xin": xc[i], "oT": oTs[i], "wo": w_out,
                     "gate": np.stack([bc_rows(mods[b, layer, 2 * D:3 * D]), bc_rows(mods[2, layer, 2 * D:3 * D])], 0)})
    res = run(build_a3(), maps)
    out = []
    for i in range(NCORE):
        xo = np.array(res[i]["xout"])
        if not with_ctx:
            xo[0:CTX] = xc[i][0:CTX]
        out.append(xo)
    return out


def kernel(x, c, ctx, c_ctx, ada_w, ada_b, norm1_g, norm2_g, mlp_w1, mlp_w2, hyb_w_in, hyb_gate_b,
           mlstm_norm_g, conv_w, hyb_w_out, att_w_in, q_norm_g, k_norm_g, att_w_out):
    f = lambda a: np.ascontiguousarray(np.asarray(a, np.float32))
    x, c, ctx, c_ctx, ada_w, ada_b = f(x), f(c), f(ctx), f(c_ctx), f(ada_w), f(ada_b)
    mods = launch_mods(c, c_ctx, ada_w, ada_b)
    xc = [np.ascontiguousarray(np.concatenate([ctx[i // 4], x[i // 4, (i % 4) * LAT:(i % 4 + 1) * LAT]], 0)) for i in range(NCORE)]
    for layer in range(4):
        if layer % 2 == 0:
            e = layer // 2
            p1 = launch_p1(xc, f(hyb_w_in[e]), f(hyb_gate_b[e]), f(norm1_g[layer]), mods, layer)
            H = launch_p2(p1)
            xc = launch_p3(xc, H, p1, f(mlstm_norm_g[e]), f(conv_w[e]), f(hyb_w_out[e]), mods, layer)
        else:
            o = layer // 2
            a1 = launch_a1(xc, f(att_w_in[o]), f(q_norm_g[o]), f(k_norm_g[o]), f(norm1_g[layer]), mods, layer)
            oTs = launch_a2g(a1)
            xc = launch_a3(xc, oTs, f(att_w_out[o]), mods, layer, layer != 3)
        xc = launch_mlp(xc, f(mlp_w1[layer]), f(mlp_w2[layer]), f(norm2_g[layer]), mods, layer)
    out = np.zeros((2, SEQ, D), np.float32)
    for i in range(NCORE):
        out[i // 4, (i % 4) * LAT:(i % 4 + 1) * LAT] = xc[i][CTX:]
    return out
```

```python
import contextlib
import numpy as np
import ml_dtypes
import concourse.bass as bass
import concourse.mybir as mybir
from concourse.bass_utils import run_bass_kernel_spmd

F32 = mybir.dt.float32
BF16 = mybir.dt.bfloat16
AF = mybir.ActivationFunctionType
ALU = mybir.AluOpType
AX = mybir.AxisListType
NPBF = ml_dtypes.bfloat16

D = 1024
SEQ = 8192
CTX = 256
NCORE = 8
LAT = 2048
NTOK = CTX + LAT
NT = NTOK // 128
HID = 4096
EPS = 1e-6
EPOCH = 20000


class Prog:
    ENG = ("pe", "act", "dve", "pool", "sp")

    def __init__(self):
        self.nc = bass.Bass("TRN2", target_bir_lowering=False)
        self.q = {e: [] for e in self.ENG}
        self.cnt = {e: 0 for e in self.ENG}
        self.esem = {e: [] for e in self.ENG}
        self.known = {e: {} for e in self.ENG}
        self.sems = []
        self.lastw = {}
        self.readers = {}
        self.dslot = {}
        self.uid = 0

    def new_sem(self, name):
        s = self.nc.alloc_semaphore(name)
        self.sems.append(s)
        return len(self.sems) - 1

    def sb(self, name, shape, dtype):
        return self.nc.alloc_sbuf_tensor(name, list(shape), dtype)

    def ps(self, name, shape, dtype):
        return self.nc.alloc_psum_tensor(name, list(shape), dtype)

    def dram_in(self, name, shape, dtype):
        return self.nc.dram_tensor(name, list(shape), dtype, kind="ExternalInput")

    def dram_out(self, name, shape, dtype):
        return self.nc.dram_tensor(name, list(shape), dtype, kind="ExternalOutput")

    def _needs(self, reads, writes):
        need = {}

        def add(tok):
            if tok is None:
                return
            s, v = tok
            if need.get(s, 0) < v:
                need[s] = v
        for r in reads:
            add(self.lastw.get(r))
        for w in writes:
            add(self.lastw.get(w))
            for s, v in self.readers.get(w, {}).items():
                add((s, v))
        return need

    def _emit_waits(self, eng, need, own_sems=()):
        kn = self.known[eng]
        for s, v in need.items():
            if s in own_sems:
                continue
            if kn.get(s, 0) >= v:
                continue
            kn[s] = v
            self.q[eng].append(("wait", s, v))

    def _record(self, tok, reads, writes):
        for r in reads:
            d = self.readers.setdefault(r, {})
            if d.get(tok[0], 0) < tok[1]:
                d[tok[0]] = tok[1]
        for w in writes:
            self.lastw[w] = tok
            self.readers[w] = {}

    def op(self, eng, fn, reads=(), writes=()):
        need = self._needs(reads, writes)
        own = tuple(self.esem[eng]) if eng == "pe" else ()
        self._emit_waits(eng, need, own)
        idx = self.cnt[eng]
        self.cnt[eng] = idx + 1
        ep = idx // EPOCH
        while len(self.esem[eng]) <= ep:
            self.esem[eng].append(self.new_sem("e_%s_%d" % (eng, len(self.esem[eng]))))
        s = self.esem[eng][ep]
        tok = (s, idx - ep * EPOCH + 1)
        self.q[eng].append(("op", fn, s, 1))
        self._record(tok, reads, writes)
        return tok

    def dma(self, eng, out, in_, slot, reads=(), writes=()):
        if slot not in self.dslot:
            self.dslot[slot] = [self.new_sem("d%d" % len(self.dslot)), 0]
        s, c = self.dslot[slot]
        need = self._needs(reads, writes)
        if c > 0 and need.get(s, 0) < c:
            need[s] = c
        self._emit_waits(eng, need)
        tok = (s, c + 16)
        self.dslot[slot][1] = c + 16
        self.q[eng].append(("op", lambda e, o=out, i=in_: e.dma_start(out=o, in_=i), s, 16))
        self._record(tok, reads, writes)
        return tok

    def finish(self):
        for slot, (s, c) in self.dslot.items():
            if c > 0 and self.known["sp"].get(s, 0) < c:
                self.known["sp"][s] = c
                self.q["sp"].append(("wait", s, c))
        nc = self.nc
        sems = self.sems

        def replay(items, e):
            for it in items:
                if it[0] == "wait":
                    e.wait_ge(sems[it[1]], it[2])
                else:
                    ins = it[1](e)
                    ins.then_inc(sems[it[2]], it[3])

        with nc.Block() as block:
            @block.tensor
            def _(e):
                replay(self.q["pe"], e)

            @block.scalar
            def _(e):
                replay(self.q["act"], e)

            @block.vector
            def _(e):
                replay(self.q["dve"], e)

            @block.gpsimd
            def _(e):
                replay(self.q["pool"], e)

            @block.sync
            def _(e):
                replay(self.q["sp"], e)
        return nc


_PROF = None


def run(prog, in_maps):
    nc = prog.finish()
    if _PROF is not None:
        res = run_bass_kernel_spmd(nc, in_maps, core_ids=list(range(NCORE)), trace=True)
        _PROF.append(getattr(res, "exec_time_ns", None))
    else:
        res = run_bass_kernel_spmd(nc, in_maps, core_ids=list(range(NCORE)))
    return res.results


def build_mods():
    P = Prog()
    CW = 6144 // NCORE
    cT = P.dram_in("cT", [128, 8, 4], F32)
    aw = P.dram_in("aw", [4, 1024, CW], F32)
    ab = P.dram_in("ab", [4, 4, CW], F32)
    out = P.dram_out("mods", [4, 4, CW], F32)
    cs = P.sb("cs", [128, 8, 4], F32)
    sg = P.sb("sg", [128, 8, 4], F32)
    w = [P.sb("w%d" % i, [128, 8, CW], F32) for i in range(2)]
    bs = P.sb("bs", [4, 4, CW], F32)
    o = P.sb("o", [4, 4, CW], F32)
    pp = [P.ps("pp%d" % i, [4, 2, 512], F32) for i in range(2)]
    P.dma("sp", cs[:], cT.ap(), "cs", writes=["cs"])
    P.dma("sp", bs[:], ab.ap().rearrange("l r c -> r l c"), "bs", writes=["bs"])
    P.op("act", lambda e: e.activation(out=sg[:], in_=cs[:], func=AF.Sigmoid), reads=["cs"], writes=["sg"])
    P.op("dve", lambda e: e.tensor_tensor(out=sg[:], in0=sg[:], in1=cs[:], op=ALU.mult), reads=["cs", "sg"], writes=["sg"])
    for L in range(4):
        wb = w[L % 2]
        P.dma("sp", wb[:], aw.ap()[L].rearrange("(c p) n -> p c n", p=128), "w%d" % (L % 2), writes=["w%d" % (L % 2)])
        for h in range(2):
            n0, n1 = h * 384, (h + 1) * 384
            for c in range(8):
                P.op("pe", lambda e, c=c, h=h, wb=wb, n0=n0, n1=n1, L=L: e.matmul(
                    pp[L % 2][:, h, 0:384], lhsT=sg[:, c, :], rhs=wb[:, c, n0:n1], start=(c == 0), stop=(c == 7)),
                    reads=["sg", "w%d" % (L % 2)], writes=["pp%d" % (L % 2)])
        for h in range(2):
            P.op("dve", lambda e, h=h, L=L: e.tensor_tensor(
                out=o[:, L, h * 384:(h + 1) * 384], in0=pp[L % 2][:, h, 0:384], in1=bs[:, L, h * 384:(h + 1) * 384], op=ALU.add),
                reads=["pp%d" % (L % 2), "bs"], writes=["o"])
    P.dma("sp", out.ap(), o[:], "o", reads=["o"])
    return P


def launch_mods(c, c_ctx, ada_w, ada_b):
    rows = np.zeros((4, D), np.float32)
    rows[0:2] = c
    rows[2] = c_ctx
    cT = np.ascontiguousarray(rows.reshape(4, 8, 128).transpose(2, 1, 0))
    CW = 6144 // NCORE
    maps = []
    for i in range(NCORE):
        sl = slice(i * CW, (i + 1) * CW)
        maps.append({"cT": cT, "aw": np.ascontiguousarray(ada_w[:, :, sl]),
                     "ab": np.ascontiguousarray(np.broadcast_to(ada_b[:, None, sl], (4, 4, CW)))})
    res = run(build_mods(), maps)
    mods = np.concatenate([r["mods"] for r in res], axis=2)
    return mods


def seg_of(tile):
    return 1 if tile < 2 else 0


def col_layout(v):
    v = np.asarray(v, np.float32)
    lead = v.shape[:-1]
    a = v.reshape(lead + (8, 128))
    return np.ascontiguousarray(np.moveaxis(a, -1, 0))


def bc_rows(v, n=128):
    v = np.asarray(v, np.float32)
    return np.ascontiguousarray(np.broadcast_to(v[None], (n,) + v.shape))


class NormT:
    def __init__(self, P, shift_idx, scale_idx):
        self.P = P
        self.ident_d = P.dram_in("ident", [128, 128], F32)
        self.g_d = P.dram_in("gcol", [128, 8], F32)
        self.m_d = P.dram_in("modcol", [128, 2, 6, 8], F32)
        self.ident = P.sb("ident_s", [128, 128], BF16)
        self.g = P.sb("g_s", [128, 8], F32)
        self.m = P.sb("m_s", [128, 2, 6, 8], F32)
        self.sc = P.sb("sc_s", [128, 2, 8], F32)
        self.junk = P.sb("junk", [128, 1024], BF16)
        self.ss = [P.sb("ss%d" % i, [128, 4], F32) for i in range(2)]
        self.xn = [P.sb("xn%d" % i, [128, 1024], BF16) for i in range(2)]
        self.pT = [P.ps("pT%d" % i, [128, 1024], BF16) for i in range(2)]
        self.n = 0
        self.shift_idx = shift_idx
        P.dma("pool", self.ident[:], self.ident_d.ap(), "ident", writes=["ident"])
        P.dma("sp", self.g[:], self.g_d.ap(), "g_s", writes=["g_s"])
        P.dma("sp", self.m[:], self.m_d.ap(), "m_s", writes=["m_s"])
        for s in range(2):
            P.op("dve", lambda e, s=s: e.scalar_tensor_tensor(
                out=self.sc[:, s, :], in0=self.m[:, s, scale_idx, :], scalar=1.0, in1=self.g[:],
                op0=ALU.add, op1=ALU.mult), reads=["g_s", "m_s"], writes=["sc_s"])

    def __call__(self, x_ap, xkey, seg, dst_fn, dkey):
        P = self.P
        b = self.n % 2
        self.n += 1
        ss, xn, pT = self.ss[b], self.xn[b], self.pT[b]
        kss, kxn, kpT = "ss%d" % b, "xn%d" % b, "pT%d" % b
        P.op("act", lambda e: e.activation(out=self.junk[:], in_=x_ap, func=AF.Square, accum_out=ss[:, 0:1]),
             reads=[xkey], writes=["junk", kss])
        P.op("dve", lambda e: e.tensor_scalar(out=ss[:, 1:2], in0=ss[:, 0:1], scalar1=1.0 / D, scalar2=EPS,
                                              op0=ALU.mult, op1=ALU.add), reads=[kss], writes=[kss])
        P.op("act", lambda e: e.activation(out=ss[:, 2:3], in_=ss[:, 1:2], func=AF.Sqrt), reads=[kss], writes=[kss])
        P.op("dve", lambda e: e.reciprocal(out=ss[:, 3:4], in_=ss[:, 2:3]), reads=[kss], writes=[kss])
        P.op("act", lambda e: e.activation(out=xn[:], in_=x_ap, func=AF.Copy, scale=ss[:, 3:4]),
             reads=[xkey, kss], writes=[kxn])
        for c in range(8):
            P.op("pe", lambda e, c=c: e.transpose(out=pT[:, c * 128:(c + 1) * 128], in_=xn[:, c * 128:(c + 1) * 128],
                                                   identity=self.ident[:]), reads=[kxn, "ident"], writes=[kpT])
        for c in range(8):
            sc = self.sc[:, seg, c:c + 1]
            tc = self.m[:, seg, self.shift_idx, c:c + 1]
            if b == 0:
                P.op("dve", lambda e, c=c, sc=sc, tc=tc: e.tensor_scalar(
                    out=dst_fn(c), in0=pT[:, c * 128:(c + 1) * 128], scalar1=sc, scalar2=tc, op0=ALU.mult, op1=ALU.add),
                    reads=[kpT, "sc_s", "m_s"], writes=[dkey])
            else:
                P.op("act", lambda e, c=c, sc=sc, tc=tc: e.activation(
                    out=dst_fn(c), in_=pT[:, c * 128:(c + 1) * 128], func=AF.Identity, scale=sc, bias=tc),
                    reads=[kpT, "sc_s", "m_s"], writes=[dkey])


def norm_inputs(gvec, mods_b, mods_c):
    m = np.stack([np.asarray(mods_b).reshape(6, D), np.asarray(mods_c).reshape(6, D)], 0)
    return {"ident": np.eye(128, dtype=np.float32), "gcol": col_layout(gvec), "modcol": col_layout(m)}


def load_weight_bf16(P, dst, src_ap, key, nsplit, split_axis_len, mk_dst, mk_src):
    for j in range(nsplit):
        P.dma("pool", mk_dst(j), mk_src(j), "%s_%d" % (key, j), writes=[key])


class Residual:
    def __init__(self, P, name):
        self.P = P
        self.gd = P.dram_in(name, [2, 128, 1024], F32)
        self.g = P.sb(name + "_s", [128, 2, 1024], F32)
        self.key = name
        self.tmp = [P.sb(name + "_t%d" % i, [128, 512], F32) for i in range(2)]
        self.n = 0
        P.dma("sp", self.g[:], self.gd.ap().rearrange("s p d -> p s d"), name, writes=[name])

    def __call__(self, py_ap, pykey, seg, half, x_ap, xkey):
        P = self.P
        b = self.n % 2
        self.n += 1
        t = self.tmp[b]
        tk = self.key + "_t%d" % b
        P.op("dve", lambda e: e.tensor_tensor(out=t[:], in0=py_ap, in1=self.g[:, seg, half * 512:(half + 1) * 512],
                                              op=ALU.mult), reads=[pykey, self.key], writes=[tk])
        P.op("pool", lambda e: e.tensor_tensor(out=x_ap, in0=x_ap, in1=t[:], op=ALU.add), reads=[tk, xkey], writes=[xkey])


MLP_GROUPS = [list(range(0, 4)), list(range(4, 8)), list(range(8, 12)), list(range(12, 16)), [16, 17]]


def build_mlp():
    P = Prog()
    xin = P.dram_in("xin", [NTOK, D], F32)
    w1 = P.dram_in("w1", [D, HID], F32)
    w2 = P.dram_in("w2", [HID, D], F32)
    xout = P.dram_out("xout", [NTOK, D], F32)
    N = NormT(P, 3, 4)
    R = Residual(P, "gate")
    xs = P.sb("xs", [128, 8, 1024], F32)
    hT = [P.sb("hT%d" % i, [128, 8, 512], BF16) for i in range(2)]
    hid = P.sb("hid", [128, 32, 512], BF16)
    w2s = P.sb("w2s", [128, 32, 1024], BF16)
    w1b = [P.sb("w1b%d" % i, [128, 8, 512], BF16) for i in range(2)]
    rl = [P.sb("rl%d" % i, [128, 512], BF16) for i in range(2)]
    ph = [P.ps("ph%d" % i, [128, 512], F32) for i in range(2)]
    py = [P.ps("py%d" % i, [128, 512], F32) for i in range(2)]
    w2v = w2.ap().rearrange("(k p) d -> p k d", p=128)
    w1v = w1.ap().rearrange("(c p) h -> p c h", p=128)
    nw = 0
    nph = 0
    npy = 0
    for gi, tiles in enumerate(MLP_GROUPS):
        n = len(tiles)
        NN = 128 * n
        hb_ = hT[gi % 2]
        hk = "hT%d" % (gi % 2)
        for j, t in enumerate(tiles):
            slot = t % 8
            xk = "xs%d" % slot
            P.dma("sp", xs[:, slot, :], xin.ap()[t * 128:(t + 1) * 128, :], xk, writes=[xk])
            N(xs[:, slot, :], xk, seg_of(t), lambda c, j=j, hb_=hb_: hb_[:, c, j * 128:(j + 1) * 128], hk)
        for hb in range(8):
            wb = w1b[nw % 2]
            wk = "w1b%d" % (nw % 2)
            nw += 1
            P.dma("pool", wb[:], w1v[:, :, hb * 512:(hb + 1) * 512], wk, writes=[wk])
            if gi == 0:
                P.dma("pool", w2s[:, hb * 4:(hb + 1) * 4, :], w2v[:, hb * 4:(hb + 1) * 4, :], "w2s_%d" % hb, writes=["w2s_%d" % hb])
            for hc in range(4):
                p = ph[nph % 2]
                pk = "ph%d" % (nph % 2)
                r = rl[nph % 2]
                rk = "rl%d" % (nph % 2)
                nph += 1
                for c in range(8):
                    P.op("pe", lambda e, c=c, p=p, wb=wb, hc=hc, hb_=hb_, NN=NN: e.matmul(
                        p[:, 0:NN], lhsT=wb[:, c, hc * 128:(hc + 1) * 128], rhs=hb_[:, c, 0:NN],
                        start=(c == 0), stop=(c == 7)), reads=[wk, hk], writes=[pk])
                P.op("act", lambda e, p=p, r=r, NN=NN: e.activation(out=r[:, 0:NN], in_=p[:, 0:NN], func=AF.Relu),
                     reads=[pk], writes=[rk])
                P.op("pool", lambda e, r=r, NN=NN, k=hb * 4 + hc: e.tensor_tensor(
                    out=hid[:, k, 0:NN], in0=r[:, 0:NN], in1=r[:, 0:NN], op=ALU.mult), reads=[rk], writes=["hid"])
        for j, t in enumerate(tiles):
            slot = t % 8
            xk = "xs%d" % slot
            for half in range(2):
                p = py[npy % 2]
                pk = "py%d" % (npy % 2)
                npy += 1
                for k in range(32):
                    P.op("pe", lambda e, k=k, p=p, j=j, half=half: e.matmul(
                        p[:], lhsT=hid[:, k, j * 128:(j + 1) * 128], rhs=w2s[:, k, half * 512:(half + 1) * 512],
                        start=(k == 0), stop=(k == 31)), reads=["hid", "w2s_%d" % (k // 4)], writes=[pk])
                R(p[:], pk, seg_of(t), half, xs[:, slot, half * 512:(half + 1) * 512], xk)
            P.dma("sp", xout.ap()[t * 128:(t + 1) * 128, :], xs[:, slot, :], xk, reads=[xk])
    return P


def launch_mlp(xc, w1, w2, g2, mods, layer):
    maps = []
    for i in range(NCORE):
        b = i // 4
        m = {"xin": xc[i], "w1": w1, "w2": w2}
        m.update(norm_inputs(g2, mods[b, layer], mods[2, layer]))
        m["gate"] = np.stack([bc_rows(mods[b, layer, 5 * D:6 * D]), bc_rows(mods[2, layer, 5 * D:6 * D])], 0)
        maps.append(m)
    res = run(build_mlp(), maps)
    return [r["xout"] for r in res]


HYB_IN = 3088


def build_p1():
    P = Prog()
    xin = P.dram_in("xin", [NTOK, D], F32)
    win = P.dram_in("win", [D, HYB_IN], F32)
    gbb = P.dram_in("gateb", [128, 16], F32)
    qk_o = P.dram_out("qk", [NTOK, 512], BF16)
    v_o = P.dram_out("v", [NTOK, 512], BF16)
    g_o = P.dram_out("gates", [NTOK, 16], F32)
    s_o = P.dram_out("side", [NTOK, 3, 512], F32)
    N = NormT(P, 0, 1)
    ws = P.sb("ws", [128, 8, HYB_IN], BF16)
    gb_s = P.sb("gb_s", [128, 16], F32)
    xs = [P.sb("xs%d" % i, [128, 1024], F32) for i in range(2)]
    hT = [P.sb("hT%d" % i, [128, 8, 128], BF16) for i in range(2)]
    qks = [P.sb("qks%d" % i, [128, 512], BF16) for i in range(2)]
    vs = [P.sb("vs%d" % i, [128, 512], BF16) for i in range(2)]
    gs = [P.sb("gs%d" % i, [128, 16], F32) for i in range(2)]
    ge = [P.sb("ge%d" % i, [128, 16], F32) for i in range(2)]
    sd = [P.sb("sd%d" % i, [128, 3, 512], F32) for i in range(2)]
    gc = [P.sb("gc%d" % i, [128, 512], F32) for i in range(2)]
    pp = [P.ps("pp%d" % i, [128, 512], F32) for i in range(4)]
    wv = win.ap().rearrange("(c p) n -> p c n", p=128)
    blocks = [(0, 512), (512, 1024), (1024, 1536), (1536, 1552), (1552, 2064), (2064, 2576), (2576, 3088)]
    for j, (a, b) in enumerate(blocks):
        P.dma("pool", ws[:, :, a:b], wv[:, :, a:b], "ws_%d" % j, writes=["ws_%d" % j])
    P.dma("sp", gb_s[:], gbb.ap(), "gb_s", writes=["gb_s"])
    npp = 0
    for t in range(NT):
        b = t % 2
        xk, hk = "xs%d" % b, "hT%d" % b
        rows = slice(t * 128, (t + 1) * 128)
        P.dma("sp", xs[b][:], xin.ap()[rows, :], xk, writes=[xk])
        N(xs[b][:], xk, seg_of(t), lambda c, b=b: hT[b][:, c, :], hk)
        for j, (a, bb) in enumerate(blocks):
            w = bb - a
            p = pp[npp % 4]
            pk = "pp%d" % (npp % 4)
            npp += 1
            for c in range(8):
                P.op("pe", lambda e, c=c, p=p, a=a, bb=bb, w=w, b=b: e.matmul(
                    p[:, 0:w], lhsT=hT[b][:, c, :], rhs=ws[:, c, a:bb], start=(c == 0), stop=(c == 7)),
                    reads=[hk, "ws_%d" % j], writes=[pk])
            if j == 0:
                P.op("act", lambda e, p=p, b=b: e.activation(out=qks[b][:, 0:256], in_=p[:, 0:256], func=AF.Copy, scale=0.125),
                     reads=[pk], writes=["qks%d" % b])
                P.op("act", lambda e, p=p, b=b: e.activation(out=qks[b][:, 256:512], in_=p[:, 256:512], func=AF.Copy),
                     reads=[pk], writes=["qks%d" % b])
                P.dma("sp", qk_o.ap()[rows, :], qks[b][:], "qks%d" % b, reads=["qks%d" % b])
            elif j == 1:
                P.op("act", lambda e, p=p, b=b: e.activation(out=vs[b][:], in_=p[:], func=AF.Copy),
                     reads=[pk], writes=["vs%d" % b])
                P.dma("sp", v_o.ap()[rows, :], vs[b][:], "vs%d" % b, reads=["vs%d" % b])
            elif j == 2:
                P.op("dve", lambda e, p=p, b=b: e.tensor_copy(out=sd[b][:, 0, :], in_=p[:]), reads=[pk], writes=["sd%d" % b])
            elif j == 3:
                gk = "gs%d" % b
                P.op("dve", lambda e, p=p, b=b: e.tensor_tensor(out=gs[b][:], in0=p[:, 0:16], in1=gb_s[:], op=ALU.add),
                     reads=[pk, "gb_s"], writes=[gk])
                P.op("act", lambda e, b=b: e.activation(out=ge[b][:], in_=gs[b][:], func=AF.Exp, scale=-1.0),
                     reads=[gk], writes=["ge%d" % b])
                P.op("act", lambda e, b=b: e.activation(out=ge[b][:], in_=ge[b][:], func=AF.Ln, bias=1.0),
                     reads=["ge%d" % b], writes=["ge%d" % b])
                for c0 in (4, 12):
                    P.op("dve", lambda e, b=b, c0=c0: e.tensor_scalar(out=gs[b][:, c0:c0 + 4], in0=ge[b][:, c0:c0 + 4],
                                                                      scalar1=-1.0, scalar2=None, op0=ALU.mult),
                         reads=["ge%d" % b, gk], writes=[gk])
                P.dma("sp", g_o.ap()[rows, :], gs[b][:], gk, reads=[gk])
            elif j == 4:
                P.op("act", lambda e, p=p, b=b: e.activation(out=sd[b][:, 1, :], in_=p[:], func=AF.Copy), reads=[pk], writes=["sd%d" % b])
            elif j == 5:
                P.op("act", lambda e, p=p, b=b: e.activation(out=gc[b][:], in_=p[:], func=AF.Copy), reads=[pk], writes=["gc%d" % b])
            else:
                P.op("dve", lambda e, p=p, b=b: e.tensor_tensor(out=sd[b][:, 2, :], in0=p[:], in1=gc[b][:], op=ALU.mult),
                     reads=[pk, "gc%d" % b], writes=["sd%d" % b])
                P.dma("sp", s_o.ap()[rows], sd[b][:], "sd%d" % b, reads=["sd%d" % b])
    return P


def launch_p1(xc, w_in, gate_b, g1, mods, layer):
    maps = []
    for i in range(NCORE):
        b = i // 4
        m = {"xin": xc[i], "win": w_in, "gateb": bc_rows(gate_b)}
        m.update(norm_inputs(g1, mods[b, layer], mods[2, layer]))
        maps.append(m)
    return run(build_p1(), maps)


NTL = (CTX + SEQ) // 128


def build_p2():
    P = Prog()
    qT_d = P.dram_in("qT", [64, NTL * 128], BF16)
    kT_d = P.dram_in("kT", [64, NTL * 128], BF16)
    kt_d = P.dram_in("ktm", [128, NTL, 64], BF16)
    v_d = P.dram_in("v", [128, NTL, 128], BF16)
    g_d = P.dram_in("g4", [128, NTL, 4], F32)
    tri_d = P.dram_in("tri", [2, 128, 128], F32)
    neg_d = P.dram_in("neg", [2, 128, 128], F32)
    idn_d = P.dram_in("ident", [128, 128], F32)
    h_o = P.dram_out("h", [128, NTL, 128], F32)
    qT = P.sb("qT_s", [64, NTL * 128], BF16)
    kT = P.sb("kT_s", [64, NTL * 128], BF16)
    ktm = P.sb("ktm_s", [128, NTL, 64], BF16)
    vp = P.sb("vp_s", [128, NTL, 129], BF16)
    g4 = P.sb("g4_s", [128, NTL, 4], F32)
    tri = P.sb("tri_s", [128, 2, 128], F32)
    neg = P.sb("neg_s", [128, 2, 128], BF16)
    idn = P.sb("idn_s", [128, 128], BF16)
    ones = P.sb("ones_s", [128, 128], F32)
    hacc = P.sb("hacc", [128, NTL, 128], F32)
    St = P.sb("St", [64, 129], F32)
    Stb = P.sb("Stb", [64, 129], BF16)
    NB = 2
    sb4 = [P.sb("sb4_%d" % i, [128, 4], F32) for i in range(NB)]
    cb = [P.sb("cb_%d" % i, [128, 8], F32) for i in range(NB)]
    LFb = [P.sb("LFb_%d" % i, [128, 128], F32) for i in range(NB)]
    Dm = [P.sb("Dm_%d" % i, [128, 128], F32) for i in range(NB)]
    Pm = [P.sb("Pm_%d" % i, [128, 128], BF16) for i in range(NB)]
    Kw = [P.sb("Kw_%d" % i, [128, 64], BF16) for i in range(NB)]
    tB = [P.sb("tB_%d" % i, [128, 129], F32) for i in range(NB)]
    num = [P.sb("num_%d" % i, [128, 129], F32) for i in range(NB)]
    pb = P.ps("pb", [128, 4], F32)
    pd = P.ps("pd", [128, 128], F32)
    pS = P.ps("pS", [128, 128], F32)
    pA = P.ps("pA", [128, 129], F32)
    pB = P.ps("pB", [128, 129], F32)
    pU = P.ps("pU", [64, 129], F32)
    P.dma("sp", qT[:], qT_d.ap(), "qT", writes=["qT"])
    P.dma("sp", kT[:], kT_d.ap(), "kT", writes=["kT"])
    P.dma("sp", ktm[:], kt_d.ap(), "ktm", writes=["ktm"])
    P.dma("sp", vp[:, :, 0:128], v_d.ap(), "vp", writes=["vp"])
    P.dma("sp", g4[:], g_d.ap(), "g4", writes=["g4"])
    P.dma("sp", tri[:], tri_d.ap().rearrange("a p n -> p a n"), "tri", writes=["tri"])
    P.dma("pool", neg[:], neg_d.ap().rearrange("a p n -> p a n"), "neg", writes=["neg"])
    P.dma("pool", idn[:], idn_d.ap(), "idn", writes=["idn"])
    P.op("dve", lambda e: e.memset(ones[:], 1.0), writes=["ones"])
    P.op("pool", lambda e: e.memset(vp[:, :, 128:129], 1.0), writes=["vp1"])
    u = 0
    for d in range(2):
        order = list(range(NTL)) if d == 0 else [1, 0] + list(range(NTL - 1, 1, -1))
        P.op("dve", lambda e: e.memset(St[:], 0.0), writes=["St"])
        P.op("dve", lambda e: e.memset(Stb[:], 0.0), writes=["Stb"])
        for t in order:
            b = u % NB
            u += 1
            cols = slice(t * 128, (t + 1) * 128)
            ig = g4[:, t, 2 * d:2 * d + 1]
            lf = g4[:, t, 2 * d + 1:2 * d + 2]
            k4, kc = "sb4_%d" % b, "cb_%d" % b
            P.op("pe", lambda e, t=t, d=d: e.matmul(pb[:, 0:2], lhsT=tri[:, d, :], rhs=g4[:, t, 2 * d:2 * d + 2], start=True, stop=True),
                 reads=["tri", "g4"], writes=["pb"])
            P.op("pe", lambda e, t=t, d=d: e.matmul(pb[:, 2:4], lhsT=ones[:], rhs=g4[:, t, 2 * d:2 * d + 2], start=True, stop=True),
                 reads=["ones", "g4"], writes=["pb"])
            P.op("dve", lambda e, b=b: e.tensor_copy(out=sb4[b][:], in_=pb[:]), reads=["pb"], writes=[k4])
            P.op("dve", lambda e, b=b, ig=ig: e.tensor_tensor(out=cb[b][:, 0:1], in0=ig, in1=sb4[b][:, 1:2], op=ALU.subtract),
                 reads=[k4, "g4"], writes=[kc])
            P.op("act", lambda e, b=b: e.activation(out=cb[b][:, 1:2], in_=sb4[b][:, 1:2], func=AF.Exp), reads=[k4, kc], writes=[kc])
            P.op("act", lambda e, b=b: e.activation(out=cb[b][:, 2:3], in_=cb[b][:, 0:1], func=AF.Exp, bias=sb4[b][:, 3:4]),
                 reads=[k4, kc], writes=[kc])
            P.op("act", lambda e, b=b: e.activation(out=cb[b][:, 3:4], in_=sb4[b][:, 3:4], func=AF.Exp), reads=[k4, kc], writes=[kc])
            P.op("dve", lambda e, b=b, lf=lf: e.tensor_scalar(out=LFb[b][:], in0=ones[:], scalar1=lf, scalar2=None, op0=ALU.mult),
                 reads=["ones", "g4"], writes=["LFb_%d" % b])
            P.op("pe", lambda e, d=d: e.matmul(pd[:], lhsT=idn[:], rhs=neg[:, d, :], start=True, stop=False),
                 reads=["idn", "neg"], writes=["pd"])
            P.op("pe", lambda e, b=b, d=d: e.matmul(pd[:], lhsT=LFb[b][:], rhs=tri[:, d, :], start=False, stop=True),
                 reads=["LFb_%d" % b, "tri"], writes=["pd"])
            P.op("act", lambda e, b=b: e.activation(out=Dm[b][:], in_=pd[:], func=AF.Exp, bias=cb[b][:, 0:1]),
                 reads=["pd", kc], writes=["Dm_%d" % b])
            P.op("pe", lambda e, cols=cols: e.matmul(pS[:], lhsT=kT[:, cols], rhs=qT[:, cols], start=True, stop=True),
                 reads=["kT", "qT"], writes=["pS"])
            P.op("dve", lambda e, b=b: e.tensor_tensor(out=Pm[b][:], in0=pS[:], in1=Dm[b][:], op=ALU.mult),
                 reads=["pS", "Dm_%d" % b], writes=["Pm_%d" % b])
            P.op("dve", lambda e, b=b, t=t: e.tensor_scalar(out=Kw[b][:], in0=ktm[:, t, :], scalar1=cb[b][:, 2:3], scalar2=None, op0=ALU.mult),
                 reads=["ktm", kc], writes=["Kw_%d" % b])
            P.op("pe", lambda e, b=b, t=t: e.matmul(pA[:], lhsT=Pm[b][:], rhs=vp[:, t, :], start=True, stop=True),
                 reads=["Pm_%d" % b, "vp", "vp1"], writes=["pA"])
            P.op("pe", lambda e, cols=cols: e.matmul(pB[:], lhsT=qT[:, cols], rhs=Stb[:], start=True, stop=True),
                 reads=["qT", "Stb"], writes=["pB"])
            P.op("act", lambda e, b=b: e.activation(out=tB[b][:], in_=pB[:], func=AF.Copy, scale=cb[b][:, 1:2]),
                 reads=["pB", kc], writes=["tB_%d" % b])
            P.op("dve", lambda e, b=b: e.tensor_tensor(out=num[b][:], in0=pA[:], in1=tB[b][:], op=ALU.add),
                 reads=["pA", "tB_%d" % b], writes=["num_%d" % b])
            P.op("act", lambda e, b=b: e.activation(out=cb[b][:, 4:5], in_=num[b][:, 128:129], func=AF.Abs),
                 reads=["num_%d" % b, kc], writes=[kc])
            P.op("dve", lambda e, b=b: e.tensor_scalar(out=cb[b][:, 4:5], in0=cb[b][:, 4:5], scalar1=1.0, scalar2=None,
                                                       op0=ALU.max), reads=[kc], writes=[kc])
            P.op("dve", lambda e, b=b: e.reciprocal(out=cb[b][:, 5:6], in_=cb[b][:, 4:5]), reads=[kc], writes=[kc])
            hk = "hacc%d" % t
            if d == 0:
                P.op("dve", lambda e, b=b, t=t: e.tensor_scalar(out=hacc[:, t, :], in0=num[b][:, 0:128], scalar1=cb[b][:, 5:6],
                                                                scalar2=None, op0=ALU.mult), reads=["num_%d" % b, kc], writes=[hk])
            else:
                P.op("dve", lambda e, b=b, t=t: e.scalar_tensor_tensor(out=hacc[:, t, :], in0=num[b][:, 0:128], scalar=cb[b][:, 5:6],
                                                                       in1=hacc[:, t, :], op0=ALU.mult, op1=ALU.add),
                     reads=["num_%d" % b, kc, hk], writes=[hk])
                P.dma("sp", h_o.ap()[:, t, :], hacc[:, t, :], "hout%d" % (t % 4), reads=[hk])
            P.op("pe", lambda e, b=b, t=t: e.matmul(pU[:], lhsT=Kw[b][:], rhs=vp[:, t, :], start=True, stop=True),
                 reads=["Kw_%d" % b, "vp", "vp1"], writes=["pU"])
            P.op("dve", lambda e, b=b: e.scalar_tensor_tensor(out=St[:], in0=St[:], scalar=cb[b][0:64, 3:4], in1=pU[:],
                                                              op0=ALU.mult, op1=ALU.add), reads=["pU", kc, "St"], writes=["St"])
            P.op("act", lambda e: e.activation(out=Stb[:], in_=St[:], func=AF.Copy), reads=["St"], writes=["Stb"])
    return P


def p2_consts():
    t = np.arange(128)
    tf = (t[:, None] <= t[None, :]).astype(np.float32)
    tb = (t[:, None] >= t[None, :]).astype(np.float32)
    negf = np.where(t[:, None] <= t[None, :], 0.0, -30000.0).astype(np.float32)
    negb = np.where(t[:, None] >= t[None, :], 0.0, -30000.0).astype(np.float32)
    return {"tri": np.stack([tf, tb]), "neg": np.stack([negf, negb]), "ident": np.eye(128, dtype=np.float32)}


def launch_p2(p1res):
    def full(name, b):
        parts = [p1res[4 * b][name][0:CTX]] + [p1res[4 * b + j][name][CTX:] for j in range(4)]
        return np.concatenate(parts, 0)
    consts = p2_consts()
    maps = []
    for i in range(NCORE):
        b, h = i // 4, i % 4
        qk = full("qk", b)
        v = full("v", b)
        g = full("gates", b)
        q = qk[:, h * 64:(h + 1) * 64]
        k = qk[:, 256 + h * 64:256 + (h + 1) * 64]
        m = {"qT": np.ascontiguousarray(q.T), "kT": np.ascontiguousarray(k.T),
             "ktm": np.ascontiguousarray(k.reshape(NTL, 128, 64).transpose(1, 0, 2)),
             "v": np.ascontiguousarray(v[:, h * 128:(h + 1) * 128].reshape(NTL, 128, 128).transpose(1, 0, 2)),
             "g4": np.ascontiguousarray(g[:, [h, 4 + h, 8 + h, 12 + h]].reshape(NTL, 128, 4).transpose(1, 0, 2))}
        m.update(consts)
        maps.append(m)
    res = run(build_p2(), maps)
    H = []
    for b in range(2):
        H.append(np.concatenate([res[4 * b + h]["h"].transpose(1, 0, 2).reshape(NTL * 128, 128) for h in range(4)], 1))
    return H


def build_p3():
    P = Prog()
    xin = P.dram_in("xin", [NTOK, D], F32)
    h_d = P.dram_in("h", [NTOK, 512], F32)
    side = P.dram_in("side", [NTOK, 3, 512], F32)
    gm1 = P.dram_in("gm1", [NTOK, 512], F32)
    gp1 = P.dram_in("gp1", [NTOK, 512], F32)
    mng = P.dram_in("mng", [128, 512], F32)
    cw = P.dram_in("cw", [128, 3, 512], F32)
    wo = P.dram_in("wo", [D, D], F32)
    idn_d = P.dram_in("ident", [128, 128], F32)
    xout = P.dram_out("xout", [NTOK, D], F32)
    R = Residual(P, "gate")
    idn = P.sb("idn", [128, 128], BF16)
    mng_s = P.sb("mng_s", [128, 512], F32)
    cw_s = P.sb("cw_s", [128, 3, 512], F32)
    wos = P.sb("wos", [128, 8, D], BF16)
    xs = [P.sb("xs%d" % i, [128, 1024], F32) for i in range(2)]
    hs = [P.sb("hs%d" % i, [128, 512], F32) for i in range(2)]
    sd = [P.sb("sd%d" % i, [128, 3, 512], F32) for i in range(2)]
    g1 = [P.sb("g1_%d" % i, [128, 2, 512], F32) for i in range(2)]
    hsq2 = [P.sb("hsq%d" % i, [128, 512], F32) for i in range(2)]
    st = [P.sb("st%d" % i, [128, 16], F32) for i in range(2)]
    og2 = [P.sb("og%d" % i, [128, 512], F32) for i in range(2)]
    t12 = [P.sb("t1_%d" % i, [128, 512], F32) for i in range(2)]
    t22 = [P.sb("t2_%d" % i, [128, 512], F32) for i in range(2)]
    mc = [P.sb("mc%d" % i, [128, 1024], BF16) for i in range(2)]
    mcT = [P.sb("mcT%d" % i, [128, 8, 128], BF16) for i in range(2)]
    pT = [P.ps("pT%d" % i, [128, 1024], BF16) for i in range(2)]
    py = [P.ps("py%d" % i, [128, 512], F32) for i in range(2)]
    P.dma("pool", idn[:], idn_d.ap(), "idn", writes=["idn"])
    P.dma("sp", mng_s[:], mng.ap(), "mng", writes=["mng"])
    P.dma("sp", cw_s[:], cw.ap(), "cw", writes=["cw"])
    wv = wo.ap().rearrange("(c p) n -> p c n", p=128)
    for j in range(2):
        P.dma("pool", wos[:, :, j * 512:(j + 1) * 512], wv[:, :, j * 512:(j + 1) * 512], "wos%d" % j, writes=["wos%d" % j])
    npy = 0
    for t in range(NT):
        b = t % 2
        rows = slice(t * 128, (t + 1) * 128)
        xk, hk, sk, gk, stk, mk, mtk, ptk = ("xs%d" % b, "hs%d" % b, "sd%d" % b, "g1_%d" % b, "st%d" % b, "mc%d" % b, "mcT%d" % b, "pT%d" % b)
        hsq, og, t1, t2 = hsq2[b], og2[b], t12[b], t22[b]
        khsq, kog, kt1, kt2 = "hsq%d" % b, "og%d" % b, "t1_%d" % b, "t2_%d" % b
        P.dma("sp", xs[b][:], xin.ap()[rows, :], xk, writes=[xk])
        P.dma("sp", hs[b][:], h_d.ap()[rows, :], hk, writes=[hk])
        P.dma("sp", sd[b][:], side.ap()[rows], sk, writes=[sk])
        P.dma("sp", g1[b][:, 0, :], gm1.ap()[rows, :], gk + "a", writes=[gk])
        P.dma("sp", g1[b][:, 1, :], gp1.ap()[rows, :], gk + "b", writes=[gk])
        P.op("pool", lambda e, b=b, hsq=hsq, og=og, t1=t1, t2=t2: e.tensor_tensor(out=hsq[:], in0=hs[b][:], in1=hs[b][:], op=ALU.mult), reads=[hk], writes=[khsq])
        P.op("dve", lambda e, b=b, hsq=hsq, og=og, t1=t1, t2=t2: e.tensor_reduce(out=st[b][:, 0:4], in_=hsq[:].rearrange("p (h d) -> p h d", h=4), axis=AX.X, op=ALU.add),
             reads=[khsq], writes=[stk])
        P.op("dve", lambda e, b=b, hsq=hsq, og=og, t1=t1, t2=t2: e.tensor_scalar(out=st[b][:, 4:8], in0=st[b][:, 0:4], scalar1=1.0 / 128, scalar2=EPS, op0=ALU.mult, op1=ALU.add),
             reads=[stk], writes=[stk])
        P.op("act", lambda e, b=b, hsq=hsq, og=og, t1=t1, t2=t2: e.activation(out=st[b][:, 8:12], in_=st[b][:, 4:8], func=AF.Sqrt), reads=[stk], writes=[stk])
        P.op("dve", lambda e, b=b, hsq=hsq, og=og, t1=t1, t2=t2: e.reciprocal(out=st[b][:, 12:16], in_=st[b][:, 8:12]), reads=[stk], writes=[stk])
        P.op("dve", lambda e, b=b, hsq=hsq, og=og, t1=t1, t2=t2: e.tensor_tensor(
            out=hsq[:].rearrange("p (h d) -> p h d", h=4), in0=hs[b][:].rearrange("p (h d) -> p h d", h=4),
            in1=st[b][:, 12:16].unsqueeze(2).to_broadcast([128, 4, 128]), op=ALU.mult), reads=[hk, stk, khsq], writes=[khsq])
        P.op("pool", lambda e, hsq=hsq, og=og, t1=t1, t2=t2: e.tensor_tensor(out=hsq[:], in0=hsq[:], in1=mng_s[:], op=ALU.mult), reads=[khsq, "mng"], writes=[khsq])
        P.op("act", lambda e, b=b, hsq=hsq, og=og, t1=t1, t2=t2: e.activation(out=og[:], in_=sd[b][:, 0, :], func=AF.Sigmoid), reads=[sk], writes=[kog])
        P.op("dve", lambda e, b=b, hsq=hsq, og=og, t1=t1, t2=t2: e.tensor_tensor(out=mc[b][:, 0:512], in0=hsq[:], in1=og[:], op=ALU.mult), reads=[khsq, kog], writes=[mk])
        P.op("pool", lambda e, b=b, hsq=hsq, og=og, t1=t1, t2=t2: e.tensor_tensor(out=t1[:], in0=g1[b][:, 0, :], in1=cw_s[:, 0, :], op=ALU.mult), reads=[gk, "cw"], writes=[kt1])
        P.op("pool", lambda e, b=b, hsq=hsq, og=og, t1=t1, t2=t2: e.tensor_tensor(out=t2[:], in0=sd[b][:, 2, :], in1=cw_s[:, 1, :], op=ALU.mult), reads=[sk, "cw"], writes=[kt2])
        P.op("pool", lambda e, hsq=hsq, og=og, t1=t1, t2=t2: e.tensor_tensor(out=t1[:], in0=t1[:], in1=t2[:], op=ALU.add), reads=[kt1, kt2], writes=[kt1])
        P.op("pool", lambda e, b=b, hsq=hsq, og=og, t1=t1, t2=t2: e.tensor_tensor(out=t2[:], in0=g1[b][:, 1, :], in1=cw_s[:, 2, :], op=ALU.mult), reads=[gk, "cw", kt1], writes=[kt2])
        P.op("pool", lambda e, hsq=hsq, og=og, t1=t1, t2=t2: e.tensor_tensor(out=t1[:], in0=t1[:], in1=t2[:], op=ALU.add), reads=[kt1, kt2], writes=[kt1])
        P.op("pool", lambda e, b=b, hsq=hsq, og=og, t1=t1, t2=t2: e.tensor_tensor(out=mc[b][:, 512:1024], in0=t1[:], in1=sd[b][:, 1, :], op=ALU.mult), reads=[kt1, sk], writes=[mk])
        for c in range(8):
            P.op("pe", lambda e, c=c, b=b: e.transpose(out=pT[b][:, c * 128:(c + 1) * 128], in_=mc[b][:, c * 128:(c + 1) * 128], identity=idn[:]),
                 reads=[mk, "idn"], writes=[ptk])
        if b == 0:
            P.op("dve", lambda e, b=b: e.tensor_copy(out=mcT[b][:], in_=pT[b][:].rearrange("p (c n) -> p c n", c=8)), reads=[ptk], writes=[mtk])
        else:
            P.op("act", lambda e, b=b: e.activation(out=mcT[b][:], in_=pT[b][:].rearrange("p (c n) -> p c n", c=8), func=AF.Copy),
                 reads=[ptk], writes=[mtk])
        for half in range(2):
            p = py[npy % 2]
            pk = "py%d" % (npy % 2)
            npy += 1
            for c in range(8):
                P.op("pe", lambda e, c=c, p=p, b=b, half=half: e.matmul(p[:], lhsT=mcT[b][:, c, :], rhs=wos[:, c, half * 512:(half + 1) * 512],
                                                                       start=(c == 0), stop=(c == 7)), reads=[mtk, "wos%d" % half], writes=[pk])
            R(p[:], pk, seg_of(t), half, xs[b][:, half * 512:(half + 1) * 512], xk)
        P.dma("sp", xout.ap()[rows, :], xs[b][:], xk, reads=[xk])
    return P


def launch_p3(xc, H, p1res, mng, conv_w, w_out, mods, layer):
    maps = []
    for i in range(NCORE):
        b, j = i // 4, i % 4
        gcu_c = p1res[4 * b]["side"][0:CTX, 2]
        gcu_l = np.concatenate([p1res[4 * b + jj]["side"][CTX:, 2] for jj in range(4)], 0)
        z = np.zeros((1, 512), np.float32)
        cm1 = np.concatenate([z, gcu_c[:-1]], 0)
        cp1 = np.concatenate([gcu_c[1:], z], 0)
        lm1 = np.concatenate([z, gcu_l[:-1]], 0)[j * LAT:(j + 1) * LAT]
        lp1 = np.concatenate([gcu_l[1:], z], 0)[j * LAT:(j + 1) * LAT]
        hh = np.concatenate([H[b][0:CTX], H[b][CTX + j * LAT:CTX + (j + 1) * LAT]], 0)
        m = {"xin": xc[i], "h": np.ascontiguousarray(hh), "side": p1res[i]["side"],
             "gm1": np.ascontiguousarray(np.concatenate([cm1, lm1], 0)), "gp1": np.ascontiguousarray(np.concatenate([cp1, lp1], 0)),
             "mng": bc_rows(mng), "cw": bc_rows(conv_w), "wo": w_out, "ident": np.eye(128, dtype=np.float32),
             "gate": np.stack([bc_rows(mods[b, layer, 2 * D:3 * D]), bc_rows(mods[2, layer, 2 * D:3 * D])], 0)}
        maps.append(m)
    res = run(build_p3(), maps)
    return [r["xout"] for r in res]


def build_a1(ntiles=NT, stage=9):
    P = Prog()
    xin = P.dram_in("xin", [NTOK, D], F32)
    win = P.dram_in("win", [D, 1536], F32)
    qkg = P.dram_in("qkg", [128, 20, 64], F32)
    cs_d = P.dram_in("ropeC", [LAT, 64], F32)
    sn_d = P.dram_in("ropeS", [LAT, 64], F32)
    qT_o = P.dram_out("qT", [1024, NTOK], BF16)
    kT_o = P.dram_out("kT", [256, NTOK], BF16)
    v_o = P.dram_out("v", [NTOK, 256], BF16)
    N = NormT(P, 0, 1)
    ws = P.sb("ws", [128, 8, 1536], BF16)
    qkg_s = P.sb("qkg_s", [128, 20, 64], F32)
    xs = [P.sb("xs%d" % i, [128, 1024], F32) for i in range(2)]
    hT = [P.sb("hT%d" % i, [128, 8, 128], BF16) for i in range(2)]
    pq = [P.sb("pq%d" % i, [128, 1280], F32) for i in range(2)]
    sq2 = [P.sb("sq%d" % i, [128, 1280], F32) for i in range(2)]
    ra2 = [P.sb("ra%d" % i, [128, 1280], F32) for i in range(2)]
    rb2 = [P.sb("rb%d" % i, [128, 1280], F32) for i in range(2)]
    st = [P.sb("st%d" % i, [128, 80], F32) for i in range(2)]
    cs = [P.sb("cs%d" % i, [128, 2, 64], F32) for i in range(2)]
    vs = [P.sb("vs%d" % i, [128, 256], BF16) for i in range(2)]
    rot = [P.sb("rot%d" % i, [128, 1280], BF16) for i in range(2)]
    qkT = [P.sb("qkT%d" % i, [128, 10, 128], BF16) for i in range(2)]
    pp = [P.ps("pp%d" % i, [128, 512], F32) for i in range(3)]
    pt1 = P.ps("pt1", [128, 1024], BF16)
    pt2 = P.ps("pt2", [128, 256], BF16)
    wv = win.ap().rearrange("(c p) n -> p c n", p=128)
    for j in range(3):
        P.dma("pool", ws[:, :, j * 512:(j + 1) * 512], wv[:, :, j * 512:(j + 1) * 512], "ws_%d" % j, writes=["ws_%d" % j])
    P.dma("sp", qkg_s[:], qkg.ap(), "qkg", writes=["qkg"])
    for t in range(ntiles):
        b = t % 2
        rows = slice(t * 128, (t + 1) * 128)
        xk, hk, pk_, stk, rk, tk = "xs%d" % b, "hT%d" % b, "pq%d" % b, "st%d" % b, "rot%d" % b, "qkT%d" % b
        sq, ra, rb = sq2[b], ra2[b], rb2[b]
        ksq, kra, krb = "sq%d" % b, "ra%d" % b, "rb%d" % b
        P.dma("sp", xs[b][:], xin.ap()[rows, :], xk, writes=[xk])
        if t >= 2:
            lr = slice((t - 2) * 128, (t - 1) * 128)
            P.dma("sp", cs[b][:, 0, :], cs_d.ap()[lr, :], "cs%da" % b, writes=["cs%d" % b])
            P.dma("sp", cs[b][:, 1, :], sn_d.ap()[lr, :], "cs%db" % b, writes=["cs%d" % b])
        N(xs[b][:], xk, seg_of(t), lambda c, b=b: hT[b][:, c, :], hk)
        for j in range(3):
            for c in range(8):
                P.op("pe", lambda e, c=c, j=j, b=b: e.matmul(pp[j][:], lhsT=hT[b][:, c, :], rhs=ws[:, c, j * 512:(j + 1) * 512],
                                                           start=(c == 0), stop=(c == 7)), reads=[hk, "ws_%d" % j], writes=["pp%d" % j])
        P.op("act", lambda e, b=b, sq=sq, ra=ra, rb=rb: e.activation(out=pq[b][:, 0:512], in_=pp[0][:], func=AF.Copy), reads=["pp0"], writes=[pk_])
        P.op("dve", lambda e, b=b, sq=sq, ra=ra, rb=rb: e.tensor_copy(out=pq[b][:, 512:1024], in_=pp[1][:]), reads=["pp1"], writes=[pk_])
        P.op("act", lambda e, b=b, sq=sq, ra=ra, rb=rb: e.activation(out=pq[b][:, 1024:1280], in_=pp[2][:, 0:256], func=AF.Copy), reads=["pp2"], writes=[pk_])
        P.op("act", lambda e, b=b, sq=sq, ra=ra, rb=rb: e.activation(out=vs[b][:], in_=pp[2][:, 256:512], func=AF.Copy), reads=["pp2"], writes=["vs%d" % b])
        P.dma("sp", v_o.ap()[rows, :], vs[b][:], "vs%d" % b, reads=["vs%d" % b])
        if stage < 2:
            continue
        P.op("pool", lambda e, b=b, sq=sq, ra=ra, rb=rb: e.tensor_tensor(out=sq[:], in0=pq[b][:], in1=pq[b][:], op=ALU.mult), reads=[pk_], writes=[ksq])
        P.op("dve", lambda e, b=b, sq=sq, ra=ra, rb=rb: e.tensor_reduce(out=st[b][:, 0:20], in_=sq[:].rearrange("p (h d) -> p h d", h=20), axis=AX.X, op=ALU.add),
             reads=[ksq], writes=[stk])
        P.op("dve", lambda e, b=b, sq=sq, ra=ra, rb=rb: e.tensor_scalar(out=st[b][:, 20:40], in0=st[b][:, 0:20], scalar1=1.0 / 64, scalar2=EPS, op0=ALU.mult, op1=ALU.add),
             reads=[stk], writes=[stk])
        P.op("act", lambda e, b=b, sq=sq, ra=ra, rb=rb: e.activation(out=st[b][:, 40:60], in_=st[b][:, 20:40], func=AF.Sqrt), reads=[stk], writes=[stk])
        P.op("dve", lambda e, b=b, sq=sq, ra=ra, rb=rb: e.reciprocal(out=st[b][:, 60:80], in_=st[b][:, 40:60]), reads=[stk], writes=[stk])
        P.op("dve", lambda e, b=b, sq=sq, ra=ra, rb=rb: e.tensor_tensor(
            out=sq[:].rearrange("p (h d) -> p h d", h=20), in0=pq[b][:].rearrange("p (h d) -> p h d", h=20),
            in1=st[b][:, 60:80].unsqueeze(2).to_broadcast([128, 20, 64]), op=ALU.mult), reads=[pk_, stk, ksq], writes=[ksq])
        if stage < 3:
            continue
        if t < 2:
            P.op("pool", lambda e, b=b, sq=sq, ra=ra, rb=rb: e.tensor_tensor(out=rot[b][:].rearrange("p (h d) -> p h d", h=20),
                                                       in0=sq[:].rearrange("p (h d) -> p h d", h=20), in1=qkg_s[:], op=ALU.mult),
                 reads=[ksq, "qkg"], writes=[rk])
        else:
            P.op("pool", lambda e, sq=sq: e.tensor_tensor(out=sq[:].rearrange("p (h d) -> p h d", h=20),
                                                  in0=sq[:].rearrange("p (h d) -> p h d", h=20), in1=qkg_s[:], op=ALU.mult),
                 reads=[ksq, "qkg"], writes=[ksq])
            P.op("dve", lambda e, b=b, sq=sq, ra=ra, rb=rb: e.tensor_tensor(
                out=ra[:].rearrange("p (h d) -> p h d", h=20), in0=sq[:].rearrange("p (h d) -> p h d", h=20),
                in1=cs[b][:, 0, :].unsqueeze(1).to_broadcast([128, 20, 64]), op=ALU.mult), reads=[ksq, "cs%d" % b], writes=[kra])
            for hf in range(2):
                P.op("dve", lambda e, b=b, hf=hf, sq=sq, ra=ra, rb=rb: e.tensor_tensor(
                    out=rb[:].rearrange("p (h r a d) -> p h r a d", h=20, r=2, a=2)[:, :, :, hf, :],
                    in0=sq[:].rearrange("p (h r a d) -> p h r a d", h=20, r=2, a=2)[:, :, :, 1 - hf, :],
                    in1=cs[b][:, 1, :].rearrange("p (r a d) -> p r a d", r=2, a=2)[:, :, hf, :].unsqueeze(1).to_broadcast([128, 20, 2, 16]),
                    op=ALU.mult), reads=[ksq, "cs%d" % b], writes=[krb])
            P.op("dve", lambda e, b=b, sq=sq, ra=ra, rb=rb: e.tensor_tensor(out=rot[b][:], in0=ra[:], in1=rb[:], op=ALU.add), reads=[kra, krb], writes=[rk])
        if stage < 4:
            continue
        for c in range(10):
            dst = pt1[:, c * 128:(c + 1) * 128] if c < 8 else pt2[:, (c - 8) * 128:(c - 7) * 128]
            P.op("pe", lambda e, c=c, b=b, dst=dst: e.transpose(out=dst, in_=rot[b][:, c * 128:(c + 1) * 128], identity=N.ident[:]),
                 reads=[rk, "ident"], writes=["pt1" if c < 8 else "pt2"])
        P.op("dve", lambda e, b=b, sq=sq, ra=ra, rb=rb: e.tensor_copy(out=qkT[b][:, 0:8, :], in_=pt1[:].rearrange("p (c n) -> p c n", c=8)), reads=["pt1"], writes=[tk])
        P.op("act", lambda e, b=b, sq=sq, ra=ra, rb=rb: e.activation(out=qkT[b][:, 8:10, :], in_=pt2[:].rearrange("p (c n) -> p c n", c=2), func=AF.Copy),
             reads=["pt2"], writes=[tk])
        if stage < 5:
            continue
        P.dma("sp", qT_o.ap()[:, rows].rearrange("(c p) n -> p c n", p=128), qkT[b][:, 0:8, :], tk + "q", reads=[tk])
        P.dma("sp", kT_o.ap()[:, rows].rearrange("(c p) n -> p c n", p=128), qkT[b][:, 8:10, :], tk + "k", reads=[tk])
    return P


def rope_tables():
    half = 32
    inv = 10000.0 ** (-np.arange(0, half, 2, dtype=np.float32) / half)
    t = np.arange(SEQ)
    row = (t // 64).astype(np.float32)
    col = (t % 64).astype(np.float32)
    ang = np.concatenate([row[:, None] * inv, col[:, None] * inv], -1).astype(np.float32)
    c, s = np.cos(ang), np.sin(ang)
    C = np.concatenate([c[:, :16], c[:, :16], c[:, 16:], c[:, 16:]], -1)
    S = np.concatenate([-s[:, :16], s[:, :16], -s[:, 16:], s[:, 16:]], -1)
    return C.astype(np.float32), S.astype(np.float32)


def launch_a1(xc, w_in, qg, kg, g1, mods, layer):
    C, S = rope_tables()
    gains = np.concatenate([np.broadcast_to(qg[None], (16, 64)), np.broadcast_to(kg[None], (4, 64))], 0)
    maps = []
    for i in range(NCORE):
        b, j = i // 4, i % 4
        m = {"xin": xc[i], "win": w_in, "qkg": bc_rows(gains),
             "ropeC": np.ascontiguousarray(C[j * LAT:(j + 1) * LAT]), "ropeS": np.ascontiguousarray(S[j * LAT:(j + 1) * LAT])}
        m.update(norm_inputs(g1, mods[b, layer], mods[2, layer]))
        maps.append(m)
    return run(build_a1(), maps)


def build_a2g():
    P = Prog()
    NK = NTL
    qT_d = P.dram_in("qT", [256, NTOK], BF16)
    kT_d = P.dram_in("kT", [64, NK * 128], BF16)
    v_d = P.dram_in("v", [NK * 128, 64], BF16)
    o_d = P.dram_out("oT", [4, 64, NTOK], BF16)
    qs = P.sb("qs", [128, 2, NTOK], BF16)
    ks = P.sb("ks", [128, NK * 128], BF16)
    vp = P.sb("vp", [128, NK, 65], BF16)
    oT = P.sb("oT_s", [65, 4, NTOK], BF16)
    ones = P.sb("ones", [1, 65], F32)
    rr = P.sb("rr", [1, 512], F32)
    bcs = P.sb("bcs", [65, 512], F32)
    NPT = 4
    pt = [P.sb("pt%d" % i, [128, 2, 512], BF16) for i in range(NPT)]
    pS = [P.ps("pS%d" % i, [128, 2, 512], F32) for i in range(2)]
    pO = [P.ps("pO%d" % i, [65, 512], F32) for i in range(4)]
    P.op("dve", lambda e: e.memset(ones[:], 1.0), writes=["ones"])
    P.op("dve", lambda e: e.memset(vp[:, :, 0:1], 1.0), writes=["vp1"])
    P.dma("sp", ks[0:64, :], kT_d.ap(), "ks", writes=["ks"])
    P.dma("sp", ks[64:128, :], kT_d.ap(), "ksb", writes=["ksb"])
    P.dma("sp", vp[:, :, 1:65], v_d.ap().rearrange("(t p) d -> p t d", p=128), "vp", writes=["vp"])
    qv = qT_d.ap().rearrange("(hp par p) n -> par p hp n", hp=2, par=2, p=64)
    P.dma("sp", qs[0:64, :, :], qv[0], "qs", writes=["qs"])
    P.dma("sp", qs[64:128, :, :], qv[1], "qsb", writes=["qsb"])
    blocks = [(0, 256, 2)] + [(CTX + 512 * j, 512, NK) for j in range(4)]
    ns = 0
    npt = 0
    LAG = 2
    for (q0, nq, nkt) in blocks:
        steps = [(kt, hp) for kt in range(nkt) for hp in range(2)]
        pend = []
        for si in range(len(steps) + LAG):
            if si < len(steps):
                kt, hp = steps[si]
                sl = ns % 2
                ns += 1
                pl = npt % NPT
                npt += 1
                for hl in range(2):
                    P.op("pe", lambda e, kt=kt, hp=hp, hl=hl, sl=sl, q0=q0, nq=nq: e.matmul(
                        pS[sl][:, hl, 0:nq], lhsT=ks[hl * 64:(hl + 1) * 64, kt * 128:(kt + 1) * 128],
                        rhs=qs[hl * 64:(hl + 1) * 64, hp, q0:q0 + nq], start=True, stop=True),
                        reads=["ks", "ksb", "qs", "qsb"], writes=["pS%d" % sl])
                P.op("act", lambda e, sl=sl, pl=pl, nq=nq: e.activation(out=pt[pl][:, :, 0:nq], in_=pS[sl][:, :, 0:nq], func=AF.Exp, scale=0.125),
                     reads=["pS%d" % sl], writes=["pt%d" % pl])
                pend.append((kt, hp, pl))
            if si >= LAG:
                kt, hp, pl = pend[si - LAG]
                for hl in range(2):
                    h = hp * 2 + hl
                    P.op("pe", lambda e, kt=kt, h=h, hl=hl, pl=pl, nq=nq, nkt=nkt: e.matmul(
                        pO[h][:, 0:nq], lhsT=vp[:, kt, :], rhs=pt[pl][:, hl, 0:nq], start=(kt == 0), stop=(kt == nkt - 1)),
                        reads=["vp", "vp1", "pt%d" % pl], writes=["pO%d" % h])
        for h in range(4):
            sl = ns % 2
            ns += 1
            P.op("dve", lambda e, h=h, nq=nq: e.reciprocal(out=rr[:, 0:nq], in_=pO[h][0:1, 0:nq]), reads=["pO%d" % h], writes=["rr"])
            P.op("pe", lambda e, sl=sl, nq=nq: e.matmul(pS[sl][0:65, 0, 0:nq], lhsT=ones[:], rhs=rr[:, 0:nq], start=True, stop=True),
                 reads=["ones", "rr"], writes=["pS%d" % sl])
            P.op("act", lambda e, sl=sl, nq=nq: e.activation(out=bcs[:, 0:nq], in_=pS[sl][0:65, 0, 0:nq], func=AF.Copy),
                 reads=["pS%d" % sl], writes=["bcs"])
            P.op("dve", lambda e, h=h, q0=q0, nq=nq: e.tensor_tensor(out=oT[:, h, q0:q0 + nq], in0=pO[h][:, 0:nq], in1=bcs[:, 0:nq], op=ALU.mult),
                 reads=["pO%d" % h, "bcs"], writes=["oT"])
    for h in range(4):
        P.dma("sp", o_d.ap()[h], oT[1:65, h, :], "oT%d" % h, reads=["oT"])
    return P


def launch_a2g(a1res):
    outs = [[] for _ in range(NCORE)]
    kTs, vs = [], []
    for b in range(2):
        kTs.append(np.concatenate([a1res[4 * b]["kT"][:, 0:CTX]] + [a1res[4 * b + j]["kT"][:, CTX:] for j in range(4)], 1))
        vs.append(np.concatenate([a1res[4 * b]["v"][0:CTX]] + [a1res[4 * b + j]["v"][CTX:] for j in range(4)], 0))
    for g in range(4):
        maps = []
        for i in range(NCORE):
            b = i // 4
            maps.append({"qT": np.ascontiguousarray(a1res[i]["qT"][g * 256:(g + 1) * 256]),
                         "kT": np.ascontiguousarray(kTs[b][g * 64:(g + 1) * 64]),
                         "v": np.ascontiguousarray(vs[b][:, g * 64:(g + 1) * 64])})
        res = run(build_a2g(), maps)
        for i in range(NCORE):
            outs[i].append(np.asarray(res[i]["oT"]))
    return [np.ascontiguousarray(np.concatenate(o, 0).transpose(1, 0, 2)) for o in outs]


def build_a3():
    P = Prog()
    xin = P.dram_in("xin", [NTOK, D], F32)
    o_d = P.dram_in("oT", [64, 16, NTOK], BF16)
    wo = P.dram_in("wo", [D, D], F32)
    xout = P.dram_out("xout", [NTOK, D], F32)
    R = Residual(P, "gate")
    oT = P.sb("oT_s", [64, 16, NTOK], BF16)
    wos = P.sb("wos", [64, 16, D], BF16)
    xs = [P.sb("xs%d" % i, [128, 1024], F32) for i in range(2)]
    py = [P.ps("py%d" % i, [128, 512], F32) for i in range(2)]
    wv = wo.ap().rearrange("(h p) n -> p h n", p=64)
    for j in range(2):
        P.dma("pool", wos[:, :, j * 512:(j + 1) * 512], wv[:, :, j * 512:(j + 1) * 512], "wos%d" % j, writes=["wos%d" % j])
    for j in range(4):
        P.dma("sp", oT[:, j * 4:(j + 1) * 4, :], o_d.ap()[:, j * 4:(j + 1) * 4, :], "oT%d" % j, writes=["oT%d" % j])
    npy = 0
    for t in range(NT):
        b = t % 2
        rows = slice(t * 128, (t + 1) * 128)
        xk = "xs%d" % b
        P.dma("sp", xs[b][:], xin.ap()[rows, :], xk, writes=[xk])
        for half in range(2):
            p = py[npy % 2]
            pk = "py%d" % (npy % 2)
            npy += 1
            for hh in range(16):
                P.op("pe", lambda e, hh=hh, p=p, half=half, t=t: e.matmul(
                    p[:], lhsT=oT[:, hh, t * 128:(t + 1) * 128], rhs=wos[:, hh, half * 512:(half + 1) * 512],
                    start=(hh == 0), stop=(hh == 15)), reads=["oT%d" % (hh // 4), "wos%d" % half], writes=[pk])
            R(p[:], pk, seg_of(t), half, xs[b][:, half * 512:(half + 1) * 512], xk)
        P.dma("sp", xout.ap()[rows, :], xs[b][:], xk, reads=[xk])
    return P


def launch_a3(xc, oTs, w_out, mods, layer, with_ctx):
    maps = []
    for i in range(NCORE):
        b = i // 4
        maps.append({"xin": xc[i], "oT": oTs[i], "wo": w_out,
                     "gate": np.stack([bc_rows(mods[b, layer, 2 * D:3 * D]), bc_rows(mods[2, layer, 2 * D:3 * D])], 0)})
    res = run(build_a3(), maps)
    out = []
    for i in range(NCORE):
        xo = np.array(res[i]["xout"])
        if not with_ctx:
            xo[0:CTX] = xc[i][0:CTX]
        out.append(xo)
    return out


def kernel(x, c, ctx, c_ctx, ada_w, ada_b, norm1_g, norm2_g, mlp_w1, mlp_w2, hyb_w_in, hyb_gate_b,
           mlstm_norm_g, conv_w, hyb_w_out, att_w_in, q_norm_g, k_norm_g, att_w_out):
    f = lambda a: np.ascontiguousarray(np.asarray(a, np.float32))
    x, c, ctx, c_ctx, ada_w, ada_b = f(x), f(c), f(ctx), f(c_ctx), f(ada_w), f(ada_b)
    mods = launch_mods(c, c_ctx, ada_w, ada_b)
    xc = [np.ascontiguousarray(np.concatenate([ctx[i // 4], x[i // 4, (i % 4) * LAT:(i % 4 + 1) * LAT]], 0)) for i in range(NCORE)]
    for layer in range(4):
        if layer % 2 == 0:
            e = layer // 2
            p1 = launch_p1(xc, f(hyb_w_in[e]), f(hyb_gate_b[e]), f(norm1_g[layer]), mods, layer)
            H = launch_p2(p1)
            xc = launch_p3(xc, H, p1, f(mlstm_norm_g[e]), f(conv_w[e]), f(hyb_w_out[e]), mods, layer)
        else:
            o = layer // 2
            a1 = launch_a1(xc, f(att_w_in[o]), f(q_norm_g[o]), f(k_norm_g[o]), f(norm1_g[layer]), mods, layer)
            oTs = launch_a2g(a1)
            xc = launch_a3(xc, oTs, f(att_w_out[o]), mods, layer, layer != 3)
        xc = launch_mlp(xc, f(mlp_w1[layer]), f(mlp_w2[layer]), f(norm2_g[layer]), mods, layer)
    out = np.zeros((2, SEQ, D), np.float32)
    for i in range(NCORE):
        out[i // 4, (i % 4) * LAT:(i % 4 + 1) * LAT] = xc[i][CTX:]
    return out
```
